# Optimizing a Trainium2 kernel written in Bass

```python
import jax, jax.numpy as jnp
from jax import lax
import numpy as np

D_MODEL = 1024
BATCH = 8
SEQ = 4096
DEPTH = 1

GRID_W = 64
CTX_LEN = 256
MIX_W = D_MODEL
F_W = MIX_W // 2
F_GROUPS = 4
F_GROUP_W = F_W // F_GROUPS
M_W = MIX_W - F_W
M_HEADS = 4
M_HEAD_DIM = M_W // M_HEADS
N_DIRS = 2
N_GATES = N_DIRS * 2 * M_HEADS
MLSTM_END = F_W + 2 * M_W + N_GATES
IN_W = MLSTM_END + M_W
CHUNK = 128
CONV_W = 3
D_FF = 5 * D_MODEL // 2
NORM_EPS = 1e-6

kernel_name = 'hybrid_fourier_mlstm_convffn_dit'


def _rmsnorm(x, w):
    xf = x.astype(jnp.float32)
    y = xf * lax.rsqrt(jnp.mean(xf * xf, axis=-1, keepdims=True) + NORM_EPS)
    return (y * w.astype(jnp.float32)).astype(x.dtype)


def _adaln(cond, w_ada, b_ada):
    mod = jax.nn.silu(cond) @ w_ada + b_ada
    return [m[..., None, :] for m in jnp.split(mod, 6, axis=-1)]


def _modnorm(x, w, shift, scale):
    return _rmsnorm(x, w) * (1 + scale) + shift


def _dwconv1d(x, w, b):
    y = lax.conv_general_dilated(x, w.astype(x.dtype)[:, None, :], (1,), 'SAME',
                                 dimension_numbers=('NWC', 'WIO', 'NWC'),
                                 feature_group_count=x.shape[-1])
    return y + b


def _dwconv_grid(x, w, b):
    bsz, length, ch = x.shape
    rows = length // GRID_W
    xg = x.reshape(bsz, rows, GRID_W, ch)
    y = lax.conv_general_dilated(xg, w.astype(x.dtype)[:, :, None, :], (1, 1), 'SAME',
                                 dimension_numbers=('NHWC', 'HWIO', 'NHWC'),
                                 feature_group_count=ch)
    return y.reshape(bsz, length, ch) + b


def _fourier_mix(u):
    bsz, length, _ = u.shape
    ug = u.astype(jnp.float32).reshape(bsz, length, F_GROUPS, F_GROUP_W)
    y = jnp.fft.fftn(ug, axes=(1, 3), norm='ortho').real
    return y.reshape(bsz, length, F_W).astype(u.dtype)


def _zero_state(bsz):
    return (jnp.zeros((bsz, M_HEADS, M_HEAD_DIM, M_HEAD_DIM), jnp.float32),
            jnp.zeros((bsz, M_HEADS, M_HEAD_DIM), jnp.float32),
            jnp.zeros((bsz, M_HEADS), jnp.float32))


def _state_update(state, k, v, li, lf):
    c_mat, n_vec, m = state
    b = jnp.cumsum(lf, axis=-1)
    b_end = b[..., -1]
    g = b_end[..., None] - b + li
    m_new = jnp.maximum(b_end + m, jnp.max(g, axis=-1))
    w_old = jnp.exp(b_end + m - m_new)
    w_tok = jnp.exp(g - m_new[..., None])
    c_new = w_old[..., None, None] * c_mat + jnp.einsum('bhj,bhjd,bhje->bhde', w_tok, k, v)
    n_new = w_old[..., None] * n_vec + jnp.einsum('bhj,bhjd->bhd', w_tok, k)
    return (c_new, n_new, m_new)


def _mlstm_chunk(state, inp):
    c_mat, n_vec, m = state
    q, k, v, li, lf = inp
    length = q.shape[-2]
    b = jnp.cumsum(lf, axis=-1)
    lower = jnp.tril(jnp.ones((length, length), dtype=bool))
    dmat = jnp.where(lower, b[..., :, None] - b[..., None, :] + li[..., None, :], -jnp.inf)
    inter = b + m[..., None]
    m_t = jnp.maximum(inter, jnp.max(dmat, axis=-1))
    w_inter = jnp.exp(inter - m_t)
    s = jnp.einsum('bhld,bhjd->bhlj', q, k) * jnp.exp(dmat - m_t[..., None])
    num = w_inter[..., None] * jnp.einsum('bhld,bhde->bhle', q, c_mat) + jnp.einsum('bhlj,bhje->bhle', s, v)
    den = w_inter * jnp.einsum('bhld,bhd->bhl', q, n_vec) + jnp.sum(s, axis=-1)
    h = num / jnp.maximum(jnp.abs(den), jnp.exp(-m_t))[..., None]
    return _state_update(state, k, v, li, lf), h


def _mlstm_scan(q, k, v, li, lf, state0):
    bsz, heads, length, dh = q.shape
    nc = length // CHUNK

    def chunks(t):
        return jnp.moveaxis(t.reshape(bsz, heads, nc, CHUNK, *t.shape[3:]), 2, 0)

    state, h = lax.scan(_mlstm_chunk, state0, (chunks(q), chunks(k), chunks(v), chunks(li), chunks(lf)))
    return jnp.moveaxis(h, 0, 2).reshape(bsz, heads, length, dh), state


def _mlstm_inputs(p, mconv_w, mconv_b, w_k, b_gate):
    bsz, length, _ = p.shape
    xm, v, g = jnp.split(p, [M_W, 2 * M_W], axis=-1)
    a = jax.nn.silu(_dwconv1d(xm, mconv_w, mconv_b))
    ah = a.reshape(bsz, length, M_HEADS, M_HEAD_DIM)
    k = (jnp.einsum('blhd,hde->bhle', ah, w_k) * M_HEAD_DIM ** -0.5).astype(jnp.float32)
    vh = v.reshape(bsz, length, M_HEADS, M_HEAD_DIM).transpose(0, 2, 1, 3).astype(jnp.float32)
    g = (g + b_gate).astype(jnp.float32).reshape(bsz, length, N_DIRS, 2, M_HEADS)
    g = jnp.transpose(g, (2, 3, 0, 4, 1))
    li = g[:, 0]
    lf = jax.nn.log_sigmoid(g[:, 1])
    return a, k, vh, li, lf


def _mlstm_head_out(h, a, z, mnorm_w, m_skip):
    bsz, heads, length, dh = h.shape
    mu = jnp.mean(h, axis=-1, keepdims=True)
    var = jnp.mean(jnp.square(h - mu), axis=-1, keepdims=True)
    hn = ((h - mu) * lax.rsqrt(var + NORM_EPS)).transpose(0, 2, 1, 3).reshape(bsz, length, M_W)
    hn = (hn * mnorm_w.astype(jnp.float32)).astype(a.dtype)
    return (hn + m_skip * a) * jax.nn.silu(z)


def _token_mixer(h, w_in, w_out, mconv_w, mconv_b, w_q, w_k, b_gate, mnorm_w, m_skip, st_f, st_b):
    bsz, length, _ = h.shape
    proj = h @ w_in
    a, k, v, li, lf = _mlstm_inputs(proj[..., F_W:MLSTM_END], mconv_w, mconv_b, w_k, b_gate)
    q = jnp.einsum('blhd,hde->bhle', a.reshape(bsz, length, M_HEADS, M_HEAD_DIM), w_q).astype(jnp.float32)
    h_f, fin_f = _mlstm_scan(q, k, v, li[0], lf[0], st_f)
    h_b, fin_b = _mlstm_scan(jnp.flip(q, 2), jnp.flip(k, 2), jnp.flip(v, 2),
                             jnp.flip(li[1], -1), jnp.flip(lf[1], -1), st_b)
    y_m = _mlstm_head_out(h_f + jnp.flip(h_b, 2), a, proj[..., MLSTM_END:], mnorm_w, m_skip)
    y = jnp.concatenate([_fourier_mix(proj[..., :F_W]), y_m], axis=-1) @ w_out
    return y, fin_f, fin_b


def _context_states(p, mconv_w, mconv_b, w_k, b_gate):
    _, k, v, li, lf = _mlstm_inputs(p, mconv_w, mconv_b, w_k, b_gate)
    zero = _zero_state(p.shape[0])
    st_f = _state_update(zero, k, v, li[0], lf[0])
    st_b = _state_update(zero, jnp.flip(k, 2), jnp.flip(v, 2), jnp.flip(li[1], -1), jnp.flip(lf[1], -1))
    return st_f, st_b


def _conv_ffn(h, w_up, b_up, fconv_w, fconv_b, w_down, b_down, on_grid):
    u = h @ w_up + b_up
    u = _dwconv_grid(u, fconv_w, fconv_b) if on_grid else _dwconv1d(u, fconv_w[1], fconv_b)
    val, gate = jnp.split(u, 2, axis=-1)
    return (val * jax.nn.silu(gate)) @ w_down + b_down


def setup_inputs(seed: int = 0) -> dict:
    key = jax.random.key(seed)
    ks = jax.random.split(key, 28)
    f32 = jnp.float32

    def nrm(k, shape, scale):
        return jax.random.normal(k, shape, f32) * scale

    L = DEPTH
    ig = nrm(ks[12], (L, N_DIRS, 1, M_HEADS), 0.1)
    fg = jnp.linspace(3.0, 6.0, M_HEADS, dtype=f32) + nrm(ks[13], (L, N_DIRS, 1, M_HEADS), 0.1)
    return {
        'x': nrm(ks[0], (BATCH, SEQ, D_MODEL), 1.0),
        'c': nrm(ks[1], (BATCH, D_MODEL), 1.0),
        'ctx': nrm(ks[2], (BATCH, CTX_LEN, D_MODEL), 1.0),
        'c_ctx': nrm(ks[3], (D_MODEL,), 1.0),
        'w_ada': nrm(ks[4], (L, D_MODEL, 6 * D_MODEL), 0.5 * D_MODEL ** -0.5),
        'b_ada': nrm(ks[5], (L, 6 * D_MODEL), 0.02),
        'norm1_w': 1.0 + nrm(ks[6], (L, D_MODEL), 0.02),
        'w_in': nrm(ks[7], (L, D_MODEL, IN_W), D_MODEL ** -0.5),
        'mconv_w': nrm(ks[8], (L, CONV_W, M_W), CONV_W ** -0.5),
        'mconv_b': nrm(ks[9], (L, M_W), 0.02),
        'w_q': nrm(ks[10], (L, M_HEADS, M_HEAD_DIM, M_HEAD_DIM), M_HEAD_DIM ** -0.5),
        'w_k': nrm(ks[11], (L, M_HEADS, M_HEAD_DIM, M_HEAD_DIM), M_HEAD_DIM ** -0.5),
        'b_gate': jnp.concatenate([ig, fg], axis=2).reshape(L, N_GATES),
        'mnorm_w': 1.0 + nrm(ks[14], (L, M_W), 0.02),
        'm_skip': 1.0 + nrm(ks[15], (L, M_W), 0.02),
        'w_out': nrm(ks[16], (L, MIX_W, D_MODEL), MIX_W ** -0.5),
        'norm2_w': 1.0 + nrm(ks[17], (L, D_MODEL), 0.02),
        'w_up': nrm(ks[18], (L, D_MODEL, 2 * D_FF), D_MODEL ** -0.5),
        'b_up': nrm(ks[19], (L, 2 * D_FF), 0.02),
        'fconv_w': nrm(ks[20], (L, CONV_W, CONV_W, 2 * D_FF), 1.0 / CONV_W),
        'fconv_b': nrm(ks[21], (L, 2 * D_FF), 0.02),
        'w_down': nrm(ks[22], (L, D_FF, D_MODEL), D_FF ** -0.5),
        'b_down': nrm(ks[23], (L, D_MODEL), 0.02),
        'final_norm_w': 1.0 + nrm(ks[24], (D_MODEL,), 0.02),
    }


def reference(x, c, ctx, c_ctx, w_ada, b_ada, norm1_w, w_in, mconv_w, mconv_b, w_q, w_k, b_gate,
              mnorm_w, m_skip, w_out, norm2_w, w_up, b_up, fconv_w, fconv_b, w_down, b_down,
              final_norm_w):
    bsz = x.shape[0]
    for l in range(DEPTH):
        sh1, sc1, g1, sh2, sc2, g2 = _adaln(c, w_ada[l], b_ada[l])
        sh1c, sc1c, g1c, sh2c, sc2c, g2c = _adaln(c_ctx, w_ada[l], b_ada[l])
        mix_p = (w_in[l], w_out[l], mconv_w[l], mconv_b[l], w_q[l], w_k[l], b_gate[l], mnorm_w[l], m_skip[l])
        ffn_p = (w_up[l], b_up[l], fconv_w[l], fconv_b[l], w_down[l], b_down[l])

        hc = _modnorm(ctx, norm1_w[l], sh1c, sc1c)
        if l + 1 < DEPTH:
            zero = _zero_state(bsz)
            yc, st_f, st_b = _token_mixer(hc, *mix_p, zero, zero)
            ctx = ctx + g1c * yc
            ctx = ctx + g2c * _conv_ffn(_modnorm(ctx, norm2_w[l], sh2c, sc2c), *ffn_p, False)
        else:
            st_f, st_b = _context_states(hc @ w_in[l][:, F_W:MLSTM_END], mconv_w[l], mconv_b[l], w_k[l], b_gate[l])

        hx = _modnorm(x, norm1_w[l], sh1, sc1)
        y, _, _ = _token_mixer(hx, *mix_p, st_f, st_b)
        x = x + g1 * y
        x = x + g2 * _conv_ffn(_modnorm(x, norm2_w[l], sh2, sc2), *ffn_p, True)
    return _rmsnorm(x, final_norm_w)
```

```python
import contextlib
import numpy as np
import ml_dtypes
import concourse.bass as bass
import concourse.mybir as mybir
from concourse.bass_utils import run_bass_kernel_spmd

F32 = mybir.dt.float32
BF16 = mybir.dt.bfloat16
ALU = mybir.AluOpType
AF = mybir.ActivationFunctionType
AX = mybir.AxisListType

T = 4096
D = 1024
CT = 256
NT = T // 128
EPS = 1e-6
import os
EVY_ACT = os.environ.get('EVY_ACT', '0') == '1'
SAME_ENG_SYNC = {'act': True, 'dve': True, 'pool': True, 'pe': False, 'sp': False}

V_N1W, V_N2W, V_BADA, V_MCW, V_MCB, V_MNW, V_MSK, V_BUP, V_FCW, V_FCB, V_BG, V_END = (
    0, 8, 16, 64, 76, 80, 84, 88, 128, 488, 528, 529)


def _merge(d, s):
    for k, v in s.items():
        if d.get(k, 0) < v:
            d[k] = v


class Buf:
    __slots__ = ('name', 'w', 'r', 'sem', 'semval', 'dram', 'key', 'excl')

    def __init__(self, name, dram=False):
        self.name = name
        self.w = {}
        self.r = {}
        self.sem = None
        self.semval = 0
        self.dram = dram
        self.key = None
        self.excl = name.startswith('p')


class K:
    def __init__(self, nc, es):
        self.nc = nc
        self.es = es
        self.eng = {'pe': nc.tensor, 'act': nc.scalar, 'dve': nc.vector, 'pool': nc.gpsimd, 'sp': nc.sync}
        self.sem = {}
        self.cur = {}
        self.waited = {n: {} for n in self.eng}
        for n in self.eng:
            self.sem[n] = es.enter_context(nc.semaphore('s_' + n))
            self.cur[n] = 0
        self.nbuf = 0
        self.ninst = 0

    def buf(self, name, dram=False):
        self.nbuf += 1
        return Buf('%s_%d' % (name, self.nbuf), dram)

    def bufs(self, name, n):
        return [self.buf(name) for _ in range(n)]

    def _wait(self, e, deps):
        for key, val in deps.items():
            if key == e and not SAME_ENG_SYNC[e]:
                continue
            if self.waited[e].get(key, 0) >= val:
                continue
            self.eng[e].wait_ge(self.sem[key], val)
            self.waited[e][key] = val
            self.ninst += 1

    def _deps(self, reads, writes):
        deps = {}
        for b in reads:
            _merge(deps, b.w)
            if b.excl:
                _merge(deps, b.r)
        for b in writes:
            _merge(deps, b.w)
            _merge(deps, b.r)
        return deps

    def _book(self, key, val, reads, writes):
        for b in writes:
            if b.dram:
                if b.w.get(key, 0) < val:
                    b.w[key] = val
            else:
                b.w = {key: val}
                b.r = {}
        for b in reads:
            if b.r.get(key, 0) < val:
                b.r[key] = val

    def op(self, e, fn, reads=(), writes=(), aw=()):
        deps = self._deps(reads, writes)
        for b in aw:
            _merge(deps, b.r)
        self._wait(e, deps)
        ins = fn()
        self.cur[e] += 1
        ins.then_inc(self.sem[e], 1)
        self.ninst += 1
        self._book(e, self.cur[e], reads, writes)
        for b in aw:
            if b.w.get(e, 0) < self.cur[e]:
                b.w[e] = self.cur[e]

    def dma(self, q, out, in_, reads, writes, sb):
        self._wait(q, self._deps(reads, writes))
        if sb.sem is None:
            sb.key = 'd_' + sb.name
            sb.sem = self.es.enter_context(self.nc.semaphore(sb.key))
            self.sem[sb.key] = sb.sem
            self.cur[sb.key] = 0
        self.cur[sb.key] += 16
        self.eng[q].dma_start(out=out, in_=in_).then_inc(sb.sem, 16)
        self.ninst += 1
        self._book(sb.key, self.cur[sb.key], reads, writes)

    def barrier(self, engines=None):
        allv = dict(self.cur)
        for e in (engines or self.eng):
            self._wait(e, {k: v for k, v in allv.items() if v > 0 and k != e})


def build(dbg=None):
    nc = bass.Bass("TRN2", target_bir_lowering=False)
    es = contextlib.ExitStack()
    with es:
        _build(nc, es, dbg)
    return nc


def _build(nc, es, dbg):
    k = K(nc, es)

    def din(name, shape, dt=F32):
        return nc.dram_tensor(name, list(shape), dt, kind="ExternalInput").ap()

    x_d = din("x", [T, D])
    ctx_d = din("ctx", [CT, D])
    cc_d = din("cc", [128, 16])
    vec_d = din("vecs", [128, V_END])
    bada_d = din("b_ada", [1, 6 * D])
    bdn_d = din("b_down", [1, D])
    fnw_d = din("final_norm_w", [1, D])
    wada_d = din("w_ada", [D, 6 * D])
    win_d = din("w_in", [D, 2064])
    wq_d = din("w_q", [4, 128, 128])
    wk_d = din("w_k", [4, 128, 128])
    wout_d = din("w_out", [D, D])
    wup_d = din("w_up", [D, 5120])
    wdn_d = din("w_down", [2560, D])
    idb_d = din("ident_bf", [128, 128], BF16)
    idf_d = din("ident_f", [128, 128])
    msk_d = din("masks", [128, 256])
    sel_d = din("sel", [4, 512])
    t1_d = din("dft1", [128, 32 * 256], BF16)
    w2_d = din("dft2", [128, 64], BF16)
    cd_d = din("dftc", [128, 256], BF16)
    out_d = nc.dram_tensor("out", [T, D], F32, kind="ExternalOutput").ap()

    U_d = nc.dram_tensor("scr_u", [T, 512], BF16).ap()
    ZD_d = nc.dram_tensor("scr_z", [2, 32, 128, 512], BF16).ap()
    SZ_d = nc.dram_tensor("scr_sz", [4, 128, T], BF16).ap()
    YT_d = nc.dram_tensor("scr_yt", [8, 128, T], BF16).ap()
    X1_d = nc.dram_tensor("scr_x1", [T, D], F32).ap()
    HT_d = nc.dram_tensor("scr_ht", [20, 128, T], BF16).ap()
    bU, bZD, bSZ, bYT, bX1, bHT = [k.buf(n, True) for n in ('U', 'ZD', 'SZ', 'YT', 'X1', 'HT')]
    bIN = k.buf('inputs', True)
    GD_d = nc.dram_tensor("scr_g", [16, T + CT], F32).ap()
    bGD = k.buf('GD', True)

    dbg_out = {}

    def dbg_tensor(name, shape, dt=F32):
        t = nc.dram_tensor("dbg_" + name, list(shape), dt, kind="ExternalOutput").ap()
        dbg_out[name] = t
        return t

    def sb(ph, name, shape, dt=F32):
        return ph.enter_context(nc.sbuf_tensor("sb_" + name, list(shape), dt))

    def ps(ph, name, shape, dt=F32):
        return ph.enter_context(nc.psum_tensor("ps_" + name, list(shape), dt))

    act, dve, pool, pe = nc.scalar, nc.vector, nc.gpsimd, nc.tensor

    with contextlib.ExitStack() as g0:
        vec = sb(g0, "vec", [128, V_END]); b_vec = k.buf('vec')
        idb = sb(g0, "idb", [128, 128], BF16); b_idb = k.buf('idb')
        idf = sb(g0, "idf", [128, 128]); b_idf = k.buf('idf')
        modT = sb(g0, "modT", [128, 48, 2]); b_modT = k.buf('modT')
        s1 = sb(g0, "s1", [128, 8, 2]); b_s1 = k.buf('s1')
        s2 = sb(g0, "s2", [128, 8]); b_s2 = k.buf('s2')
        g1b = sb(g0, "g1b", [128, D]); b_g1b = k.buf('g1b')
        g2b = sb(g0, "g2b", [128, D]); b_g2b = k.buf('g2b')
        k.dma('sp', vec[:], vec_d, [bIN], [b_vec], b_vec)
        k.dma('sp', idb[:], idb_d, [bIN], [b_idb], b_idb)
        k.dma('sp', idf[:], idf_d, [bIN], [b_idf], b_idf)

        with contextlib.ExitStack() as ph:
            wada = sb(ph, "wada", [128, 8, 6 * D], BF16); b_wada = k.bufs('wada', 8)
            cc = sb(ph, "cc", [128, 16]); b_cc = k.buf('cc')
            scc = sb(ph, "scc", [128, 8, 2], BF16); b_scc = k.buf('scc')
            rep = sb(ph, "rep", [128, 8, 128], BF16); b_rep = k.buf('rep')
            badab = sb(ph, "badab", [128, 2, D]); b_badab = k.buf('badab')
            psm = ps(ph, "psm", [128, 48, 2]); b_psm = k.buf('psm')
            psg = [ps(ph, "psg%d" % i, [128, 512]) for i in range(4)]; b_psg = k.bufs('psg', 4)
            k.dma('sp', cc[:], cc_d, [bIN], [b_cc], b_cc)
            wv = wada_d.rearrange("(j p) n -> p j n", p=128)
            for j in range(8):
                k.dma('pool', wada[:, j, :], wv[:, j, :], [bIN], [b_wada[j]], b_wada[j])
            k.dma('sp', badab[:, 0, :], bada_d[:, 2 * D:3 * D].partition_broadcast(128), [bIN], [b_badab], b_badab)
            k.dma('sp', badab[:, 1, :], bada_d[:, 5 * D:6 * D].partition_broadcast(128), [bIN], [b_badab], b_badab)
            k.op('act', lambda: act.activation(out=scc[:].rearrange("p j r -> p (j r)"), in_=cc[:], func=AF.Silu),
                 [b_cc], [b_scc])
            for j in range(8):
                k.op('dve', lambda j=j: dve.tensor_copy(out=rep[:, j, :], in_=scc[:, j, 0:1].to_broadcast([128, 128])),
                     [b_scc], [b_rep])
            secs = [0, 1, 3, 4]

            def mm_mod():
                last = None
                for s in secs:
                    for jj in range(8):
                        col = s * 8 + jj
                        for kk in range(8):
                            last = pe.matmul(psm[:, col, :], lhsT=wada[:, kk, col * 128:(col + 1) * 128],
                                             rhs=scc[:, kk, :], start=(kk == 0), stop=(kk == 7))
                return last
            k.op('pe', mm_mod, b_wada + [b_scc], [b_psm])
            k.op('dve', lambda: dve.tensor_tensor(
                out=modT[:], in0=psm[:], in1=vec[:, V_BADA:V_BADA + 48].unsqueeze(2).to_broadcast([128, 48, 2]),
                op=ALU.add), [b_psm, b_vec], [b_modT])
            k.op('dve', lambda: dve.scalar_tensor_tensor(
                out=s1[:], in0=modT[:, 8:16, :], scalar=1.0,
                in1=vec[:, V_N1W:V_N1W + 8].unsqueeze(2).to_broadcast([128, 8, 2]),
                op0=ALU.add, op1=ALU.mult), [b_modT, b_vec], [b_s1])
            k.op('dve', lambda: dve.scalar_tensor_tensor(
                out=s2[:], in0=modT[:, 32:40, 0], scalar=1.0, in1=vec[:, V_N2W:V_N2W + 8],
                op0=ALU.add, op1=ALU.mult), [b_modT, b_vec], [b_s2])
            for gi, sec in enumerate((2, 5)):
                for hf in range(2):
                    pt = psg[gi * 2 + hf]
                    c0 = sec * D + hf * 512

                    def mm_g(pt=pt, c0=c0):
                        last = None
                        for kk in range(8):
                            last = pe.matmul(pt[:], lhsT=rep[:, kk, :], rhs=wada[:, kk, c0:c0 + 512],
                                             start=(kk == 0), stop=(kk == 7))
                        return last
                    k.op('pe', mm_g, b_wada + [b_rep], [b_psg[gi * 2 + hf]])
                    dst = (g1b, g2b)[gi]
                    bd = (b_g1b, b_g2b)[gi]
                    k.op('dve', lambda pt=pt, dst=dst, gi=gi, hf=hf: dve.tensor_tensor(
                        out=dst[:, hf * 512:(hf + 1) * 512], in0=pt[:], in1=badab[:, gi, hf * 512:(hf + 1) * 512],
                        op=ALU.add), [b_psg[gi * 2 + hf], b_badab], [bd])
            if dbg == 'p0':
                t = dbg_tensor('modT', [128, 96])
                k.dma('sp', t, modT[:].rearrange("p a b -> p (a b)"), [b_modT], [], b_modT)
                t = dbg_tensor('g1b', [128, D])
                k.dma('sp', t, g1b[:], [b_g1b], [], b_g1b)
                t = dbg_tensor('s1', [128, 16])
                k.dma('sp', t, s1[:].rearrange("p a b -> p (a b)"), [b_s1], [], b_s1)
            k.barrier()
        if dbg == 'p0':
            k.barrier()
            return

        with contextlib.ExitStack() as gm:
            xmT = sb(gm, "xmT", [128, 4, T + 2], BF16); b_xmT = k.bufs('xmT', 4)
            xcT = sb(gm, "xcT", [128, 4, CT + 2], BF16); b_xcT = k.bufs('xcT', 4)
            Vaug = sb(gm, "Vaug", [128, NT, 4, 130], BF16); b_V = k.bufs('V', NT)
            Vc = sb(gm, "Vc", [128, 2, 4, 130], BF16); b_Vc = k.bufs('Vc', 2)
            with contextlib.ExitStack() as ph:
                hxT = sb(ph, "hxT", [128, 8, T], BF16); b_hxT = k.bufs('hxT', NT)
                hcT = sb(ph, "hcT", [128, 8, CT], BF16); b_hcT = k.bufs('hcT', 2)
                win = sb(ph, "win", [128, 8, 2064], BF16); b_win = k.bufs('win', 8)
                wvw = win_d.rearrange("(j p) n -> p j n", p=128)
                for j in range(8):
                    k.dma('pool', win[:, j, :], wvw[:, j, :], [bIN], [b_win[j]], b_win[j])
                with contextlib.ExitStack() as p1:
                    NB = 3
                    xt = [sb(p1, "xt%d" % i, [128, D]) for i in range(NB)]; b_xt = k.bufs('xt', NB)
                    xn = [sb(p1, "xn%d" % i, [128, D], BF16) for i in range(2)]; b_xn = k.bufs('xn', 2)
                    sq = sb(p1, "sq", [128, D], BF16); b_sq = k.buf('sq')
                    st = [sb(p1, "st%d" % i, [128, 2]) for i in range(2)]; b_st = k.bufs('st', 2)
                    ptrA = [ps(p1, "ptrA%d" % i, [128, 4, 128], BF16) for i in range(2)]; b_ptrA = k.bufs('ptrA', 2)
                    ptrB = [ps(p1, "ptrB%d" % i, [128, 4, 128], BF16) for i in range(2)]; b_ptrB = k.bufs('ptrB', 2)
                    tiles = [('c', i) for i in range(2)] + [('x', i) for i in range(NT)]

                    def load(n):
                        kind, i = tiles[n]
                        src = (ctx_d if kind == 'c' else x_d)[i * 128:(i + 1) * 128, :]
                        k.dma('sp', xt[n % NB][:], src, [bIN], [b_xt[n % NB]], b_xt[n % NB])
                    load(0); load(1)

                    def p1A(n):
                        kind, i = tiles[n]
                        if n + 2 < len(tiles):
                            load(n + 2)
                        X = xt[n % NB]; bX = b_xt[n % NB]
                        S = st[n % 2]; bS = b_st[n % 2]
                        XN = xn[n % 2]; bXN = b_xn[n % 2]
                        PA = ptrA[n % 2]; bPA = b_ptrA[n % 2]; PB = ptrB[n % 2]; bPB = b_ptrB[n % 2]
                        k.op('act', lambda X=X, S=S: act.activation(out=sq[:], in_=X[:], func=AF.Square, accum_out=S[:, 0:1]),
                             [bX], [b_sq, bS])
                        k.op('dve', lambda S=S: dve.tensor_scalar(out=S[:, 1:2], in0=S[:, 0:1], scalar1=1.0 / D, scalar2=EPS,
                                                                  op0=ALU.mult, op1=ALU.add), [bS], [bS])
                        k.op('act', lambda S=S: act.activation(out=S[:, 1:2], in_=S[:, 1:2], func=AF.Sqrt), [bS], [bS])
                        k.op('dve', lambda S=S: dve.reciprocal(out=S[:, 1:2], in_=S[:, 1:2]), [bS], [bS])
                        k.op('dve', lambda X=X, S=S, XN=XN: dve.tensor_scalar(out=XN[:], in0=X[:], scalar1=S[:, 1:2], scalar2=None, op0=ALU.mult),
                             [bX, bS], [bXN])

                        def tr(XN=XN, PA=PA, PB=PB):
                            last = None
                            for j in range(8):
                                last = pe.transpose((PA if j % 2 == 0 else PB)[:, j // 2, :], XN[:, j * 128:(j + 1) * 128], idb[:])
                            return last
                        k.op('pe', tr, [bXN, b_idb], [bPA, bPB])

                    def p1B(n):
                        kind, i = tiles[n]
                        PA = ptrA[n % 2]; bPA = b_ptrA[n % 2]; PB = ptrB[n % 2]; bPB = b_ptrB[n % 2]
                        col = 1 if kind == 'c' else 0
                        dstT = hcT if kind == 'c' else hxT
                        bD = (b_hcT if kind == 'c' else b_hxT)[i]
                        for j in range(8):
                            if j % 2 == 1:
                                k.op('act', lambda j=j, PB=PB, dstT=dstT, i=i, col=col: act.activation(
                                    out=dstT[:, j, i * 128:(i + 1) * 128], in_=PB[:, j // 2, :], func=AF.Identity,
                                    scale=s1[:, j, col:col + 1], bias=modT[:, j, col:col + 1]),
                                    [bPB, b_s1, b_modT], [], aw=[bD])
                            else:
                                k.op('dve', lambda j=j, PA=PA, dstT=dstT, i=i, col=col: dve.tensor_scalar(
                                    out=dstT[:, j, i * 128:(i + 1) * 128], in0=PA[:, j // 2, :],
                                    scalar1=s1[:, j, col:col + 1], scalar2=modT[:, j, col:col + 1],
                                    op0=ALU.mult, op1=ALU.add), [bPA, b_s1, b_modT], [], aw=[bD])
                    p1A(0)
                    for n in range(len(tiles)):
                        if n + 1 < len(tiles):
                            p1A(n + 1)
                        p1B(n)
                    if dbg == 'p1':
                        t = dbg_tensor('hxT', [128, 8 * T], BF16)
                        k.dma('sp', t, hxT[:].rearrange("p a b -> p (a b)"), b_hxT, [], b_hxT[0])
                        t = dbg_tensor('hcT', [128, 8 * CT], BF16)
                        k.dma('sp', t, hcT[:].rearrange("p a b -> p (a b)"), b_hcT, [], b_hcT[0])
                    k.barrier()
                if dbg == 'p1':
                    k.barrier()
                    return
                with contextlib.ExitStack() as p2:
                    pA = [ps(p2, "pA%d" % i, [128, 512]) for i in range(4)]; b_pA = k.bufs('pA', 4)
                    ust = [sb(p2, "ust%d" % i, [128, 512], BF16) for i in range(2)]; b_ust = k.bufs('ust', 2)
                    szs = [sb(p2, "szs%d" % i, [128, 512], BF16) for i in range(2)]; b_szs = k.bufs('szs', 2)
                    gst = [sb(p2, "gst%d" % i, [16, 512]) for i in range(2)]; b_gst = k.bufs('gst', 2)
                    pi = [0]

                    def nextp():
                        pi[0] += 1
                        return pA[pi[0] % 4], b_pA[pi[0] % 4]
                    k.op('dve', lambda: dve.memset(Vaug[:, :, :, 128:129], 1.0), [], b_V)
                    k.op('dve', lambda: dve.memset(Vc[:, :, :, 128:129], 1.0), [], b_Vc)
                    k.op('dve', lambda: dve.memset(xmT[:, :, 0:1], 0.0), [], b_xmT)
                    k.op('dve', lambda: dve.memset(xmT[:, :, T + 1:T + 2], 0.0), [], b_xmT)
                    k.op('dve', lambda: dve.memset(xcT[:, :, 0:1], 0.0), [], b_xcT)
                    k.op('dve', lambda: dve.memset(xcT[:, :, CT + 1:CT + 2], 0.0), [], b_xcT)
                    for i in range(NT):
                        P, bP = nextp()

                        def mm(P=P, i=i, c0=0):
                            last = None
                            for kk in range(8):
                                last = pe.matmul(P[:], lhsT=hxT[:, kk, i * 128:(i + 1) * 128], rhs=win[:, kk, c0:c0 + 512],
                                                 start=(kk == 0), stop=(kk == 7))
                            return last
                        k.op('pe', mm, [b_hxT[i]] + b_win, [bP])
                        us = ust[i % 2]; bus = b_ust[i % 2]
                        k.op('act', lambda P=P, us=us: act.copy(out=us[:], in_=P[:]), [bP], [bus])
                        k.dma('pool', U_d[i * 128:(i + 1) * 128, :], us[:], [bus], [bU], bus)
                        P, bP = nextp()
                        k.op('pe', lambda P=P, i=i: mm(P, i, 1024), [b_hxT[i]] + b_win, [bP])
                        k.op('dve', lambda P=P, i=i: dve.tensor_copy(out=Vaug[:, i, :, 0:128],
                                                                   in_=P[:].rearrange("p (h e) -> p h e", h=4)),
                             [bP], [b_V[i]])
                    for i in range(2):
                        P, bP = nextp()

                        def mmc(P=P, i=i):
                            last = None
                            for kk in range(8):
                                last = pe.matmul(P[:], lhsT=hcT[:, kk, i * 128:(i + 1) * 128], rhs=win[:, kk, 1024:1536],
                                                 start=(kk == 0), stop=(kk == 7))
                            return last
                        k.op('pe', mmc, [b_hcT[i]] + b_win, [bP])
                        k.op('dve', lambda P=P, i=i: dve.tensor_copy(out=Vc[:, i, :, 0:128],
                                                                   in_=P[:].rearrange("p (h e) -> p h e", h=4)),
                             [bP], [b_Vc[i]])
                    for tt in range(8):
                        hsl = b_hxT[tt * 4:(tt + 1) * 4]
                        for ch in range(4):
                            for which in range(2):
                                c0 = (512 if which == 0 else 1552) + ch * 128
                                P, bP = nextp()

                                def mmf(P=P, c0=c0, tt=tt, M=128):
                                    last = None
                                    for kk in range(8):
                                        last = pe.matmul(P[0:M, :], lhsT=win[:, kk, c0:c0 + M], rhs=hxT[:, kk, tt * 512:(tt + 1) * 512],
                                                         start=(kk == 0), stop=(kk == 7))
                                    return last
                                k.op('pe', mmf, hsl + b_win, [bP])
                                if which == 0:
                                    k.op('dve', lambda P=P, ch=ch, tt=tt: dve.tensor_copy(
                                        out=xmT[:, ch, 1 + tt * 512:1 + (tt + 1) * 512], in_=P[:]), [bP], [b_xmT[ch]])
                                else:
                                    zs = szs[(tt * 4 + ch) % 2]; bzs = b_szs[(tt * 4 + ch) % 2]
                                    k.op('act', lambda P=P, zs=zs: act.activation(out=zs[:], in_=P[:], func=AF.Silu), [bP], [bzs])
                                    k.dma('pool', SZ_d[ch, :, tt * 512:(tt + 1) * 512], zs[:], [bzs], [bSZ], bzs)
                        P, bP = nextp()
                        k.op('pe', lambda P=P, tt=tt: mmf(P, 1536, tt, 16), hsl + b_win, [bP])
                        gs = gst[tt % 2]; bgs = b_gst[tt % 2]
                        k.op('act', lambda P=P, gs=gs: act.activation(out=gs[:], in_=P[0:16, :],
                                                                     func=AF.Identity, bias=vec[0:16, V_BG:V_BG + 1]),
                             [bP, b_vec], [bgs])
                        k.dma('pool', GD_d[:, tt * 512:(tt + 1) * 512], gs[:], [bgs], [bGD], bgs)
                    for ch in range(4):
                        P, bP = nextp()

                        def mmx(P=P, c0=512 + ch * 128, M=128):
                            last = None
                            for kk in range(8):
                                last = pe.matmul(P[0:M, 0:CT], lhsT=win[:, kk, c0:c0 + M], rhs=hcT[:, kk, :],
                                                 start=(kk == 0), stop=(kk == 7))
                            return last
                        k.op('pe', mmx, b_hcT + b_win, [bP])
                        k.op('dve', lambda P=P, ch=ch: dve.tensor_copy(out=xcT[:, ch, 1:1 + CT], in_=P[:, 0:CT]), [bP], [b_xcT[ch]])
                    P, bP = nextp()
                    k.op('pe', lambda P=P: mmx(P, 1536, 16), b_hcT + b_win, [bP])
                    k.op('act', lambda P=P: act.activation(out=gst[0][:, 0:CT], in_=P[0:16, 0:CT], func=AF.Identity,
                                                           bias=vec[0:16, V_BG:V_BG + 1]), [bP, b_vec], [b_gst[0]])
                    k.dma('pool', GD_d[:, T:T + CT], gst[0][:, 0:CT], [b_gst[0]], [bGD], b_gst[0])
                    if dbg == 'p2':
                        t = dbg_tensor('xmT', [128, 4 * (T + 2)], BF16)
                        k.dma('sp', t, xmT[:].rearrange("p a b -> p (a b)"), b_xmT, [], b_xmT[0])
                        t = dbg_tensor('V', [128, NT * 4 * 130], BF16)
                        k.dma('sp', t, Vaug[:].rearrange("p a b c -> p (a b c)"), b_V, [], b_V[0])
                        k.barrier()
                        t = dbg_tensor('GD', [16, T + CT])
                        k.dma('sp', t, GD_d, [bGD], [], b_gst[0])
                        t = dbg_tensor('U', [T, 512], BF16)
                        k.dma('sp', t, U_d, [bU], [], b_gst[0])
                        t = dbg_tensor('SZ', [4 * 128, T], BF16)
                        k.dma('sp', t, SZ_d.rearrange("a p t -> (a p) t"), [bSZ], [], b_gst[0])
                    k.barrier()
            if dbg == 'p2':
                k.barrier()
                return
            NCH = NT + 2
            with contextlib.ExitStack() as p4:
                def aTv(h, a, b):
                    if b <= T:
                        return xmT[:, h, 1 + a:1 + b]
                    return xcT[:, h, 1 + a - T:1 + b - T]
                b_aT = [[b_xmT[i], b_xcT[i]] for i in range(4)]
                EC = sb(p4, "EC", [128, 2, NCH, 3, 4]); b_EC = k.bufs('EC', 2)
                WO = sb(p4, "WO", [128, 2, 4, NCH]); b_WO = k.buf('WO')
                mask = sb(p4, "mask", [128, 256]); b_mask = k.buf('mask')
                wqb = sb(p4, "wqb", [128, 4, 128], BF16); b_wqb = k.buf('wqb')
                wkb = sb(p4, "wkb", [128, 4, 128], BF16); b_wkb = k.buf('wkb')
                k.dma('sp', mask[:], msk_d, [bIN], [b_mask], b_mask)
                k.dma('pool', wqb[:], wq_d.rearrange("h d e -> d h e"), [bIN], [b_wqb], b_wqb)
                k.dma('pool', wkb[:], wk_d.rearrange("h d e -> d h e"), [bIN], [b_wkb], b_wkb)
                with contextlib.ExitStack() as pa:
                    ctmp = [sb(pa, "ctmp%d" % i, [128, 1024]) for i in range(2)]; b_ctmp = k.bufs('ctmp', 2)
                    nonlocal_n = [0]
                    n = 0
                    for ch in range(4):
                        w = lambda t, ch=ch: vec[:, V_MCW + ch * 3 + t:V_MCW + ch * 3 + t + 1]

                        def ctmp_of(src, bsrc, t0, ln, ch=ch, w=w):
                            nonlocal_n[0] += 1
                            tmp = ctmp[nonlocal_n[0] % 2]; btmp = b_ctmp[nonlocal_n[0] % 2]
                            k.op('dve', lambda: dve.tensor_scalar(
                                out=tmp[:, 0:ln], in0=src[:, ch, t0:t0 + ln], scalar1=w(0), scalar2=vec[:, V_MCB + ch:V_MCB + ch + 1],
                                op0=ALU.mult, op1=ALU.add), [bsrc[ch], b_vec], [btmp])
                            for t in (1, 2):
                                k.op('dve', lambda t=t: dve.scalar_tensor_tensor(
                                    out=tmp[:, 0:ln], in0=src[:, ch, t0 + t:t0 + t + ln], scalar=w(t), in1=tmp[:, 0:ln],
                                    op0=ALU.mult, op1=ALU.add), [bsrc[ch], b_vec, btmp], [btmp])
                            return tmp, btmp

                        def cwrite(src, bsrc, t0, ln, tmp, btmp, ch=ch):
                            k.op('act', lambda: act.activation(out=src[:, ch, 1 + t0:1 + t0 + ln], in_=tmp[:, 0:ln], func=AF.Silu),
                                 [btmp], [bsrc[ch]])
                        cur = ctmp_of(xmT, b_xmT, 0, 1024)
                        for sidx in range(4):
                            nxt = ctmp_of(xmT, b_xmT, (sidx + 1) * 1024, 1024) if sidx < 3 else None
                            cwrite(xmT, b_xmT, sidx * 1024, 1024, *cur)
                            cur = nxt
                        cc_ = ctmp_of(xcT, b_xcT, 0, CT)
                        cwrite(xcT, b_xcT, 0, CT, *cc_)
                    k.barrier()
                TT = T + CT
                orders = [[32, 33] + list(range(32)), [33, 32] + list(range(31, -1, -1))]
                with contextlib.ExitStack() as pb:
                    sel = sb(pb, "sel", [4, 512]); b_sel = k.buf('sel')
                    k.dma('sp', sel[:], sel_d, [bIN], [b_sel], b_sel)
                    t_li_ = [sb(pb, "t_li%d" % i, [4, TT]) for i in range(2)]; b_li_ = k.bufs('t_li', 2)
                    t_g_ = [sb(pb, "t_g%d" % i, [4, TT]) for i in range(2)]; b_g_ = k.bufs('t_g', 2)
                    t_p_ = [sb(pb, "t_p%d" % i, [4, TT]) for i in range(2)]; b_p_ = k.bufs('t_p', 2)
                    ones = sb(pb, "ones", [4, T], BF16); b_ones = k.buf('ones')
                    sm_ = [sb(pb, "sm%d" % i, [4, 8 + 4 * NCH]) for i in range(2)]; b_sm_ = k.bufs('sm', 2)
                    pEC = [ps(pb, "pEC%d" % i, [128, NCH, 3, 4]) for i in range(2)]; b_pEC = k.bufs('pEC', 2)
                    pWO = ps(pb, "pWO", [128, 2, 4, NCH]); b_pWO = k.buf('pWO')
                    k.op('dve', lambda: dve.memset(ones[:], 1.0), [], [b_ones])
                    segs = [(0, T), (T, TT)]
                    v3 = lambda tl: tl[:].rearrange("p (c j) -> p c j", j=128)
                    bc3 = lambda ap: ap.unsqueeze(2).to_broadcast([4, NCH, 128])
                    D2 = range(2)
                    for d in D2:
                        k.dma('sp', t_li_[d][:], GD_d[d * 8:d * 8 + 4, :], [bGD], [b_li_[d]], b_li_[d])
                        k.dma('sp', t_g_[d][:], GD_d[d * 8 + 4:d * 8 + 8, :], [bGD], [b_g_[d]], b_g_[d])
                    for d in D2:
                        t_g = t_g_[d]; b_g = b_g_[d]
                        k.op('act', lambda t_g=t_g: act.activation(out=t_g[:], in_=t_g[:], func=AF.Exp, scale=-1.0), [b_g], [b_g])
                        k.op('act', lambda t_g=t_g: act.activation(out=t_g[:], in_=t_g[:], func=AF.Ln, bias=1.0), [b_g], [b_g])
                    for si, (a0, a1) in enumerate(segs):
                        for d in D2:
                            t_g = t_g_[d]; b_g = b_g_[d]; t_p = t_p_[d]; b_p = b_p_[d]; sm = sm_[d]; b_sm = b_sm_[d]
                            k.op('dve', lambda a0=a0, a1=a1, t_p=t_p, t_g=t_g: dve.tensor_tensor_scan(
                                out=t_p[:, a0:a1], data0=ones[:, 0:a1 - a0], data1=t_g[:, a0:a1], initial=0.0,
                                op0=ALU.mult, op1=ALU.add), [b_ones, b_g], [b_p])
                            k.op('dve', lambda a1=a1, si=si, sm=sm, t_p=t_p: dve.tensor_copy(out=sm[:, si:si + 1], in_=t_p[:, a1 - 1:a1]),
                                 [b_p], [b_sm])
                            if d == 1:
                                k.op('dve', lambda a0=a0, a1=a1, si=si, sm=sm, t_p=t_p, t_g=t_g: dve.scalar_tensor_tensor(
                                    out=t_p[:, a0:a1], in0=t_g[:, a0:a1], scalar=sm[:, si:si + 1], in1=t_p[:, a0:a1],
                                    op0=ALU.add, op1=ALU.subtract), [b_g, b_sm, b_p], [b_p])
                    for d in D2:
                        t_li = t_li_[d]; b_li = b_li_[d]; t_p = t_p_[d]; b_p = b_p_[d]; sm = sm_[d]; b_sm = b_sm_[d]
                        cm = sm[:, 8:8 + NCH]
                        k.op('dve', lambda t_li=t_li, t_p=t_p: dve.tensor_tensor(out=t_li[:], in0=t_li[:], in1=t_p[:], op=ALU.add),
                             [b_li, b_p], [b_li])
                        k.op('dve', lambda cm=cm, t_li=t_li: dve.tensor_reduce(out=cm, in_=v3(t_li), axis=AX.X, op=ALU.max), [b_li], [b_sm])
                    prevs = [None, None]
                    for s_ in range(NCH):
                        for d in D2:
                            sm = sm_[d]; b_sm = b_sm_[d]
                            cm = sm[:, 8:8 + NCH]; MS = sm[:, 8 + NCH:8 + 2 * NCH]; ME = sm[:, 8 + 2 * NCH:8 + 3 * NCH]
                            c = orders[d][s_]; prev = prevs[d]
                            if s_ == 0:
                                k.op('dve', lambda c=c, MS=MS: dve.memset(MS[:, c:c + 1], 0.0), [], [b_sm])
                            elif s_ == 2:
                                k.op('dve', lambda c=c, prev=prev, MS=MS, ME=ME, sm=sm: dve.tensor_tensor(
                                    out=MS[:, c:c + 1], in0=ME[:, prev:prev + 1], in1=sm[:, 1:2], op=ALU.subtract), [b_sm], [b_sm])
                            else:
                                k.op('dve', lambda c=c, prev=prev, MS=MS, ME=ME: dve.tensor_copy(out=MS[:, c:c + 1], in_=ME[:, prev:prev + 1]),
                                     [b_sm], [b_sm])
                            k.op('dve', lambda c=c, MS=MS, ME=ME, cm=cm: dve.tensor_tensor(
                                out=ME[:, c:c + 1], in0=MS[:, c:c + 1], in1=cm[:, c:c + 1], op=ALU.max), [b_sm], [b_sm])
                            prevs[d] = c
                    for d in D2:
                        t_li = t_li_[d]; b_li = b_li_[d]; t_g = t_g_[d]; b_g = b_g_[d]; t_p = t_p_[d]; b_p = b_p_[d]
                        sm = sm_[d]; b_sm = b_sm_[d]
                        MS = sm[:, 8 + NCH:8 + 2 * NCH]; ME = sm[:, 8 + 2 * NCH:8 + 3 * NCH]
                        k.op('dve', lambda t_p=t_p, MS=MS: dve.tensor_tensor(out=v3(t_p), in0=v3(t_p), in1=bc3(MS), op=ALU.subtract),
                             [b_p, b_sm], [b_p])
                        k.op('act', lambda t_p=t_p: act.activation(out=t_p[:], in_=t_p[:], func=AF.Exp), [b_p], [b_p])
                        k.op('dve', lambda t_g=t_g, t_li=t_li, MS=MS: dve.tensor_tensor(out=v3(t_g), in0=v3(t_li), in1=bc3(MS), op=ALU.subtract),
                             [b_li, b_sm], [b_g])
                        k.op('act', lambda t_g=t_g: act.activation(out=t_g[:], in_=t_g[:], func=AF.Exp), [b_g], [b_g])
                    for d in D2:
                        t_li = t_li_[d]; b_li = b_li_[d]; sm = sm_[d]; b_sm = b_sm_[d]
                        MS = sm[:, 8 + NCH:8 + 2 * NCH]; ME = sm[:, 8 + 2 * NCH:8 + 3 * NCH]; WOL = sm[:, 8 + 3 * NCH:8 + 4 * NCH]
                        k.op('dve', lambda t_li=t_li, ME=ME: dve.tensor_tensor(out=v3(t_li), in0=v3(t_li), in1=bc3(ME), op=ALU.subtract),
                             [b_li, b_sm], [b_li])
                        k.op('act', lambda t_li=t_li: act.activation(out=t_li[:], in_=t_li[:], func=AF.Exp), [b_li], [b_li])
                        k.op('dve', lambda WOL=WOL, MS=MS, ME=ME: dve.tensor_tensor(out=WOL, in0=MS, in1=ME, op=ALU.subtract), [b_sm], [b_sm])
                        k.op('act', lambda WOL=WOL: act.activation(out=WOL, in_=WOL, func=AF.Exp), [b_sm], [b_sm])
                    for d in D2:
                        t_li = t_li_[d]; b_li = b_li_[d]; t_g = t_g_[d]; b_g = b_g_[d]; t_p = t_p_[d]; b_p = b_p_[d]
                        sm = sm_[d]; b_sm = b_sm_[d]; WOL = sm[:, 8 + 3 * NCH:8 + 4 * NCH]

                        def trE(d=d, t_g=t_g, t_li=t_li, t_p=t_p):
                            last = None
                            for c in range(NCH):
                                for q_, tl in enumerate((t_g, t_li, t_p)):
                                    last = pe.transpose(pEC[d][:, c, q_, :], tl[:, c * 128:(c + 1) * 128], idf[0:4, 0:4])
                            return last
                        k.op('pe', trE, [b_g, b_li, b_p, b_idf], [b_pEC[d]])
                        k.op('dve', lambda d=d: dve.tensor_copy(out=EC[:, d], in_=pEC[d][:]), [b_pEC[d]], [b_EC[d]])

                        def mmW(d=d, WOL=WOL):
                            last = None
                            for h in range(4):
                                last = pe.matmul(pWO[:, d, h, :], lhsT=sel[:, h * 128:(h + 1) * 128], rhs=WOL, start=True, stop=True)
                            return last
                        k.op('pe', mmW, [b_sel, b_sm], [b_pWO])
                        k.op('dve', lambda d=d: dve.tensor_copy(out=WO[:, d], in_=pWO[:, d]), [b_pWO], [b_WO])
                    k.barrier()
                if dbg == 'p4b':
                    t = dbg_tensor('EC', [128, 2 * NCH * 12])
                    k.dma('sp', t, EC[:].rearrange("p a b c d -> p (a b c d)"), b_EC, [], b_EC[0])
                    t = dbg_tensor('WO', [128, 8 * NCH])
                    k.dma('sp', t, WO[:].rearrange("p a b c -> p (a b c)"), [b_WO], [], b_WO)
                    k.barrier()
                    return
                with contextlib.ExitStack() as pc:
                    qT = sb(pc, "qT", [128, T], BF16); b_qT = k.bufs('qT', 8)
                    kT = sb(pc, "kT", [128, T], BF16); b_kT = k.bufs('kT', 8)
                    ktm = sb(pc, "ktm", [128, NCH, 128], BF16); b_ktm = k.bufs('ktm', NCH)
                    Hacc = sb(pc, "Hacc", [128, NT, 128]); b_H = k.bufs('Hacc', NT)
                    Hraw = sb(pc, "Hraw", [128, 2, NT, 130]); b_Hraw = [k.bufs('Hraw', NT) for i in range(2)]
                    rcb = sb(pc, "rcb", [128, 2, 3, NT]); b_rcb = k.bufs('rcb', 2)
                    Cf = [sb(pc, "Cf%d" % i, [128, 129]) for i in range(2)]; b_Cf = k.bufs('Cf', 2)
                    Cball = sb(pc, "Cball", [128, 2, NCH, 130], BF16); b_Cball = [k.bufs('Cball', NCH) for i in range(2)]
                    sTall = sb(pc, "sTall", [128, 2, NT, 128], BF16); b_sTall = [k.bufs('sTall', NT) for i in range(2)]
                    kw = [[sb(pc, "kw%d_%d" % (i, j), [128, 128], BF16) for j in range(3)] for i in range(2)]
                    b_kw = [k.bufs('kw', 3) for i in range(2)]
                    pP = [ps(pc, "pP%d" % i, [128, 512]) for i in range(2)]; b_pP = k.bufs('pP', 2)
                    pS = [ps(pc, "pS%d" % i, [128, 128]) for i in range(2)]; b_pS = k.bufs('pS', 2)
                    pH = [ps(pc, "pH%d" % i, [128, 129]) for i in range(2)]; b_pH = k.bufs('pH', 2)
                    pC = [ps(pc, "pC%d" % i, [128, 129]) for i in range(2)]; b_pC = k.bufs('pC', 2)
                    pT = [pP[i][:].bitcast(BF16)[:, 0:512].rearrange("p (a b) -> p a b", b=128) for i in range(2)]; b_pT = b_pP
                    t1 = [sb(pc, "t1_%d" % i, [128, 512]) for i in range(2)]; b_t1 = k.bufs('t1', 2)
                    szl = [sb(pc, "szl%d" % i, [128, 512], BF16) for i in range(2)]; b_szl = k.bufs('szl', 2)
                    ysb = [sb(pc, "ysb%d" % i, [128, 512], BF16) for i in range(2)]; b_ysb = k.bufs('ysb', 2)
                    ppi = [0]
                    KS = 128 ** -0.5
                    for h in ([1] if dbg == 'p4only1' else range(4)):
                        for tt in range(8):
                            for which in range(2):
                                ppi[0] += 1
                                P = pP[ppi[0] % 2]; bP = b_pP[ppi[0] % 2]
                                wb = wqb if which == 0 else wkb
                                bwb = b_wqb if which == 0 else b_wkb
                                k.op('pe', lambda P=P, wb=wb, tt=tt, h=h: pe.matmul(
                                    P[:], lhsT=wb[:, h, :], rhs=aTv(h, tt * 512, (tt + 1) * 512), start=True, stop=True),
                                    [bwb] + b_aT[h], [bP])
                                if which == 0:
                                    k.op('act', lambda P=P, tt=tt: act.copy(out=qT[:, tt * 512:(tt + 1) * 512], in_=P[:]), [bP], [b_qT[tt]])
                                else:
                                    k.op('act', lambda P=P, tt=tt: act.mul(out=kT[:, tt * 512:(tt + 1) * 512], in_=P[:], mul=KS), [bP], [b_kT[tt]])
                        for c0 in range(0, NCH, 4):
                            ppi[0] += 1
                            P = pP[ppi[0] % 2]; bP = b_pP[ppi[0] % 2]
                            nn = min(4, NCH - c0)

                            def mmk(P=P, c0=c0, nn=nn, h=h):
                                last = None
                                for i in range(nn):
                                    c = c0 + i
                                    last = pe.matmul(P[:, i * 128:(i + 1) * 128], lhsT=aTv(h, c * 128, (c + 1) * 128), rhs=wkb[:, h, :],
                                                     start=True, stop=True)
                                return last
                            k.op('pe', mmk, [b_wkb] + b_aT[h], [bP])
                            k.op('dve', lambda P=P, c0=c0, nn=nn: dve.tensor_scalar(
                                out=ktm[:, c0:c0 + nn, :], in0=P[:, 0:nn * 128].rearrange("p (a b) -> p a b", b=128),
                                scalar1=KS, scalar2=None, op0=ALU.mult), [bP], b_ktm[c0:c0 + nn])
                        nS = 0
                        nC = 0
                        for s_ in range(NCH):
                            for d in range(2):
                                c = orders[d][s_]
                                isctx = c >= NT
                                Vt = Vc[:, c - NT, h, 0:129] if isctx else Vaug[:, c, h, 0:129]
                                bV = b_Vc[c - NT] if isctx else b_V[c]
                                if not isctx:
                                    tq = c // 4
                                    S_ = pS[nS % 2]; bS_ = b_pS[nS % 2]; nS += 1
                                    k.op('pe', lambda S_=S_, c=c: pe.matmul(
                                        S_[:], lhsT=kT[:, c * 128:(c + 1) * 128], rhs=qT[:, c * 128:(c + 1) * 128], start=True, stop=True),
                                        [b_kT[tq], b_qT[tq]], [bS_])
                                    k.op('dve', lambda S_=S_, d=d, c=c, h=h: dve.scalar_tensor_tensor(
                                        out=sTall[:, d, c, :], in0=S_[:], scalar=EC[:, d, c, 0, h:h + 1], in1=mask[:, d * 128:(d + 1) * 128],
                                        op0=ALU.mult, op1=ALU.mult), [bS_, b_EC[d], b_mask], [b_sTall[d][c]])
                                kwt = kw[d][s_ % 3]; bkw = b_kw[d][s_ % 3]
                                k.op('act', lambda kwt=kwt, c=c, d=d, h=h: act.activation(
                                    out=kwt[:], in_=ktm[:, c, :], func=AF.Copy, scale=EC[:, d, c, 1, h:h + 1]),
                                    [b_ktm[c], b_EC[d]], [bkw])
                                C_ = pC[nC % 2]; bC_ = b_pC[nC % 2]; nC += 1
                                k.op('pe', lambda C_=C_, kwt=kwt, Vt=Vt: pe.matmul(C_[:], lhsT=kwt[:], rhs=Vt, start=True, stop=True),
                                     [bkw, bV], [bC_])
                                if s_ == 0:
                                    k.op('dve', lambda C_=C_, d=d: dve.tensor_copy(out=Cf[d][:], in_=C_[:]), [bC_], [b_Cf[d]])
                                else:
                                    k.op('dve', lambda C_=C_, d=d, h=h, c=c: dve.scalar_tensor_tensor(
                                        out=Cf[d][:], in0=Cf[d][:], scalar=WO[:, d, h, c:c + 1], in1=C_[:],
                                        op0=ALU.mult, op1=ALU.add), [bC_, b_Cf[d], b_WO], [b_Cf[d]])
                                if s_ < NCH - 1:
                                    k.op('pool', lambda d=d, s_=s_: pool.tensor_copy(out=Cball[:, d, s_, 0:129], in_=Cf[d][:]),
                                         [b_Cf[d]], [b_Cball[d][s_]])
                        for s_ in range(2, NCH):
                            for d in range(2):
                                c = orders[d][s_]
                                tq = c // 4
                                H_ = pH[d]; bH_ = b_pH[d]

                                def mmh(H_=H_, c=c, d=d, s_=s_, h=h):
                                    pe.matmul(H_[:], lhsT=qT[:, c * 128:(c + 1) * 128], rhs=Cball[:, d, s_ - 1, 0:129], start=True, stop=False)
                                    return pe.matmul(H_[:], lhsT=sTall[:, d, c, :], rhs=Vaug[:, c, h, 0:129], start=False, stop=True)
                                k.op('pe', mmh, [b_qT[tq], b_Cball[d][s_ - 1], b_sTall[d][c], b_V[c]], [bH_])
                                if d == 0:
                                    k.op('act', lambda H_=H_, c=c: act.copy(out=Hraw[:, 0, c, 0:129], in_=H_[:]), [bH_], [b_Hraw[0][c]])
                                else:
                                    k.op('dve', lambda H_=H_, c=c: dve.tensor_copy(out=Hraw[:, 1, c, 0:129], in_=H_[:]), [bH_], [b_Hraw[1][c]])
                        for d in range(2):
                            den = Hraw[:, d, :, 128]
                            e3 = EC[:, d, 0:NT, 2, h]
                            neg = rcb[:, d, 0, :]; cl = rcb[:, d, 1, :]; rc = rcb[:, d, 2, :]
                            k.op('dve', lambda den=den, neg=neg: dve.tensor_scalar(out=neg, in0=den, scalar1=-1.0, scalar2=None, op0=ALU.mult),
                                 b_Hraw[d], [b_rcb[d]])
                            k.op('dve', lambda den=den, neg=neg, cl=cl: dve.tensor_tensor(out=cl, in0=den, in1=neg, op=ALU.max),
                                 b_Hraw[d] + [b_rcb[d]], [b_rcb[d]])
                            k.op('dve', lambda e3=e3, cl=cl: dve.tensor_tensor(out=cl, in0=cl, in1=e3, op=ALU.max), [b_EC[d], b_rcb[d]], [b_rcb[d]])
                            k.op('dve', lambda cl=cl, rc=rc: dve.reciprocal(out=rc, in_=cl), [b_rcb[d]], [b_rcb[d]])
                        bcr = lambda ap: ap.unsqueeze(2).to_broadcast([128, NT, 128])
                        k.op('dve', lambda: dve.tensor_tensor(out=Hacc[:], in0=Hraw[:, 0, :, 0:128], in1=bcr(rcb[:, 0, 2, :]), op=ALU.mult),
                             b_Hraw[0] + [b_rcb[0]], b_H)
                        k.op('pool', lambda: pool.tensor_tensor(out=Hraw[:, 1, :, 0:128], in0=Hraw[:, 1, :, 0:128], in1=bcr(rcb[:, 1, 2, :]),
                                                                op=ALU.mult), b_Hraw[1] + [b_rcb[1]], b_Hraw[1])
                        k.op('dve', lambda: dve.tensor_tensor(out=Hacc[:], in0=Hacc[:], in1=Hraw[:, 1, :, 0:128], op=ALU.add),
                             b_H + b_Hraw[1], b_H)
                        if dbg == 'p4s1' and h == 1:
                            k.barrier()
                            return
                        if dbg == 'p4c' and h == 0:
                            t = dbg_tensor('Hacc', [128, NT * 128])
                            k.dma('sp', t, Hacc[:].rearrange("p a b -> p (a b)"), b_H, [], b_H[0])
                            t = dbg_tensor('Cf', [128, 129])
                            k.dma('sp', t, Cf[0][:], [b_Cf[0]], [], b_Cf[0])
                            k.barrier()
                            return
                        HC = Hraw[:, 0, :, 0:128]
                        SQ = Hraw[:, 1, :, 0:128]
                        HB = sTall[:, 0]
                        mean = rcb[:, 0, 0, :]; rstd = rcb[:, 0, 1, :]
                        k.op('dve', lambda: dve.tensor_reduce(out=mean, in_=Hacc[:], axis=AX.X, op=ALU.add), b_H, [b_rcb[0]])
                        k.op('dve', lambda: dve.tensor_scalar(out=mean, in0=mean, scalar1=1.0 / 128, scalar2=None, op0=ALU.mult),
                             [b_rcb[0]], [b_rcb[0]])
                        k.op('pool', lambda: pool.tensor_tensor(out=HC, in0=Hacc[:], in1=bcr(mean), op=ALU.subtract),
                             b_H + [b_rcb[0]], b_Hraw[0])
                        k.op('act', lambda: act.activation(out=SQ, in_=HC, func=AF.Square), b_Hraw[0], b_Hraw[1])
                        k.op('dve', lambda: dve.tensor_reduce(out=rstd, in_=SQ, axis=AX.X, op=ALU.add), b_Hraw[1], [b_rcb[0]])
                        k.op('dve', lambda: dve.tensor_scalar(out=rstd, in0=rstd, scalar1=1.0 / 128, scalar2=EPS, op0=ALU.mult, op1=ALU.add),
                             [b_rcb[0]], [b_rcb[0]])
                        k.op('act', lambda: act.activation(out=rstd, in_=rstd, func=AF.Sqrt), [b_rcb[0]], [b_rcb[0]])
                        k.op('dve', lambda: dve.reciprocal(out=rstd, in_=rstd), [b_rcb[0]], [b_rcb[0]])
                        k.op('dve', lambda: dve.tensor_tensor(out=HB, in0=HC, in1=bcr(rstd), op=ALU.mult),
                             b_Hraw[0] + [b_rcb[0]], b_sTall[0])

                        def ho_tr(tb):
                            pt = pT[tb % 2]; bpt = b_pT[tb % 2]

                            def trh(pt=pt, tb=tb):
                                last = None
                                for i in range(4):
                                    last = pe.transpose(pt[:, i, :], HB[:, tb * 4 + i, :], idb[:])
                                return last
                            k.op('pe', trh, b_sTall[0][tb * 4:(tb + 1) * 4] + [b_idb], [bpt])
                            zl = szl[tb % 2]; bzl = b_szl[tb % 2]
                            k.dma('sp', zl[:], SZ_d[h, :, tb * 512:(tb + 1) * 512], [bSZ], [bzl], bzl)

                        def ho_out(tb):
                            pt = pT[tb % 2]; bpt = b_pT[tb % 2]
                            tl = t1[tb % 2]; btl = b_t1[tb % 2]
                            zl = szl[tb % 2]; bzl = b_szl[tb % 2]
                            yb = ysb[tb % 2]; byb = b_ysb[tb % 2]
                            k.op('act', lambda tl=tl, tb=tb, h=h: act.activation(
                                out=tl[:], in_=aTv(h, tb * 512, (tb + 1) * 512), func=AF.Copy, scale=vec[:, V_MSK + h:V_MSK + h + 1]),
                                b_aT[h] + [b_vec], [btl])
                            k.op('dve', lambda tl=tl, pt=pt, h=h: dve.scalar_tensor_tensor(
                                out=tl[:], in0=pt.rearrange("p a b -> p (a b)"), scalar=vec[:, V_MNW + h:V_MNW + h + 1], in1=tl[:],
                                op0=ALU.mult, op1=ALU.add), [bpt, btl, b_vec], [btl])
                            k.op('pool', lambda tl=tl, zl=zl, yb=yb: pool.tensor_tensor(out=yb[:], in0=tl[:], in1=zl[:], op=ALU.mult),
                                 [btl, bzl], [byb])
                            k.dma('pool', YT_d[4 + h, :, tb * 512:(tb + 1) * 512], yb[:], [byb], [bYT], byb)
                        ho_tr(0)
                        for tb in range(8):
                            if tb + 1 < 8:
                                ho_tr(tb + 1)
                            ho_out(tb)
                        if dbg in ('p4h0', 'p4only1') or (dbg == 'p4h1' and h == 1):
                            k.barrier()
                            return
                    k.barrier()
            k.barrier()
        if dbg == 'p4':
            with contextlib.ExitStack() as pd:
                tmpd = sb(pd, "tmpd", [128, 4, T], BF16); b_tmpd = k.buf('tmpd')
                k.dma('sp', tmpd[:], YT_d[4:8].rearrange("a p t -> p a t"), [bYT], [b_tmpd], b_tmpd)
                t = dbg_tensor('ym', [128, 4 * T], BF16)
                k.dma('sp', t, tmpd[:].rearrange("p a t -> p (a t)"), [b_tmpd], [], b_tmpd)
                k.barrier()
            return
        with contextlib.ExitStack() as f1:
            T1 = sb(f1, "T1", [128, 32, 2, 128], BF16); b_T1 = k.buf('T1')
            Ul = sb(f1, "Ul", [128, 32, 512], BF16); b_Ul = k.bufs('Ul', 4)
            zs = [sb(f1, "zs%d" % i, [128, 4, 2, 512], BF16) for i in range(2)]
            b_zs = [[[k.buf('zs') for _ in range(2)] for _ in range(4)] for _ in range(2)]
            pZ = [ps(f1, "pZ%d" % i, [128, 512]) for i in range(4)]; b_pZ = k.bufs('pZ', 4)
            k.dma('sp', T1[:].rearrange("p a b c -> p (a b c)"), t1_d, [bIN], [b_T1], b_T1)
            Uv = U_d.rearrange("(a b) c -> a b c", b=32)
            for q in range(4):
                k.dma('sp', Ul[:, q * 8:(q + 1) * 8, :], Uv[:, q * 8:(q + 1) * 8, :], [bU], [b_Ul[q]], b_Ul[q])
            n = 0
            for l2 in range(32):
                grp, i = l2 // 4, l2 % 4
                Z = zs[grp % 2]
                for ri in range(2):
                    P = pZ[n % 4]; bP = b_pZ[n % 4]
                    k.op('pe', lambda P=P, l2=l2, ri=ri: pe.matmul(P[:], lhsT=T1[:, l2, ri, :], rhs=Ul[:, l2, :], start=True, stop=True),
                         [b_T1, b_Ul[l2 // 8]], [bP])
                    bz = b_zs[grp % 2][i][ri]
                    if n % 2 == 0:
                        k.op('act', lambda P=P, Z=Z, i=i, ri=ri: act.copy(out=Z[:, i, ri, :], in_=P[:]), [bP], [bz])
                    else:
                        k.op('dve', lambda P=P, Z=Z, i=i, ri=ri: dve.tensor_copy(out=Z[:, i, ri, :], in_=P[:]), [bP], [bz])
                    n += 1
                if i == 3:
                    for ri in range(2):
                        rb = [b_zs[grp % 2][ii][ri] for ii in range(4)]
                        k.dma('pool', ZD_d[ri, l2 - 3:l2 + 1].rearrange("l k c -> k l c"), Z[:, :, ri, :], rb, [bZD], rb[0])
            k.barrier()
        if dbg == 'p3a':
            k.barrier()
            return
        with contextlib.ExitStack() as f2:
            W2 = sb(f2, "W2", [128, 64], BF16); b_W2 = k.buf('W2')
            cd = sb(f2, "cd", [128, 256], BF16); b_cd = k.buf('cd')
            Yt = sb(f2, "Yt", [128, 4, 2, T], BF16)
            b_Yt = [[k.buf('Yt') for _ in range(32)] for _ in range(4)]
            Zt = [sb(f2, "Zt%d" % i, [128, 16, 512], BF16) for i in range(2)]; b_Zt = k.bufs('Zt', 2)
            for i in range(2):
                k.op('dve', lambda i=i: dve.memset(Zt[i][:], 0.0), [], [b_Zt[i]])
            pY = [ps(f2, "pY%d" % i, [128, 8, 64]) for i in range(4)]; b_pY = k.bufs('pY', 4)
            pF = [ps(f2, "pF%d" % i, [128, 512]) for i in range(2)]; b_pF = k.bufs('pF', 2)
            yst = [sb(f2, "yst%d" % i, [128, 512], BF16) for i in range(2)]; b_yst = k.bufs('yst', 2)
            k.dma('sp', W2[:], w2_d, [bIN], [b_W2], b_W2)
            k.dma('sp', cd[:], cd_d, [bIN], [b_cd], b_cd)
            ZDv = ZD_d.rearrange("r l k c -> (r l) k c")
            n = 0
            for slab in range(8):
                Zs = Zt[slab % 2]; bZs = b_Zt[slab % 2]
                k.dma('sp', Zs[0:64], ZDv[:, slab * 16:(slab + 1) * 16, :], [bZD], [bZs], bZs)
                if dbg == 'p3b0':
                    k.barrier()
                    return
                for g in range(4):
                    for half in range(2):
                        P = pY[n % 4]; bP = b_pY[n % 4]

                        def mmy(P=P, Zs=Zs, g=g, half=half):
                            last = None
                            for i in range(8):
                                last = pe.matmul(P[:, i, :], lhsT=Zs[:, half * 8 + i, g * 128:(g + 1) * 128], rhs=W2[:], start=True, stop=True)
                            return last
                        k.op('pe', mmy, [bZs, b_W2], [bP])
                        k1_0 = slab * 16 + half * 8
                        for ri in ([] if dbg == 'p3b2' else range(2)):
                            bo = b_Yt[g][(slab * 2 + half) * 2 + ri]
                            Yv = Yt[:, g, ri, :].rearrange("p (k2 k1) -> p k2 k1", k1=128)

                            def evy(P=P, Yv=Yv, ri=ri, k1_0=k1_0, n=n):
                                last = None
                                for i in range(8):
                                    if n % 2 == 0:
                                        last = act.copy(out=Yv[:, :, k1_0 + i], in_=P[:, i, ri * 32:(ri + 1) * 32])
                                    else:
                                        last = dve.tensor_copy(out=Yv[:, :, k1_0 + i], in_=P[:, i, ri * 32:(ri + 1) * 32])
                                return last
                            k.op('act' if n % 2 == 0 else 'dve', evy, [bP], [bo])
                        n += 1
                if dbg in ('p3b1', 'p3b2'):
                    k.barrier()
                    return
            if dbg == 'p3b':
                k.barrier()
                return
            n = 0
            for g in range(4):
                for tt in range(8):
                    P = pF[n % 2]; bP = b_pF[n % 2]

                    def mmc2(P=P, g=g, tt=tt):
                        pe.matmul(P[:], lhsT=cd[:, 0:128], rhs=Yt[:, g, 0, tt * 512:(tt + 1) * 512], start=True, stop=False)
                        return pe.matmul(P[:], lhsT=cd[:, 128:256], rhs=Yt[:, g, 1, tt * 512:(tt + 1) * 512], start=False, stop=True)
                    k.op('pe', mmc2, [b_cd] + b_Yt[g], [bP])
                    ys = yst[n % 2]; bys = b_yst[n % 2]
                    if n % 2 == 0:
                        k.op('act', lambda P=P, ys=ys: act.copy(out=ys[:], in_=P[:]), [bP], [bys])
                    else:
                        k.op('dve', lambda P=P, ys=ys: dve.tensor_copy(out=ys[:], in_=P[:]), [bP], [bys])
                    k.dma('pool', YT_d[g, :, tt * 512:(tt + 1) * 512], ys[:], [bys], [bYT], bys)
                    n += 1
            k.barrier()
        if dbg == 'p3':
            with contextlib.ExitStack() as pd:
                tmpd = sb(pd, "tmpd", [128, 8, T], BF16); b_tmpd = k.buf('tmpd')
                k.dma('sp', tmpd[:], YT_d.rearrange("a p t -> p a t"), [bYT], [b_tmpd], b_tmpd)
                t = dbg_tensor('yt', [128, 8 * T], BF16)
                k.dma('sp', t, tmpd[:].rearrange("p a t -> p (a t)"), [b_tmpd], [], b_tmpd)
                k.barrier()
            return
        with contextlib.ExitStack() as gf:
            hx2T = sb(gf, "hx2T", [128, 8, T], BF16); b_hx2T = k.bufs('hx2T', NT)
            with contextlib.ExitStack() as p5:
                wo = sb(p5, "wo", [128, 8, D], BF16); b_wo = k.bufs('wo', 8)
                wof = [sb(p5, "wof%d" % i, [128, D]) for i in range(2)]; b_wof = k.bufs('wof', 2)
                yl = [sb(p5, "yl%d" % i, [128, 8, 512], BF16) for i in range(2)]; b_yl = k.bufs('yl', 2)
                xt = [sb(p5, "x5t%d" % i, [128, D]) for i in range(2)]; b_xt = k.bufs('x5t', 2)
                x1t = [sb(p5, "x1t%d" % i, [128, D]) for i in range(2)]; b_x1t = k.bufs('x1t', 2)
                xn = [sb(p5, "x5n%d" % i, [128, D], BF16) for i in range(2)]; b_xn = k.bufs('x5n', 2)
                sq = sb(p5, "sq5", [128, D], BF16); b_sq = k.buf('sq5')
                st = [sb(p5, "st5_%d" % i, [128, 2]) for i in range(2)]; b_st = k.bufs('st5', 2)
                pO = [ps(p5, "pO%d" % i, [128, 512]) for i in range(4)]; b_pO = k.bufs('pO', 4)
                ptrA = [ps(p5, "ptr5A%d" % i, [128, 4, 128], BF16) for i in range(2)]; b_ptrA = k.bufs('ptr5A', 2)
                ptrB = [ps(p5, "ptr5B%d" % i, [128, 5, 128], BF16) for i in range(2)]; b_ptrB = k.bufs('ptr5B', 2)
                JA = {2: 0, 4: 1, 6: 2}
                JB = {0: 0, 1: 1, 3: 2, 5: 3, 7: 4}
                for j in range(8):
                    k.dma('sp', wof[j % 2][:], wout_d[j * 128:(j + 1) * 128, :], [bIN], [b_wof[j % 2]], b_wof[j % 2])
                    k.op('dve', lambda j=j: dve.tensor_tensor(out=wo[:, j, :], in0=wof[j % 2][:], in1=g1b[:], op=ALU.mult),
                         [b_wof[j % 2], b_g1b], [b_wo[j]])
                YTv = YT_d.rearrange("a p t -> p a t")

                def ld_yl(tb):
                    k.dma('sp', yl[tb % 2][:], YTv[:, :, tb * 512:(tb + 1) * 512], [bYT], [b_yl[tb % 2]], b_yl[tb % 2])

                def ld_x(i):
                    k.dma('sp', xt[i % 2][:], x_d[i * 128:(i + 1) * 128, :], [bIN], [b_xt[i % 2]], b_xt[i % 2])

                def stageA(i):
                    if i % 4 == 0 and i // 4 + 1 < 8:
                        ld_yl(i // 4 + 1)
                    Y = yl[(i // 4) % 2]; bY = b_yl[(i // 4) % 2]
                    X = xt[i % 2]; bX = b_xt[i % 2]
                    X1 = x1t[i % 2]; bX1t = b_x1t[i % 2]
                    for hf in range(2):
                        P = pO[(i * 2 + hf) % 4]; bP = b_pO[(i * 2 + hf) % 4]

                        def mmo(P=P, Y=Y, i=i, hf=hf):
                            last = None
                            for j in range(8):
                                last = pe.matmul(P[:], lhsT=Y[:, j, (i % 4) * 128:(i % 4 + 1) * 128], rhs=wo[:, j, hf * 512:(hf + 1) * 512],
                                                 start=(j == 0), stop=(j == 7))
                            return last
                        k.op('pe', mmo, [bY] + b_wo, [bP])
                        k.op('dve', lambda P=P, X=X, X1=X1, hf=hf: dve.tensor_tensor(
                            out=X1[:, hf * 512:(hf + 1) * 512], in0=P[:], in1=X[:, hf * 512:(hf + 1) * 512], op=ALU.add), [bP, bX], [bX1t])
                    if i + 2 < NT:
                        ld_x(i + 2)
                    k.dma('pool', X1_d[i * 128:(i + 1) * 128, :], X1[:], [bX1t], [bX1], bX1t)

                def stageB(i):
                    X1 = x1t[i % 2]; bX1t = b_x1t[i % 2]
                    S = st[i % 2]; bS = b_st[i % 2]
                    XN = xn[i % 2]; bXN = b_xn[i % 2]
                    PA = ptrA[i % 2]; bPA = b_ptrA[i % 2]; PB = ptrB[i % 2]; bPB = b_ptrB[i % 2]
                    k.op('act', lambda X1=X1, S=S: act.activation(out=sq[:], in_=X1[:], func=AF.Square, accum_out=S[:, 0:1]),
                         [bX1t], [b_sq, bS])
                    k.op('dve', lambda S=S: dve.tensor_scalar(out=S[:, 1:2], in0=S[:, 0:1], scalar1=1.0 / D, scalar2=EPS,
                                                              op0=ALU.mult, op1=ALU.add), [bS], [bS])
                    k.op('act', lambda S=S: act.activation(out=S[:, 1:2], in_=S[:, 1:2], func=AF.Sqrt), [bS], [bS])
                    k.op('dve', lambda S=S: dve.reciprocal(out=S[:, 1:2], in_=S[:, 1:2]), [bS], [bS])
                    k.op('pool', lambda X1=X1, S=S, XN=XN: pool.tensor_scalar(out=XN[:], in0=X1[:], scalar1=S[:, 1:2], scalar2=None, op0=ALU.mult),
                         [bX1t, bS], [bXN])

                    def tr5(XN=XN, PA=PA, PB=PB):
                        last = None
                        for j in range(8):
                            dst = PA[:, JA[j], :] if j in JA else PB[:, JB[j], :]
                            last = pe.transpose(dst, XN[:, j * 128:(j + 1) * 128], idb[:])
                        return last
                    k.op('pe', tr5, [bXN, b_idb], [bPA, bPB])

                def stageB2(i):
                    PA = ptrA[i % 2]; bPA = b_ptrA[i % 2]; PB = ptrB[i % 2]; bPB = b_ptrB[i % 2]
                    for j in range(8):
                        if j in JB:
                            k.op('act', lambda j=j, PB=PB, i=i: act.activation(
                                out=hx2T[:, j, i * 128:(i + 1) * 128], in_=PB[:, JB[j], :], func=AF.Identity,
                                scale=s2[:, j:j + 1], bias=modT[:, 24 + j, 0:1]), [bPB, b_s2, b_modT], [], aw=[b_hx2T[i]])
                        else:
                            k.op('dve', lambda j=j, PA=PA, i=i: dve.tensor_scalar(
                                out=hx2T[:, j, i * 128:(i + 1) * 128], in0=PA[:, JA[j], :], scalar1=s2[:, j:j + 1],
                                scalar2=modT[:, 24 + j, 0:1], op0=ALU.mult, op1=ALU.add), [bPA, b_s2, b_modT], [], aw=[b_hx2T[i]])
                ld_yl(0); ld_x(0); ld_x(1)
                stageA(0)
                for i in range(NT + 1):
                    if i + 1 < NT:
                        stageA(i + 1)
                    if i < NT:
                        stageB(i)
                    if i >= 1:
                        stageB2(i - 1)
                k.barrier()
            with contextlib.ExitStack() as p6:
                wu = [sb(p6, "wu%d" % i, [128, 8, 2, 128], BF16) for i in range(2)]; b_wu = [k.bufs('wu', 2) for i in range(2)]
                wuf = [sb(p6, "wuf%d" % i, [128, 8, 2, 128]) for i in range(2)]; b_wuf = [k.bufs('wuf', 2) for i in range(2)]
                dg = [sb(p6, "dg%d" % i, [128, 2, 9, 128], BF16) for i in range(2)]; b_dg = k.bufs('dg', 2)
                upad = [sb(p6, "upad%d" % i, [128, 2, 66 * 66], BF16) for i in range(2)]
                b_up = [k.bufs('upad', 2) for i in range(2)]
                vs = [sb(p6, "vs%d" % i, [128, 512]) for i in range(2)]; b_vs = k.bufs('vs', 2)
                gs = [sb(p6, "gs%d" % i, [128, 512]) for i in range(2)]; b_gs = k.bufs('gs', 2)
                hst = [sb(p6, "hst%d" % i, [128, 512], BF16) for i in range(2)]; b_hst = k.bufs('hst', 2)
                pU = [ps(p6, "pU%d" % i, [128, 512]) for i in range(3)]; b_pU = k.bufs('pU', 3)
                pV = [ps(p6, "pV%d" % i, [128, 512]) for i in range(4)]; b_pV = k.bufs('pV', 4)
                for i in range(2):
                    k.op('dve', lambda i=i: dve.memset(upad[i][:], 0.0), [], b_up[i])
                wupv = wup_d.rearrange("(kk p) n -> p kk n", p=128)
                nu = 0
                nv = 0
                def ld_wu(jj):
                    for v in range(2):
                        c0 = v * 2560 + jj * 128
                        k.dma('sp', wuf[jj % 2][:, :, v, :], wupv[:, :, c0:c0 + 128], [bIN], [b_wuf[jj % 2][v]], b_wuf[jj % 2][v])
                ld_wu(0)
                for jj in range(20):
                    bi = jj % 2
                    if jj + 1 < 20:
                        ld_wu(jj + 1)
                    for v in range(2):
                        k.op('dve', lambda bi=bi, v=v: dve.tensor_copy(out=wu[bi][:, :, v, :], in_=wuf[bi][:, :, v, :]),
                             [b_wuf[bi][v]], [b_wu[bi][v]])
                    for v in range(2):
                        ch = v * 20 + jj
                        for t in range(9):
                            k.op('dve', lambda bi=bi, v=v, t=t, ch=ch: dve.tensor_scalar(
                                out=dg[bi][:, v, t, :], in0=idb[:], scalar1=vec[:, V_FCW + ch * 9 + t:V_FCW + ch * 9 + t + 1], scalar2=None,
                                op0=ALU.mult), [b_idb, b_vec], [b_dg[bi]])
                    for v in range(2):
                        ch = v * 20 + jj
                        g3 = upad[bi][:, v, :].rearrange("p (r c) -> p r c", c=66)
                        for tt in range(8):
                            P = pU[nu % 3]; bP = b_pU[nu % 3]; nu += 1

                            def mmu(P=P, bi=bi, v=v, tt=tt):
                                last = None
                                for kk in range(8):
                                    last = pe.matmul(P[:], lhsT=wu[bi][:, kk, v, :], rhs=hx2T[:, kk, tt * 512:(tt + 1) * 512],
                                                     start=(kk == 0), stop=(kk == 7))
                                return last
                            k.op('pe', mmu, [b_wu[bi][v]] + b_hx2T[tt * 4:(tt + 1) * 4], [bP])
                            k.op('act', lambda P=P, g3=g3, tt=tt, ch=ch: act.activation(
                                out=g3[:, 1 + 8 * tt:9 + 8 * tt, 1:65], in_=P[:].rearrange("p (r c) -> p r c", c=64), func=AF.Identity,
                                bias=vec[:, V_BUP + ch:V_BUP + ch + 1]), [bP, b_vec], [b_up[bi][v]])
                    for tt in range(8):
                        outs = []
                        for v in range(2):
                            ch = v * 20 + jj
                            g3 = upad[bi][:, v, :].rearrange("p (r c) -> p r c", c=66)
                            P = pV[nv % 4]; bP = b_pV[nv % 4]; nv += 1

                            def mmv(P=P, bi=bi, v=v, tt=tt, g3=g3):
                                last = None
                                for t in range(9):
                                    dr, dc_ = t // 3 - 1, t % 3 - 1
                                    last = pe.matmul(P[:].rearrange("p (r c) -> p r c", c=64), lhsT=dg[bi][:, v, t, :],
                                                     rhs=g3[:, 8 * tt + dr + 1:8 * tt + dr + 9, dc_ + 1:dc_ + 65],
                                                     start=(t == 0), stop=(t == 8))
                                return last
                            k.op('pe', mmv, [b_dg[bi], b_up[bi][v]], [bP])
                            outs.append((P, bP, ch))
                        n2 = (jj * 8 + tt) % 2
                        (Pa, bPa, cha), (Pb, bPb, chb) = outs
                        k.op('act', lambda Pa=Pa, n2=n2, cha=cha: act.activation(out=vs[n2][:], in_=Pa[:], func=AF.Identity,
                                                                              bias=vec[:, V_FCB + cha:V_FCB + cha + 1]),
                             [bPa, b_vec], [b_vs[n2]])
                        k.op('act', lambda Pb=Pb, n2=n2, chb=chb: act.activation(out=gs[n2][:], in_=Pb[:], func=AF.Silu,
                                                                              bias=vec[:, V_FCB + chb:V_FCB + chb + 1]),
                             [bPb, b_vec], [b_gs[n2]])
                        k.op('dve', lambda n2=n2: dve.tensor_tensor(out=hst[n2][:], in0=vs[n2][:], in1=gs[n2][:], op=ALU.mult),
                             [b_vs[n2], b_gs[n2]], [b_hst[n2]])
                        k.dma('pool', HT_d[jj, :, tt * 512:(tt + 1) * 512], hst[n2][:], [b_hst[n2]], [bHT], b_hst[n2])
                k.barrier()
        with contextlib.ExitStack() as p7:
            wd = sb(p7, "wd", [128, 20, D], BF16); b_wd = k.bufs('wd', 20)
            wdf = [sb(p7, "wdf%d" % i, [128, D]) for i in range(2)]; b_wdf = k.bufs('wdf', 2)
            bdg = sb(p7, "bdg", [128, D]); b_bdg = k.buf('bdg')
            fnwb = sb(p7, "fnwb", [128, D]); b_fnwb = k.buf('fnwb')
            hl = [sb(p7, "hl%d" % i, [128, 20, 512], BF16) for i in range(2)]; b_hl = k.bufs('hl', 2)
            x1l = [sb(p7, "x1l%d" % i, [128, D]) for i in range(2)]; b_x1l = k.bufs('x1l', 2)
            x2 = [sb(p7, "x2_%d" % i, [128, D]) for i in range(2)]; b_x2 = k.bufs('x2', 2)
            ot = [sb(p7, "ot%d" % i, [128, D]) for i in range(2)]; b_ot = k.bufs('ot', 2)
            sq = sb(p7, "sq7", [128, D], BF16); b_sq = k.buf('sq7')
            st = [sb(p7, "st7_%d" % i, [128, 2]) for i in range(2)]; b_st = k.bufs('st7', 2)
            pD = [ps(p7, "pD%d" % i, [128, 512]) for i in range(4)]; b_pD = k.bufs('pD', 4)
            k.dma('sp', bdg[:], bdn_d.partition_broadcast(128), [bIN], [b_bdg], b_bdg)
            k.dma('sp', fnwb[:], fnw_d.partition_broadcast(128), [bIN], [b_fnwb], b_fnwb)
            k.op('dve', lambda: dve.tensor_tensor(out=bdg[:], in0=bdg[:], in1=g2b[:], op=ALU.mult), [b_bdg, b_g2b], [b_bdg])
            for j in range(20):
                k.dma('sp', wdf[j % 2][:], wdn_d[j * 128:(j + 1) * 128, :], [bIN], [b_wdf[j % 2]], b_wdf[j % 2])
                k.op('dve', lambda j=j: dve.tensor_tensor(out=wd[:, j, :], in0=wdf[j % 2][:], in1=g2b[:], op=ALU.mult),
                     [b_wdf[j % 2], b_g2b], [b_wd[j]])
            HTv = HT_d.rearrange("a p t -> p a t")
            def ld_hl(tb):
                k.dma('sp', hl[tb % 2][:], HTv[:, :, tb * 512:(tb + 1) * 512], [bHT], [b_hl[tb % 2]], b_hl[tb % 2])

            def ld_x1(i):
                k.dma('sp', x1l[i % 2][:], X1_d[i * 128:(i + 1) * 128, :], [bX1], [b_x1l[i % 2]], b_x1l[i % 2])
            ld_hl(0); ld_x1(0); ld_x1(1)
            for i in range(NT):
                if i % 4 == 0 and i // 4 + 1 < 8:
                    ld_hl(i // 4 + 1)
                Hh = hl[(i // 4) % 2]; bHh = b_hl[(i // 4) % 2]
                XL = x1l[i % 2]; bXL = b_x1l[i % 2]
                X2 = x2[i % 2]; bX2 = b_x2[i % 2]
                O = ot[i % 2]; bO = b_ot[i % 2]
                S = st[i % 2]; bS = b_st[i % 2]
                k.op('dve', lambda XL=XL: dve.tensor_tensor(out=XL[:], in0=XL[:], in1=bdg[:], op=ALU.add), [bXL, b_bdg], [bXL])
                for hf in range(2):
                    P = pD[(i * 2 + hf) % 4]; bP = b_pD[(i * 2 + hf) % 4]

                    def mmd(P=P, Hh=Hh, i=i, hf=hf):
                        last = None
                        for j in range(20):
                            last = pe.matmul(P[:], lhsT=Hh[:, j, (i % 4) * 128:(i % 4 + 1) * 128], rhs=wd[:, j, hf * 512:(hf + 1) * 512],
                                             start=(j == 0), stop=(j == 19))
                        return last
                    k.op('pe', mmd, [bHh] + b_wd, [bP])
                    k.op('dve', lambda P=P, XL=XL, X2=X2, hf=hf: dve.tensor_tensor(
                        out=X2[:, hf * 512:(hf + 1) * 512], in0=P[:], in1=XL[:, hf * 512:(hf + 1) * 512], op=ALU.add), [bP, bXL], [bX2])
                k.op('act', lambda X2=X2, S=S: act.activation(out=sq[:], in_=X2[:], func=AF.Square, accum_out=S[:, 0:1]),
                     [bX2], [b_sq, bS])
                k.op('dve', lambda S=S: dve.tensor_scalar(out=S[:, 1:2], in0=S[:, 0:1], scalar1=1.0 / D, scalar2=EPS,
                                                          op0=ALU.mult, op1=ALU.add), [bS], [bS])
                k.op('act', lambda S=S: act.activation(out=S[:, 1:2], in_=S[:, 1:2], func=AF.Sqrt), [bS], [bS])
                k.op('dve', lambda S=S: dve.reciprocal(out=S[:, 1:2], in_=S[:, 1:2]), [bS], [bS])
                k.op('dve', lambda X2=X2, S=S, O=O: dve.scalar_tensor_tensor(out=O[:], in0=X2[:], scalar=S[:, 1:2], in1=fnwb[:],
                                                                           op0=ALU.mult, op1=ALU.mult), [bX2, bS, b_fnwb], [bO])
                if i + 2 < NT:
                    ld_x1(i + 2)
                k.dma('pool', out_d[i * 128:(i + 1) * 128, :], O[:], [bO], [], bO)
            k.barrier()


def _consts():
    f32 = np.float32
    bf = ml_dtypes.bfloat16
    c = {}
    c['ident_bf'] = np.eye(128, dtype=f32).astype(bf)
    c['ident_f'] = np.eye(128, dtype=f32)
    j = np.arange(128)[:, None]
    l = np.arange(128)[None, :]
    c['masks'] = np.concatenate([(j <= l), (j >= l)], axis=1).astype(f32)
    sel = np.zeros((4, 4, 128), f32)
    for h in range(4):
        sel[h, h, :] = 1.0
    c['sel'] = sel.reshape(4, 512)
    l1 = np.arange(128, dtype=np.float64)[:, None, None]
    l2 = np.arange(32, dtype=np.float64)[None, :, None]
    k1 = np.arange(128, dtype=np.float64)[None, None, :]
    th = 2 * np.pi * (l1 * k1 / 128.0 + l2 * k1 / 4096.0)
    t1 = np.stack([np.cos(th), np.sin(th)], axis=2) / np.sqrt(128.0)
    c['dft1'] = t1.reshape(128, 32 * 256).astype(f32).astype(bf)
    a = np.arange(32, dtype=np.float64)
    ph = 2 * np.pi * np.outer(a, a) / 32.0
    cph, sph = np.cos(ph), np.sin(ph)
    w2 = np.block([[cph, sph], [-sph, cph]]) / np.sqrt(32.0)
    c['dft2'] = np.concatenate([w2, np.zeros_like(w2)], axis=0).astype(f32).astype(bf)
    b = np.arange(128, dtype=np.float64)
    ps_ = 2 * np.pi * np.outer(b, b) / 128.0
    c['dftc'] = (np.concatenate([np.cos(ps_), -np.sin(ps_)], axis=1) / np.sqrt(128.0)).astype(f32).astype(bf)
    return c


def prep_inputs(inp):
    f32 = np.float32
    g = lambda n: np.asarray(inp[n], dtype=f32)
    consts = _consts()
    pm = lambda v: np.ascontiguousarray(v.reshape(-1, 128).T)
    vec = np.zeros((128, V_END), f32)
    vec[:, V_N1W:V_N1W + 8] = pm(g('norm1_w')[0])
    vec[:, V_N2W:V_N2W + 8] = pm(g('norm2_w')[0])
    vec[:, V_BADA:V_BADA + 48] = pm(g('b_ada')[0])
    mcw = g('mconv_w')[0]
    for t in range(3):
        vec[:, V_MCW + np.arange(4) * 3 + t] = pm(mcw[t])
    vec[:, V_MCB:V_MCB + 4] = pm(g('mconv_b')[0])
    vec[:, V_MNW:V_MNW + 4] = pm(g('mnorm_w')[0])
    vec[:, V_MSK:V_MSK + 4] = pm(g('m_skip')[0])
    vec[:, V_BUP:V_BUP + 40] = pm(g('b_up')[0])
    fcw = g('fconv_w')[0].reshape(9, 5120)
    for t in range(9):
        vec[:, V_FCW + np.arange(40) * 9 + t] = pm(fcw[t])
    vec[:, V_FCB:V_FCB + 40] = pm(g('fconv_b')[0])
    vec[0:16, V_BG] = g('b_gate')[0]
    shared = {
        'vecs': vec,
        'b_ada': g('b_ada'), 'b_down': g('b_down'), 'final_norm_w': g('final_norm_w').reshape(1, D),
        'w_ada': g('w_ada')[0], 'w_in': g('w_in')[0], 'w_q': g('w_q')[0], 'w_k': g('w_k')[0],
        'w_out': g('w_out')[0], 'w_up': g('w_up')[0], 'w_down': g('w_down')[0],
    }
    shared.update(consts)
    x = g('x'); ctx = g('ctx'); c = g('c'); cctx = g('c_ctx')
    maps = []
    for b in range(8):
        cc = np.stack([pm(c[b]), pm(cctx)], axis=2).reshape(128, 16)
        m = dict(shared)
        m['x'] = x[b]
        m['ctx'] = ctx[b]
        m['cc'] = np.ascontiguousarray(cc)
        maps.append(m)
    return maps


def kernel(**inputs):
    nc = build()
    maps = prep_inputs(inputs)
    res = run_bass_kernel_spmd(nc, maps, core_ids=list(range(8)))
    return np.stack([np.asarray(r['out'], dtype=np.float32) for r in res.results], axis=0)
```

```python
import contextlib
import numpy as np
import ml_dtypes
import concourse.bass as bass
import concourse.mybir as mybir
from concourse.bass_utils import run_bass_kernel_spmd

F32 = mybir.dt.float32
BF16 = mybir.dt.bfloat16
ALU = mybir.AluOpType
AF = mybir.ActivationFunctionType
AX = mybir.AxisListType

T = 4096
D = 1024
CT = 256
NT = T // 128
EPS = 1e-6
import os
EVY_ACT = os.environ.get('EVY_ACT', '0') == '1'
SAME_ENG_SYNC = {'act': True, 'dve': True, 'pool': True, 'pe': False, 'sp': False}

V_N1W, V_N2W, V_BADA, V_MCW, V_MCB, V_MNW, V_MSK, V_BUP, V_FCW, V_FCB, V_BG, V_END = (
    0, 8, 16, 64, 76, 80, 84, 88, 128, 488, 528, 529)


def _merge(d, s):
    for k, v in s.items():
        if d.get(k, 0) < v:
            d[k] = v


class Buf:
    __slots__ = ('name', 'w', 'r', 'sem', 'semval', 'dram', 'key', 'excl')

    def __init__(self, name, dram=False):
        self.name = name
        self.w = {}
        self.r = {}
        self.sem = None
        self.semval = 0
        self.dram = dram
        self.key = None
        self.excl = name.startswith('p')


class K:
    def __init__(self, nc, es):
        self.nc = nc
        self.es = es
        self.eng = {'pe': nc.tensor, 'act': nc.scalar, 'dve': nc.vector, 'pool': nc.gpsimd, 'sp': nc.sync}
        self.sem = {}
        self.cur = {}
        self.waited = {n: {} for n in self.eng}
        for n in self.eng:
            self.sem[n] = es.enter_context(nc.semaphore('s_' + n))
            self.cur[n] = 0
        self.nbuf = 0
        self.ninst = 0

    def buf(self, name, dram=False):
        self.nbuf += 1
        return Buf('%s_%d' % (name, self.nbuf), dram)

    def bufs(self, name, n):
        return [self.buf(name) for _ in range(n)]

    def _wait(self, e, deps):
        for key, val in deps.items():
            if key == e and not SAME_ENG_SYNC[e]:
                continue
            if self.waited[e].get(key, 0) >= val:
                continue
            self.eng[e].wait_ge(self.sem[key], val)
            self.waited[e][key] = val
            self.ninst += 1

    def _deps(self, reads, writes):
        deps = {}
        for b in reads:
            _merge(deps, b.w)
            if b.excl:
                _merge(deps, b.r)
        for b in writes:
            _merge(deps, b.w)
            _merge(deps, b.r)
        return deps

    def _book(self, key, val, reads, writes):
        for b in writes:
            if b.dram:
                if b.w.get(key, 0) < val:
                    b.w[key] = val
            else:
                b.w = {key: val}
                b.r = {}
        for b in reads:
            if b.r.get(key, 0) < val:
                b.r[key] = val

    def op(self, e, fn, reads=(), writes=(), aw=()):
        deps = self._deps(reads, writes)
        for b in aw:
            _merge(deps, b.r)
        self._wait(e, deps)
        ins = fn()
        self.cur[e] += 1
        ins.then_inc(self.sem[e], 1)
        self.ninst += 1
        self._book(e, self.cur[e], reads, writes)
        for b in aw:
            if b.w.get(e, 0) < self.cur[e]:
                b.w[e] = self.cur[e]

    def dma(self, q, out, in_, reads, writes, sb):
        self._wait(q, self._deps(reads, writes))
        if sb.sem is None:
            sb.key = 'd_' + sb.name
            sb.sem = self.es.enter_context(self.nc.semaphore(sb.key))
            self.sem[sb.key] = sb.sem
            self.cur[sb.key] = 0
        self.cur[sb.key] += 16
        self.eng[q].dma_start(out=out, in_=in_).then_inc(sb.sem, 16)
        self.ninst += 1
        self._book(sb.key, self.cur[sb.key], reads, writes)

    def barrier(self, engines=None):
        allv = dict(self.cur)
        for e in (engines or self.eng):
            self._wait(e, {k: v for k, v in allv.items() if v > 0 and k != e})


def build(dbg=None):
    nc = bass.Bass("TRN2", target_bir_lowering=False)
    es = contextlib.ExitStack()
    with es:
        _build(nc, es, dbg)
    return nc


def _build(nc, es, dbg):
    k = K(nc, es)

    def din(name, shape, dt=F32):
        return nc.dram_tensor(name, list(shape), dt, kind="ExternalInput").ap()

    x_d = din("x", [T, D])
    ctx_d = din("ctx", [CT, D])
    cc_d = din("cc", [128, 16])
    vec_d = din("vecs", [128, V_END])
    bada_d = din("b_ada", [1, 6 * D])
    bdn_d = din("b_down", [1, D])
    fnw_d = din("final_norm_w", [1, D])
    wada_d = din("w_ada", [D, 6 * D])
    win_d = din("w_in", [D, 2064])
    wq_d = din("w_q", [4, 128, 128])
    wk_d = din("w_k", [4, 128, 128])
    wout_d = din("w_out", [D, D])
    wup_d = din("w_up", [D, 5120])
    wdn_d = din("w_down", [2560, D])
    idb_d = din("ident_bf", [128, 128], BF16)
    idf_d = din("ident_f", [128, 128])
    msk_d = din("masks", [128, 256])
    sel_d = din("sel", [4, 512])
    t1_d = din("dft1", [128, 32 * 256], BF16)
    w2_d = din("dft2", [128, 64], BF16)
    cd_d = din("dftc", [128, 256], BF16)
    out_d = nc.dram_tensor("out", [T, D], F32, kind="ExternalOutput").ap()

    U_d = nc.dram_tensor("scr_u", [T, 512], BF16).ap()
    ZD_d = nc.dram_tensor("scr_z", [2, 32, 128, 512], BF16).ap()
    SZ_d = nc.dram_tensor("scr_sz", [4, 128, T], BF16).ap()
    YT_d = nc.dram_tensor("scr_yt", [8, 128, T], BF16).ap()
    X1_d = nc.dram_tensor("scr_x1", [T, D], F32).ap()
    HT_d = nc.dram_tensor("scr_ht", [20, 128, T], BF16).ap()
    bU, bZD, bSZ, bYT, bX1, bHT = [k.buf(n, True) for n in ('U', 'ZD', 'SZ', 'YT', 'X1', 'HT')]
    bIN = k.buf('inputs', True)
    GD_d = nc.dram_tensor("scr_g", [16, T + CT], F32).ap()
    bGD = k.buf('GD', True)

    dbg_out = {}

    def dbg_tensor(name, shape, dt=F32):
        t = nc.dram_tensor("dbg_" + name, list(shape), dt, kind="ExternalOutput").ap()
        dbg_out[name] = t
        return t

    def sb(ph, name, shape, dt=F32):
        return ph.enter_context(nc.sbuf_tensor("sb_" + name, list(shape), dt))

    def ps(ph, name, shape, dt=F32):
        return ph.enter_context(nc.psum_tensor("ps_" + name, list(shape), dt))

    act, dve, pool, pe = nc.scalar, nc.vector, nc.gpsimd, nc.tensor

    with contextlib.ExitStack() as g0:
        vec = sb(g0, "vec", [128, V_END]); b_vec = k.buf('vec')
        idb = sb(g0, "idb", [128, 128], BF16); b_idb = k.buf('idb')
        idf = sb(g0, "idf", [128, 128]); b_idf = k.buf('idf')
        modT = sb(g0, "modT", [128, 48, 2]); b_modT = k.buf('modT')
        s1 = sb(g0, "s1", [128, 8, 2]); b_s1 = k.buf('s1')
        s2 = sb(g0, "s2", [128, 8]); b_s2 = k.buf('s2')
        g1b = sb(g0, "g1b", [128, D]); b_g1b = k.buf('g1b')
        g2b = sb(g0, "g2b", [128, D]); b_g2b = k.buf('g2b')
        k.dma('sp', vec[:], vec_d, [bIN], [b_vec], b_vec)
        k.dma('sp', idb[:], idb_d, [bIN], [b_idb], b_idb)
        k.dma('sp', idf[:], idf_d, [bIN], [b_idf], b_idf)

        with contextlib.ExitStack() as ph:
            wada = sb(ph, "wada", [128, 8, 6 * D], BF16); b_wada = k.bufs('wada', 8)
            cc = sb(ph, "cc", [128, 16]); b_cc = k.buf('cc')
            scc = sb(ph, "scc", [128, 8, 2], BF16); b_scc = k.buf('scc')
            rep = sb(ph, "rep", [128, 8, 128], BF16); b_rep = k.buf('rep')
            badab = sb(ph, "badab", [128, 2, D]); b_badab = k.buf('badab')
            psm = ps(ph, "psm", [128, 48, 2]); b_psm = k.buf('psm')
            psg = [ps(ph, "psg%d" % i, [128, 512]) for i in range(4)]; b_psg = k.bufs('psg', 4)
            k.dma('sp', cc[:], cc_d, [bIN], [b_cc], b_cc)
            wv = wada_d.rearrange("(j p) n -> p j n", p=128)
            for j in range(8):
                k.dma('pool', wada[:, j, :], wv[:, j, :], [bIN], [b_wada[j]], b_wada[j])
            k.dma('sp', badab[:, 0, :], bada_d[:, 2 * D:3 * D].partition_broadcast(128), [bIN], [b_badab], b_badab)
            k.dma('sp', badab[:, 1, :], bada_d[:, 5 * D:6 * D].partition_broadcast(128), [bIN], [b_badab], b_badab)
            k.op('act', lambda: act.activation(out=scc[:].rearrange("p j r -> p (j r)"), in_=cc[:], func=AF.Silu),
                 [b_cc], [b_scc])
            for j in range(8):
                k.op('dve', lambda j=j: dve.tensor_copy(out=rep[:, j, :], in_=scc[:, j, 0:1].to_broadcast([128, 128])),
                     [b_scc], [b_rep])
            secs = [0, 1, 3, 4]

            def mm_mod():
                last = None
                for s in secs:
                    for jj in range(8):
                        col = s * 8 + jj
                        for kk in range(8):
                            last = pe.matmul(psm[:, col, :], lhsT=wada[:, kk, col * 128:(col + 1) * 128],
                                             rhs=scc[:, kk, :], start=(kk == 0), stop=(kk == 7))
                return last
            k.op('pe', mm_mod, b_wada + [b_scc], [b_psm])
            k.op('dve', lambda: dve.tensor_tensor(
                out=modT[:], in0=psm[:], in1=vec[:, V_BADA:V_BADA + 48].unsqueeze(2).to_broadcast([128, 48, 2]),
                op=ALU.add), [b_psm, b_vec], [b_modT])
            k.op('dve', lambda: dve.scalar_tensor_tensor(
                out=s1[:], in0=modT[:, 8:16, :], scalar=1.0,
                in1=vec[:, V_N1W:V_N1W + 8].unsqueeze(2).to_broadcast([128, 8, 2]),
                op0=ALU.add, op1=ALU.mult), [b_modT, b_vec], [b_s1])
            k.op('dve', lambda: dve.scalar_tensor_tensor(
                out=s2[:], in0=modT[:, 32:40, 0], scalar=1.0, in1=vec[:, V_N2W:V_N2W + 8],
                op0=ALU.add, op1=ALU.mult), [b_modT, b_vec], [b_s2])
            for gi, sec in enumerate((2, 5)):
                for hf in range(2):
                    pt = psg[gi * 2 + hf]
                    c0 = sec * D + hf * 512

                    def mm_g(pt=pt, c0=c0):
                        last = None
                        for kk in range(8):
                            last = pe.matmul(pt[:], lhsT=rep[:, kk, :], rhs=wada[:, kk, c0:c0 + 512],
                                             start=(kk == 0), stop=(kk == 7))
                        return last
                    k.op('pe', mm_g, b_wada + [b_rep], [b_psg[gi * 2 + hf]])
                    dst = (g1b, g2b)[gi]
                    bd = (b_g1b, b_g2b)[gi]
                    k.op('dve', lambda pt=pt, dst=dst, gi=gi, hf=hf: dve.tensor_tensor(
                        out=dst[:, hf * 512:(hf + 1) * 512], in0=pt[:], in1=badab[:, gi, hf * 512:(hf + 1) * 512],
                        op=ALU.add), [b_psg[gi * 2 + hf], b_badab], [bd])
            if dbg == 'p0':
                t = dbg_tensor('modT', [128, 96])
                k.dma('sp', t, modT[:].rearrange("p a b -> p (a b)"), [b_modT], [], b_modT)
                t = dbg_tensor('g1b', [128, D])
                k.dma('sp', t, g1b[:], [b_g1b], [], b_g1b)
                t = dbg_tensor('s1', [128, 16])
                k.dma('sp', t, s1[:].rearrange("p a b -> p (a b)"), [b_s1], [], b_s1)
            k.barrier()
        if dbg == 'p0':
            k.barrier()
            return

        with contextlib.ExitStack() as gm:
            xmT = sb(gm, "xmT", [128, 4, T + 2], BF16); b_xmT = k.bufs('xmT', 4)
            xcT = sb(gm, "xcT", [128, 4, CT + 2], BF16); b_xcT = k.bufs('xcT', 4)
            Vaug = sb(gm, "Vaug", [128, NT, 4, 130], BF16); b_V = k.bufs('V', NT)
            Vc = sb(gm, "Vc", [128, 2, 4, 130], BF16); b_Vc = k.bufs('Vc', 2)
            with contextlib.ExitStack() as ph:
                hxT = sb(ph, "hxT", [128, 8, T], BF16); b_hxT = k.bufs('hxT', NT)
                hcT = sb(ph, "hcT", [128, 8, CT], BF16); b_hcT = k.bufs('hcT', 2)
                win = sb(ph, "win", [128, 8, 2064], BF16); b_win = k.bufs('win', 8)
                wvw = win_d.rearrange("(j p) n -> p j n", p=128)
                for j in range(8):
                    k.dma('pool', win[:, j, :], wvw[:, j, :], [bIN], [b_win[j]], b_win[j])
                with contextlib.ExitStack() as p1:
                    NB = 3
                    xt = [sb(p1, "xt%d" % i, [128, D]) for i in range(NB)]; b_xt = k.bufs('xt', NB)
                    xn = [sb(p1, "xn%d" % i, [128, D], BF16) for i in range(2)]; b_xn = k.bufs('xn', 2)
                    sq = sb(p1, "sq", [128, D], BF16); b_sq = k.buf('sq')
                    st = [sb(p1, "st%d" % i, [128, 2]) for i in range(2)]; b_st = k.bufs('st', 2)
                    ptrA = [ps(p1, "ptrA%d" % i, [128, 4, 128], BF16) for i in range(2)]; b_ptrA = k.bufs('ptrA', 2)
                    ptrB = [ps(p1, "ptrB%d" % i, [128, 4, 128], BF16) for i in range(2)]; b_ptrB = k.bufs('ptrB', 2)
                    tiles = [('c', i) for i in range(2)] + [('x', i) for i in range(NT)]

                    def load(n):
                        kind, i = tiles[n]
                        src = (ctx_d if kind == 'c' else x_d)[i * 128:(i + 1) * 128, :]
                        k.dma('sp', xt[n % NB][:], src, [bIN], [b_xt[n % NB]], b_xt[n % NB])
                    load(0); load(1)

                    def p1A(n):
                        kind, i = tiles[n]
                        if n + 2 < len(tiles):
                            load(n + 2)
                        X = xt[n % NB]; bX = b_xt[n % NB]
                        S = st[n % 2]; bS = b_st[n % 2]
                        XN = xn[n % 2]; bXN = b_xn[n % 2]
                        PA = ptrA[n % 2]; bPA = b_ptrA[n % 2]; PB = ptrB[n % 2]; bPB = b_ptrB[n % 2]
                        k.op('act', lambda X=X, S=S: act.activation(out=sq[:], in_=X[:], func=AF.Square, accum_out=S[:, 0:1]),
                             [bX], [b_sq, bS])
                        k.op('dve', lambda S=S: dve.tensor_scalar(out=S[:, 1:2], in0=S[:, 0:1], scalar1=1.0 / D, scalar2=EPS,
                                                                  op0=ALU.mult, op1=ALU.add), [bS], [bS])
                        k.op('act', lambda S=S: act.activation(out=S[:, 1:2], in_=S[:, 1:2], func=AF.Sqrt), [bS], [bS])
                        k.op('dve', lambda S=S: dve.reciprocal(out=S[:, 1:2], in_=S[:, 1:2]), [bS], [bS])
                        k.op('dve', lambda X=X, S=S, XN=XN: dve.tensor_scalar(out=XN[:], in0=X[:], scalar1=S[:, 1:2], scalar2=None, op0=ALU.mult),
                             [bX, bS], [bXN])

                        def tr(XN=XN, PA=PA, PB=PB):
                            last = None
                            for j in range(8):
                                last = pe.transpose((PA if j % 2 == 0 else PB)[:, j // 2, :], XN[:, j * 128:(j + 1) * 128], idb[:])
                            return last
                        k.op('pe', tr, [bXN, b_idb], [bPA, bPB])

                    def p1B(n):
                        kind, i = tiles[n]
                        PA = ptrA[n % 2]; bPA = b_ptrA[n % 2]; PB = ptrB[n % 2]; bPB = b_ptrB[n % 2]
                        col = 1 if kind == 'c' else 0
                        dstT = hcT if kind == 'c' else hxT
                        bD = (b_hcT if kind == 'c' else b_hxT)[i]
                        for j in range(8):
                            if j % 2 == 1:
                                k.op('act', lambda j=j, PB=PB, dstT=dstT, i=i, col=col: act.activation(
                                    out=dstT[:, j, i * 128:(i + 1) * 128], in_=PB[:, j // 2, :], func=AF.Identity,
                                    scale=s1[:, j, col:col + 1], bias=modT[:, j, col:col + 1]),
                                    [bPB, b_s1, b_modT], [], aw=[bD])
                            else:
                                k.op('dve', lambda j=j, PA=PA, dstT=dstT, i=i, col=col: dve.tensor_scalar(
                                    out=dstT[:, j, i * 128:(i + 1) * 128], in0=PA[:, j // 2, :],
                                    scalar1=s1[:, j, col:col + 1], scalar2=modT[:, j, col:col + 1],
                                    op0=ALU.mult, op1=ALU.add), [bPA, b_s1, b_modT], [], aw=[bD])
                    p1A(0)
                    for n in range(len(tiles)):
                        if n + 1 < len(tiles):
                            p1A(n + 1)
                        p1B(n)
                    if dbg == 'p1':
                        t = dbg_tensor('hxT', [128, 8 * T], BF16)
                        k.dma('sp', t, hxT[:].rearrange("p a b -> p (a b)"), b_hxT, [], b_hxT[0])
                        t = dbg_tensor('hcT', [128, 8 * CT], BF16)
                        k.dma('sp', t, hcT[:].rearrange("p a b -> p (a b)"), b_hcT, [], b_hcT[0])
                    k.barrier()
                if dbg == 'p1':
                    k.barrier()
                    return
                with contextlib.ExitStack() as p2:
                    pA = [ps(p2, "pA%d" % i, [128, 512]) for i in range(4)]; b_pA = k.bufs('pA', 4)
                    ust = [sb(p2, "ust%d" % i, [128, 512], BF16) for i in range(2)]; b_ust = k.bufs('ust', 2)
                    szs = [sb(p2, "szs%d" % i, [128, 512], BF16) for i in range(2)]; b_szs = k.bufs('szs', 2)
                    gst = [sb(p2, "gst%d" % i, [16, 512]) for i in range(2)]; b_gst = k.bufs('gst', 2)
                    pi = [0]

                    def nextp():
                        pi[0] += 1
                        return pA[pi[0] % 4], b_pA[pi[0] % 4]
                    k.op('dve', lambda: dve.memset(Vaug[:, :, :, 128:129], 1.0), [], b_V)
                    k.op('dve', lambda: dve.memset(Vc[:, :, :, 128:129], 1.0), [], b_Vc)
                    k.op('dve', lambda: dve.memset(xmT[:, :, 0:1], 0.0), [], b_xmT)
                    k.op('dve', lambda: dve.memset(xmT[:, :, T + 1:T + 2], 0.0), [], b_xmT)
                    k.op('dve', lambda: dve.memset(xcT[:, :, 0:1], 0.0), [], b_xcT)
                    k.op('dve', lambda: dve.memset(xcT[:, :, CT + 1:CT + 2], 0.0), [], b_xcT)
                    for i in range(NT):
                        P, bP = nextp()

                        def mm(P=P, i=i, c0=0):
                            last = None
                            for kk in range(8):
                                last = pe.matmul(P[:], lhsT=hxT[:, kk, i * 128:(i + 1) * 128], rhs=win[:, kk, c0:c0 + 512],
                                                 start=(kk == 0), stop=(kk == 7))
                            return last
                        k.op('pe', mm, [b_hxT[i]] + b_win, [bP])
                        us = ust[i % 2]; bus = b_ust[i % 2]
                        k.op('act', lambda P=P, us=us: act.copy(out=us[:], in_=P[:]), [bP], [bus])
                        k.dma('pool', U_d[i * 128:(i + 1) * 128, :], us[:], [bus], [bU], bus)
                        P, bP = nextp()
                        k.op('pe', lambda P=P, i=i: mm(P, i, 1024), [b_hxT[i]] + b_win, [bP])
                        k.op('dve', lambda P=P, i=i: dve.tensor_copy(out=Vaug[:, i, :, 0:128],
                                                                   in_=P[:].rearrange("p (h e) -> p h e", h=4)),
                             [bP], [b_V[i]])
                    for i in range(2):
                        P, bP = nextp()

                        def mmc(P=P, i=i):
                            last = None
                            for kk in range(8):
                                last = pe.matmul(P[:], lhsT=hcT[:, kk, i * 128:(i + 1) * 128], rhs=win[:, kk, 1024:1536],
                                                 start=(kk == 0), stop=(kk == 7))
                            return last
                        k.op('pe', mmc, [b_hcT[i]] + b_win, [bP])
                        k.op('dve', lambda P=P, i=i: dve.tensor_copy(out=Vc[:, i, :, 0:128],
                                                                   in_=P[:].rearrange("p (h e) -> p h e", h=4)),
                             [bP], [b_Vc[i]])
                    for tt in range(8):
                        hsl = b_hxT[tt * 4:(tt + 1) * 4]
                        for ch in range(4):
                            for which in range(2):
                                c0 = (512 if which == 0 else 1552) + ch * 128
                                P, bP = nextp()

                                def mmf(P=P, c0=c0, tt=tt, M=128):
                                    last = None
                                    for kk in range(8):
                                        last = pe.matmul(P[0:M, :], lhsT=win[:, kk, c0:c0 + M], rhs=hxT[:, kk, tt * 512:(tt + 1) * 512],
                                                         start=(kk == 0), stop=(kk == 7))
                                    return last
                                k.op('pe', mmf, hsl + b_win, [bP])
                                if which == 0:
                                    k.op('dve', lambda P=P, ch=ch, tt=tt: dve.tensor_copy(
                                        out=xmT[:, ch, 1 + tt * 512:1 + (tt + 1) * 512], in_=P[:]), [bP], [b_xmT[ch]])
                                else:
                                    zs = szs[(tt * 4 + ch) % 2]; bzs = b_szs[(tt * 4 + ch) % 2]
                                    k.op('act', lambda P=P, zs=zs: act.activation(out=zs[:], in_=P[:], func=AF.Silu), [bP], [bzs])
                                    k.dma('pool', SZ_d[ch, :, tt * 512:(tt + 1) * 512], zs[:], [bzs], [bSZ], bzs)
                        P, bP = nextp()
                        k.op('pe', lambda P=P, tt=tt: mmf(P, 1536, tt, 16), hsl + b_win, [bP])
                        gs = gst[tt % 2]; bgs = b_gst[tt % 2]
                        k.op('act', lambda P=P, gs=gs: act.activation(out=gs[:], in_=P[0:16, :],
                                                                     func=AF.Identity, bias=vec[0:16, V_BG:V_BG + 1]),
                             [bP, b_vec], [bgs])
                        k.dma('pool', GD_d[:, tt * 512:(tt + 1) * 512], gs[:], [bgs], [bGD], bgs)
                    for ch in range(4):
                        P, bP = nextp()

                        def mmx(P=P, c0=512 + ch * 128, M=128):
                            last = None
                            for kk in range(8):
                                last = pe.matmul(P[0:M, 0:CT], lhsT=win[:, kk, c0:c0 + M], rhs=hcT[:, kk, :],
                                                 start=(kk == 0), stop=(kk == 7))
                            return last
                        k.op('pe', mmx, b_hcT + b_win, [bP])
                        k.op('dve', lambda P=P, ch=ch: dve.tensor_copy(out=xcT[:, ch, 1:1 + CT], in_=P[:, 0:CT]), [bP], [b_xcT[ch]])
                    P, bP = nextp()
                    k.op('pe', lambda P=P: mmx(P, 1536, 16), b_hcT + b_win, [bP])
                    k.op('act', lambda P=P: act.activation(out=gst[0][:, 0:CT], in_=P[0:16, 0:CT], func=AF.Identity,
                                                           bias=vec[0:16, V_BG:V_BG + 1]), [bP, b_vec], [b_gst[0]])
                    k.dma('pool', GD_d[:, T:T + CT], gst[0][:, 0:CT], [b_gst[0]], [bGD], b_gst[0])
                    if dbg == 'p2':
                        t = dbg_tensor('xmT', [128, 4 * (T + 2)], BF16)
                        k.dma('sp', t, xmT[:].rearrange("p a b -> p (a b)"), b_xmT, [], b_xmT[0])
                        t = dbg_tensor('V', [128, NT * 4 * 130], BF16)
                        k.dma('sp', t, Vaug[:].rearrange("p a b c -> p (a b c)"), b_V, [], b_V[0])
                        k.barrier()
                        t = dbg_tensor('GD', [16, T + CT])
                        k.dma('sp', t, GD_d, [bGD], [], b_gst[0])
                        t = dbg_tensor('U', [T, 512], BF16)
                        k.dma('sp', t, U_d, [bU], [], b_gst[0])
                        t = dbg_tensor('SZ', [4 * 128, T], BF16)
                        k.dma('sp', t, SZ_d.rearrange("a p t -> (a p) t"), [bSZ], [], b_gst[0])
                    k.barrier()
            if dbg == 'p2':
                k.barrier()
                return
            NCH = NT + 2
            with contextlib.ExitStack() as p4:
                def aTv(h, a, b):
                    if b <= T:
                        return xmT[:, h, 1 + a:1 + b]
                    return xcT[:, h, 1 + a - T:1 + b - T]
                b_aT = [[b_xmT[i], b_xcT[i]] for i in range(4)]
                EC = sb(p4, "EC", [128, 2, NCH, 3, 4]); b_EC = k.bufs('EC', 2)
                WO = sb(p4, "WO", [128, 2, 4, NCH]); b_WO = k.buf('WO')
                mask = sb(p4, "mask", [128, 256]); b_mask = k.buf('mask')
                wqb = sb(p4, "wqb", [128, 4, 128], BF16); b_wqb = k.buf('wqb')
                wkb = sb(p4, "wkb", [128, 4, 128], BF16); b_wkb = k.buf('wkb')
                k.dma('sp', mask[:], msk_d, [bIN], [b_mask], b_mask)
                k.dma('pool', wqb[:], wq_d.rearrange("h d e -> d h e"), [bIN], [b_wqb], b_wqb)
                k.dma('pool', wkb[:], wk_d.rearrange("h d e -> d h e"), [bIN], [b_wkb], b_wkb)
                with contextlib.ExitStack() as pa:
                    ctmp = [sb(pa, "ctmp%d" % i, [128, 1024]) for i in range(2)]; b_ctmp = k.bufs('ctmp', 2)
                    nonlocal_n = [0]
                    n = 0
                    for ch in range(4):
                        w = lambda t, ch=ch: vec[:, V_MCW + ch * 3 + t:V_MCW + ch * 3 + t + 1]

                        def ctmp_of(src, bsrc, t0, ln, ch=ch, w=w):
                            nonlocal_n[0] += 1
                            tmp = ctmp[nonlocal_n[0] % 2]; btmp = b_ctmp[nonlocal_n[0] % 2]
                            k.op('dve', lambda: dve.tensor_scalar(
                                out=tmp[:, 0:ln], in0=src[:, ch, t0:t0 + ln], scalar1=w(0), scalar2=vec[:, V_MCB + ch:V_MCB + ch + 1],
                                op0=ALU.mult, op1=ALU.add), [bsrc[ch], b_vec], [btmp])
                            for t in (1, 2):
                                k.op('dve', lambda t=t: dve.scalar_tensor_tensor(
                                    out=tmp[:, 0:ln], in0=src[:, ch, t0 + t:t0 + t + ln], scalar=w(t), in1=tmp[:, 0:ln],
                                    op0=ALU.mult, op1=ALU.add), [bsrc[ch], b_vec, btmp], [btmp])
                            return tmp, btmp

                        def cwrite(src, bsrc, t0, ln, tmp, btmp, ch=ch):
                            k.op('act', lambda: act.activation(out=src[:, ch, 1 + t0:1 + t0 + ln], in_=tmp[:, 0:ln], func=AF.Silu),
                                 [btmp], [bsrc[ch]])
                        cur = ctmp_of(xmT, b_xmT, 0, 1024)
                        for sidx in range(4):
                            nxt = ctmp_of(xmT, b_xmT, (sidx + 1) * 1024, 1024) if sidx < 3 else None
                            cwrite(xmT, b_xmT, sidx * 1024, 1024, *cur)
                            cur = nxt
                        cc_ = ctmp_of(xcT, b_xcT, 0, CT)
                        cwrite(xcT, b_xcT, 0, CT, *cc_)
                    k.barrier()
                TT = T + CT
                orders = [[32, 33] + list(range(32)), [33, 32] + list(range(31, -1, -1))]
                with contextlib.ExitStack() as pb:
                    sel = sb(pb, "sel", [4, 512]); b_sel = k.buf('sel')
                    k.dma('sp', sel[:], sel_d, [bIN], [b_sel], b_sel)
                    t_li_ = [sb(pb, "t_li%d" % i, [4, TT]) for i in range(2)]; b_li_ = k.bufs('t_li', 2)
                    t_g_ = [sb(pb, "t_g%d" % i, [4, TT]) for i in range(2)]; b_g_ = k.bufs('t_g', 2)
                    t_p_ = [sb(pb, "t_p%d" % i, [4, TT]) for i in range(2)]; b_p_ = k.bufs('t_p', 2)
                    ones = sb(pb, "ones", [4, T], BF16); b_ones = k.buf('ones')
                    sm_ = [sb(pb, "sm%d" % i, [4, 8 + 4 * NCH]) for i in range(2)]; b_sm_ = k.bufs('sm', 2)
                    pEC = [ps(pb, "pEC%d" % i, [128, NCH, 3, 4]) for i in range(2)]; b_pEC = k.bufs('pEC', 2)
                    pWO = ps(pb, "pWO", [128, 2, 4, NCH]); b_pWO = k.buf('pWO')
                    k.op('dve', lambda: dve.memset(ones[:], 1.0), [], [b_ones])
                    segs = [(0, T), (T, TT)]
                    v3 = lambda tl: tl[:].rearrange("p (c j) -> p c j", j=128)
                    bc3 = lambda ap: ap.unsqueeze(2).to_broadcast([4, NCH, 128])
                    D2 = range(2)
                    for d in D2:
                        k.dma('sp', t_li_[d][:], GD_d[d * 8:d * 8 + 4, :], [bGD], [b_li_[d]], b_li_[d])
                        k.dma('sp', t_g_[d][:], GD_d[d * 8 + 4:d * 8 + 8, :], [bGD], [b_g_[d]], b_g_[d])
                    for d in D2:
                        t_g = t_g_[d]; b_g = b_g_[d]
                        k.op('act', lambda t_g=t_g: act.activation(out=t_g[:], in_=t_g[:], func=AF.Exp, scale=-1.0), [b_g], [b_g])
                        k.op('act', lambda t_g=t_g: act.activation(out=t_g[:], in_=t_g[:], func=AF.Ln, bias=1.0), [b_g], [b_g])
                    for si, (a0, a1) in enumerate(segs):
                        for d in D2:
                            t_g = t_g_[d]; b_g = b_g_[d]; t_p = t_p_[d]; b_p = b_p_[d]; sm = sm_[d]; b_sm = b_sm_[d]
                            k.op('dve', lambda a0=a0, a1=a1, t_p=t_p, t_g=t_g: dve.tensor_tensor_scan(
                                out=t_p[:, a0:a1], data0=ones[:, 0:a1 - a0], data1=t_g[:, a0:a1], initial=0.0,
                                op0=ALU.mult, op1=ALU.add), [b_ones, b_g], [b_p])
                            k.op('dve', lambda a1=a1, si=si, sm=sm, t_p=t_p: dve.tensor_copy(out=sm[:, si:si + 1], in_=t_p[:, a1 - 1:a1]),
                                 [b_p], [b_sm])
                            if d == 1:
                                k.op('dve', lambda a0=a0, a1=a1, si=si, sm=sm, t_p=t_p, t_g=t_g: dve.scalar_tensor_tensor(
                                    out=t_p[:, a0:a1], in0=t_g[:, a0:a1], scalar=sm[:, si:si + 1], in1=t_p[:, a0:a1],
                                    op0=ALU.add, op1=ALU.subtract), [b_g, b_sm, b_p], [b_p])
                    for d in D2:
                        t_li = t_li_[d]; b_li = b_li_[d]; t_p = t_p_[d]; b_p = b_p_[d]; sm = sm_[d]; b_sm = b_sm_[d]
                        cm = sm[:, 8:8 + NCH]
                        k.op('dve', lambda t_li=t_li, t_p=t_p: dve.tensor_tensor(out=t_li[:], in0=t_li[:], in1=t_p[:], op=ALU.add),
                             [b_li, b_p], [b_li])
                        k.op('dve', lambda cm=cm, t_li=t_li: dve.tensor_reduce(out=cm, in_=v3(t_li), axis=AX.X, op=ALU.max), [b_li], [b_sm])
                    prevs = [None, None]
                    for s_ in range(NCH):
                        for d in D2:
                            sm = sm_[d]; b_sm = b_sm_[d]
                            cm = sm[:, 8:8 + NCH]; MS = sm[:, 8 + NCH:8 + 2 * NCH]; ME = sm[:, 8 + 2 * NCH:8 + 3 * NCH]
                            c = orders[d][s_]; prev = prevs[d]
                            if s_ == 0:
                                k.op('dve', lambda c=c, MS=MS: dve.memset(MS[:, c:c + 1], 0.0), [], [b_sm])
                            elif s_ == 2:
                                k.op('dve', lambda c=c, prev=prev, MS=MS, ME=ME, sm=sm: dve.tensor_tensor(
                                    out=MS[:, c:c + 1], in0=ME[:, prev:prev + 1], in1=sm[:, 1:2], op=ALU.subtract), [b_sm], [b_sm])
                            else:
                                k.op('dve', lambda c=c, prev=prev, MS=MS, ME=ME: dve.tensor_copy(out=MS[:, c:c + 1], in_=ME[:, prev:prev + 1]),
                                     [b_sm], [b_sm])
                            k.op('dve', lambda c=c, MS=MS, ME=ME, cm=cm: dve.tensor_tensor(
                                out=ME[:, c:c + 1], in0=MS[:, c:c + 1], in1=cm[:, c:c + 1], op=ALU.max), [b_sm], [b_sm])
                            prevs[d] = c
                    for d in D2:
                        t_li = t_li_[d]; b_li = b_li_[d]; t_g = t_g_[d]; b_g = b_g_[d]; t_p = t_p_[d]; b_p = b_p_[d]
                        sm = sm_[d]; b_sm = b_sm_[d]
                        MS = sm[:, 8 + NCH:8 + 2 * NCH]; ME = sm[:, 8 + 2 * NCH:8 + 3 * NCH]
                        k.op('dve', lambda t_p=t_p, MS=MS: dve.tensor_tensor(out=v3(t_p), in0=v3(t_p), in1=bc3(MS), op=ALU.subtract),
                             [b_p, b_sm], [b_p])
                        k.op('act', lambda t_p=t_p: act.activation(out=t_p[:], in_=t_p[:], func=AF.Exp), [b_p], [b_p])
                        k.op('dve', lambda t_g=t_g, t_li=t_li, MS=MS: dve.tensor_tensor(out=v3(t_g), in0=v3(t_li), in1=bc3(MS), op=ALU.subtract),
                             [b_li, b_sm], [b_g])
                        k.op('act', lambda t_g=t_g: act.activation(out=t_g[:], in_=t_g[:], func=AF.Exp), [b_g], [b_g])
                    for d in D2:
                        t_li = t_li_[d]; b_li = b_li_[d]; sm = sm_[d]; b_sm = b_sm_[d]
                        MS = sm[:, 8 + NCH:8 + 2 * NCH]; ME = sm[:, 8 + 2 * NCH:8 + 3 * NCH]; WOL = sm[:, 8 + 3 * NCH:8 + 4 * NCH]
                        k.op('dve', lambda t_li=t_li, ME=ME: dve.tensor_tensor(out=v3(t_li), in0=v3(t_li), in1=bc3(ME), op=ALU.subtract),
                             [b_li, b_sm], [b_li])
                        k.op('act', lambda t_li=t_li: act.activation(out=t_li[:], in_=t_li[:], func=AF.Exp), [b_li], [b_li])
                        k.op('dve', lambda WOL=WOL, MS=MS, ME=ME: dve.tensor_tensor(out=WOL, in0=MS, in1=ME, op=ALU.subtract), [b_sm], [b_sm])
                        k.op('act', lambda WOL=WOL: act.activation(out=WOL, in_=WOL, func=AF.Exp), [b_sm], [b_sm])
                    for d in D2:
                        t_li = t_li_[d]; b_li = b_li_[d]; t_g = t_g_[d]; b_g = b_g_[d]; t_p = t_p_[d]; b_p = b_p_[d]
                        sm = sm_[d]; b_sm = b_sm_[d]; WOL = sm[:, 8 + 3 * NCH:8 + 4 * NCH]

                        def trE(d=d, t_g=t_g, t_li=t_li, t_p=t_p):
                            last = None
                            for c in range(NCH):
                                for q_, tl in enumerate((t_g, t_li, t_p)):
                                    last = pe.transpose(pEC[d][:, c, q_, :], tl[:, c * 128:(c + 1) * 128], idf[0:4, 0:4])
                            return last
                        k.op('pe', trE, [b_g, b_li, b_p, b_idf], [b_pEC[d]])
                        k.op('dve', lambda d=d: dve.tensor_copy(out=EC[:, d], in_=pEC[d][:]), [b_pEC[d]], [b_EC[d]])

                        def mmW(d=d, WOL=WOL):
                            last = None
                            for h in range(4):
                                last = pe.matmul(pWO[:, d, h, :], lhsT=sel[:, h * 128:(h + 1) * 128], rhs=WOL, start=True, stop=True)
                            return last
                        k.op('pe', mmW, [b_sel, b_sm], [b_pWO])
                        k.op('dve', lambda d=d: dve.tensor_copy(out=WO[:, d], in_=pWO[:, d]), [b_pWO], [b_WO])
                    k.barrier()
                if dbg == 'p4b':
                    t = dbg_tensor('EC', [128, 2 * NCH * 12])
                    k.dma('sp', t, EC[:].rearrange("p a b c d -> p (a b c d)"), b_EC, [], b_EC[0])
                    t = dbg_tensor('WO', [128, 8 * NCH])
                    k.dma('sp', t, WO[:].rearrange("p a b c -> p (a b c)"), [b_WO], [], b_WO)
                    k.barrier()
                    return
                with contextlib.ExitStack() as pc:
                    qT = sb(pc, "qT", [128, T], BF16); b_qT = k.bufs('qT', 8)
                    kT = sb(pc, "kT", [128, T], BF16); b_kT = k.bufs('kT', 8)
                    ktm = sb(pc, "ktm", [128, NCH, 128], BF16); b_ktm = k.bufs('ktm', NCH)
                    Hacc = sb(pc, "Hacc", [128, NT, 128]); b_H = k.bufs('Hacc', NT)
                    Hraw = sb(pc, "Hraw", [128, 2, NT, 130]); b_Hraw = [k.bufs('Hraw', NT) for i in range(2)]
                    rcb = sb(pc, "rcb", [128, 2, 3, NT]); b_rcb = k.bufs('rcb', 2)
                    Cf = [sb(pc, "Cf%d" % i, [128, 129]) for i in range(2)]; b_Cf = k.bufs('Cf', 2)
                    Cball = sb(pc, "Cball", [128, 2, NCH, 130], BF16); b_Cball = [k.bufs('Cball', NCH) for i in range(2)]
                    sTall = sb(pc, "sTall", [128, 2, NT, 128], BF16); b_sTall = [k.bufs('sTall', NT) for i in range(2)]
                    kw = [[sb(pc, "kw%d_%d" % (i, j), [128, 128], BF16) for j in range(3)] for i in range(2)]
                    b_kw = [k.bufs('kw', 3) for i in range(2)]
                    pP = [ps(pc, "pP%d" % i, [128, 512]) for i in range(2)]; b_pP = k.bufs('pP', 2)
                    pS = [ps(pc, "pS%d" % i, [128, 128]) for i in range(2)]; b_pS = k.bufs('pS', 2)
                    pH = [ps(pc, "pH%d" % i, [128, 129]) for i in range(2)]; b_pH = k.bufs('pH', 2)
                    pC = [ps(pc, "pC%d" % i, [128, 129]) for i in range(2)]; b_pC = k.bufs('pC', 2)
                    pT = [pP[i][:].bitcast(BF16)[:, 0:512].rearrange("p (a b) -> p a b", b=128) for i in range(2)]; b_pT = b_pP
                    t1 = [sb(pc, "t1_%d" % i, [128, 512]) for i in range(2)]; b_t1 = k.bufs('t1', 2)
                    szl = [sb(pc, "szl%d" % i, [128, 512], BF16) for i in range(2)]; b_szl = k.bufs('szl', 2)
                    ysb = [sb(pc, "ysb%d" % i, [128, 512], BF16) for i in range(2)]; b_ysb = k.bufs('ysb', 2)
                    ppi = [0]
                    KS = 128 ** -0.5
                    for h in ([1] if dbg == 'p4only1' else range(4)):
                        for tt in range(8):
                            for which in range(2):
                                ppi[0] += 1
                                P = pP[ppi[0] % 2]; bP = b_pP[ppi[0] % 2]
                                wb = wqb if which == 0 else wkb
                                bwb = b_wqb if which == 0 else b_wkb
                                k.op('pe', lambda P=P, wb=wb, tt=tt, h=h: pe.matmul(
                                    P[:], lhsT=wb[:, h, :], rhs=aTv(h, tt * 512, (tt + 1) * 512), start=True, stop=True),
                                    [bwb] + b_aT[h], [bP])
                                if which == 0:
                                    k.op('act', lambda P=P, tt=tt: act.copy(out=qT[:, tt * 512:(tt + 1) * 512], in_=P[:]), [bP], [b_qT[tt]])
                                else:
                                    k.op('act', lambda P=P, tt=tt: act.mul(out=kT[:, tt * 512:(tt + 1) * 512], in_=P[:], mul=KS), [bP], [b_kT[tt]])
                        for c0 in range(0, NCH, 4):
                            ppi[0] += 1
                            P = pP[ppi[0] % 2]; bP = b_pP[ppi[0] % 2]
                            nn = min(4, NCH - c0)

                            def mmk(P=P, c0=c0, nn=nn, h=h):
                                last = None
                                for i in range(nn):
                                    c = c0 + i
                                    last = pe.matmul(P[:, i * 128:(i + 1) * 128], lhsT=aTv(h, c * 128, (c + 1) * 128), rhs=wkb[:, h, :],
                                                     start=True, stop=True)
                                return last
                            k.op('pe', mmk, [b_wkb] + b_aT[h], [bP])
                            k.op('dve', lambda P=P, c0=c0, nn=nn: dve.tensor_scalar(
                                out=ktm[:, c0:c0 + nn, :], in0=P[:, 0:nn * 128].rearrange("p (a b) -> p a b", b=128),
                                scalar1=KS, scalar2=None, op0=ALU.mult), [bP], b_ktm[c0:c0 + nn])
                        nS = 0
                        nC = 0
                        for s_ in range(NCH):
                            for d in range(2):
                                c = orders[d][s_]
                                isctx = c >= NT
                                Vt = Vc[:, c - NT, h, 0:129] if isctx else Vaug[:, c, h, 0:129]
                                bV = b_Vc[c - NT] if isctx else b_V[c]
                                if not isctx:
                                    tq = c // 4
                                    S_ = pS[nS % 2]; bS_ = b_pS[nS % 2]; nS += 1
                                    k.op('pe', lambda S_=S_, c=c: pe.matmul(
                                        S_[:], lhsT=kT[:, c * 128:(c + 1) * 128], rhs=qT[:, c * 128:(c + 1) * 128], start=True, stop=True),
                                        [b_kT[tq], b_qT[tq]], [bS_])
                                    k.op('dve', lambda S_=S_, d=d, c=c, h=h: dve.scalar_tensor_tensor(
                                        out=sTall[:, d, c, :], in0=S_[:], scalar=EC[:, d, c, 0, h:h + 1], in1=mask[:, d * 128:(d + 1) * 128],
                                        op0=ALU.mult, op1=ALU.mult), [bS_, b_EC[d], b_mask], [b_sTall[d][c]])
                                kwt = kw[d][s_ % 3]; bkw = b_kw[d][s_ % 3]
                                k.op('act', lambda kwt=kwt, c=c, d=d, h=h: act.activation(
                                    out=kwt[:], in_=ktm[:, c, :], func=AF.Copy, scale=EC[:, d, c, 1, h:h + 1]),
                                    [b_ktm[c], b_EC[d]], [bkw])
                                C_ = pC[nC % 2]; bC_ = b_pC[nC % 2]; nC += 1
                                k.op('pe', lambda C_=C_, kwt=kwt, Vt=Vt: pe.matmul(C_[:], lhsT=kwt[:], rhs=Vt, start=True, stop=True),
                                     [bkw, bV], [bC_])
                                if s_ == 0:
                                    k.op('dve', lambda C_=C_, d=d: dve.tensor_copy(out=Cf[d][:], in_=C_[:]), [bC_], [b_Cf[d]])
                                else:
                                    k.op('dve', lambda C_=C_, d=d, h=h, c=c: dve.scalar_tensor_tensor(
                                        out=Cf[d][:], in0=Cf[d][:], scalar=WO[:, d, h, c:c + 1], in1=C_[:],
                                        op0=ALU.mult, op1=ALU.add), [bC_, b_Cf[d], b_WO], [b_Cf[d]])
                                if s_ < NCH - 1:
                                    k.op('pool', lambda d=d, s_=s_: pool.tensor_copy(out=Cball[:, d, s_, 0:129], in_=Cf[d][:]),
                                         [b_Cf[d]], [b_Cball[d][s_]])
                        for s_ in range(2, NCH):
                            for d in range(2):
                                c = orders[d][s_]
                                tq = c // 4
                                H_ = pH[d]; bH_ = b_pH[d]

                                def mmh(H_=H_, c=c, d=d, s_=s_, h=h):
                                    pe.matmul(H_[:], lhsT=qT[:, c * 128:(c + 1) * 128], rhs=Cball[:, d, s_ - 1, 0:129], start=True, stop=False)
                                    return pe.matmul(H_[:], lhsT=sTall[:, d, c, :], rhs=Vaug[:, c, h, 0:129], start=False, stop=True)
                                k.op('pe', mmh, [b_qT[tq], b_Cball[d][s_ - 1], b_sTall[d][c], b_V[c]], [bH_])
                                if d == 0:
                                    k.op('act', lambda H_=H_, c=c: act.copy(out=Hraw[:, 0, c, 0:129], in_=H_[:]), [bH_], [b_Hraw[0][c]])
                                else:
                                    k.op('dve', lambda H_=H_, c=c: dve.tensor_copy(out=Hraw[:, 1, c, 0:129], in_=H_[:]), [bH_], [b_Hraw[1][c]])
                        for d in range(2):
                            den = Hraw[:, d, :, 128]
                            e3 = EC[:, d, 0:NT, 2, h]
                            neg = rcb[:, d, 0, :]; cl = rcb[:, d, 1, :]; rc = rcb[:, d, 2, :]
                            k.op('dve', lambda den=den, neg=neg: dve.tensor_scalar(out=neg, in0=den, scalar1=-1.0, scalar2=None, op0=ALU.mult),
                                 b_Hraw[d], [b_rcb[d]])
                            k.op('dve', lambda den=den, neg=neg, cl=cl: dve.tensor_tensor(out=cl, in0=den, in1=neg, op=ALU.max),
                                 b_Hraw[d] + [b_rcb[d]], [b_rcb[d]])
                            k.op('dve', lambda e3=e3, cl=cl: dve.tensor_tensor(out=cl, in0=cl, in1=e3, op=ALU.max), [b_EC[d], b_rcb[d]], [b_rcb[d]])
                            k.op('dve', lambda cl=cl, rc=rc: dve.reciprocal(out=rc, in_=cl), [b_rcb[d]], [b_rcb[d]])
                        bcr = lambda ap: ap.unsqueeze(2).to_broadcast([128, NT, 128])
                        k.op('dve', lambda: dve.tensor_tensor(out=Hacc[:], in0=Hraw[:, 0, :, 0:128], in1=bcr(rcb[:, 0, 2, :]), op=ALU.mult),
                             b_Hraw[0] + [b_rcb[0]], b_H)
                        k.op('pool', lambda: pool.tensor_tensor(out=Hraw[:, 1, :, 0:128], in0=Hraw[:, 1, :, 0:128], in1=bcr(rcb[:, 1, 2, :]),
                                                                op=ALU.mult), b_Hraw[1] + [b_rcb[1]], b_Hraw[1])
                        k.op('dve', lambda: dve.tensor_tensor(out=Hacc[:], in0=Hacc[:], in1=Hraw[:, 1, :, 0:128], op=ALU.add),
                             b_H + b_Hraw[1], b_H)
                        if dbg == 'p4s1' and h == 1:
                            k.barrier()
                            return
                        if dbg == 'p4c' and h == 0:
                            t = dbg_tensor('Hacc', [128, NT * 128])
                            k.dma('sp', t, Hacc[:].rearrange("p a b -> p (a b)"), b_H, [], b_H[0])
                            t = dbg_tensor('Cf', [128, 129])
                            k.dma('sp', t, Cf[0][:], [b_Cf[0]], [], b_Cf[0])
                            k.barrier()
                            return
                        HC = Hraw[:, 0, :, 0:128]
                        SQ = Hraw[:, 1, :, 0:128]
                        HB = sTall[:, 0]
                        mean = rcb[:, 0, 0, :]; rstd = rcb[:, 0, 1, :]
                        k.op('dve', lambda: dve.tensor_reduce(out=mean, in_=Hacc[:], axis=AX.X, op=ALU.add), b_H, [b_rcb[0]])
                        k.op('dve', lambda: dve.tensor_scalar(out=mean, in0=mean, scalar1=1.0 / 128, scalar2=None, op0=ALU.mult),
                             [b_rcb[0]], [b_rcb[0]])
                        k.op('pool', lambda: pool.tensor_tensor(out=HC, in0=Hacc[:], in1=bcr(mean), op=ALU.subtract),
                             b_H + [b_rcb[0]], b_Hraw[0])
                        k.op('act', lambda: act.activation(out=SQ, in_=HC, func=AF.Square), b_Hraw[0], b_Hraw[1])
                        k.op('dve', lambda: dve.tensor_reduce(out=rstd, in_=SQ, axis=AX.X, op=ALU.add), b_Hraw[1], [b_rcb[0]])
                        k.op('dve', lambda: dve.tensor_scalar(out=rstd, in0=rstd, scalar1=1.0 / 128, scalar2=EPS, op0=ALU.mult, op1=ALU.add),
                             [b_rcb[0]], [b_rcb[0]])
                        k.op('act', lambda: act.activation(out=rstd, in_=rstd, func=AF.Sqrt), [b_rcb[0]], [b_rcb[0]])
                        k.op('dve', lambda: dve.reciprocal(out=rstd, in_=rstd), [b_rcb[0]], [b_rcb[0]])
                        k.op('dve', lambda: dve.tensor_tensor(out=HB, in0=HC, in1=bcr(rstd), op=ALU.mult),
                             b_Hraw[0] + [b_rcb[0]], b_sTall[0])

                        def ho_tr(tb):
                            pt = pT[tb % 2]; bpt = b_pT[tb % 2]

                            def trh(pt=pt, tb=tb):
                                last = None
                                for i in range(4):
                                    last = pe.transpose(pt[:, i, :], HB[:, tb * 4 + i, :], idb[:])
                                return last
                            k.op('pe', trh, b_sTall[0][tb * 4:(tb + 1) * 4] + [b_idb], [bpt])
                            zl = szl[tb % 2]; bzl = b_szl[tb % 2]
                            k.dma('sp', zl[:], SZ_d[h, :, tb * 512:(tb + 1) * 512], [bSZ], [bzl], bzl)

                        def ho_out(tb):
                            pt = pT[tb % 2]; bpt = b_pT[tb % 2]
                            tl = t1[tb % 2]; btl = b_t1[tb % 2]
                            zl = szl[tb % 2]; bzl = b_szl[tb % 2]
                            yb = ysb[tb % 2]; byb = b_ysb[tb % 2]
                            k.op('act', lambda tl=tl, tb=tb, h=h: act.activation(
                                out=tl[:], in_=aTv(h, tb * 512, (tb + 1) * 512), func=AF.Copy, scale=vec[:, V_MSK + h:V_MSK + h + 1]),
                                b_aT[h] + [b_vec], [btl])
                            k.op('dve', lambda tl=tl, pt=pt, h=h: dve.scalar_tensor_tensor(
                                out=tl[:], in0=pt.rearrange("p a b -> p (a b)"), scalar=vec[:, V_MNW + h:V_MNW + h + 1], in1=tl[:],
                                op0=ALU.mult, op1=ALU.add), [bpt, btl, b_vec], [btl])
                            k.op('pool', lambda tl=tl, zl=zl, yb=yb: pool.tensor_tensor(out=yb[:], in0=tl[:], in1=zl[:], op=ALU.mult),
                                 [btl, bzl], [byb])
                            k.dma('pool', YT_d[4 + h, :, tb * 512:(tb + 1) * 512], yb[:], [byb], [bYT], byb)
                        ho_tr(0)
                        for tb in range(8):
                            if tb + 1 < 8:
                                ho_tr(tb + 1)
                            ho_out(tb)
                        if dbg in ('p4h0', 'p4only1') or (dbg == 'p4h1' and h == 1):
                            k.barrier()
                            return
                    k.barrier()
            k.barrier()
        if dbg == 'p4':
            with contextlib.ExitStack() as pd:
                tmpd = sb(pd, "tmpd", [128, 4, T], BF16); b_tmpd = k.buf('tmpd')
                k.dma('sp', tmpd[:], YT_d[4:8].rearrange("a p t -> p a t"), [bYT], [b_tmpd], b_tmpd)
                t = dbg_tensor('ym', [128, 4 * T], BF16)
                k.dma('sp', t, tmpd[:].rearrange("p a t -> p (a t)"), [b_tmpd], [], b_tmpd)
                k.barrier()
            return
        with contextlib.ExitStack() as f1:
            T1 = sb(f1, "T1", [128, 32, 2, 128], BF16); b_T1 = k.buf('T1')
            Ul = sb(f1, "Ul", [128, 32, 512], BF16); b_Ul = k.bufs('Ul', 4)
            zs = [sb(f1, "zs%d" % i, [128, 4, 2, 512], BF16) for i in range(2)]
            b_zs = [[[k.buf('zs') for _ in range(2)] for _ in range(4)] for _ in range(2)]
            pZ = [ps(f1, "pZ%d" % i, [128, 512]) for i in range(4)]; b_pZ = k.bufs('pZ', 4)
            k.dma('sp', T1[:].rearrange("p a b c -> p (a b c)"), t1_d, [bIN], [b_T1], b_T1)
            Uv = U_d.rearrange("(a b) c -> a b c", b=32)
            for q in range(4):
                k.dma('sp', Ul[:, q * 8:(q + 1) * 8, :], Uv[:, q * 8:(q + 1) * 8, :], [bU], [b_Ul[q]], b_Ul[q])
            n = 0
            for l2 in range(32):
                grp, i = l2 // 4, l2 % 4
                Z = zs[grp % 2]
                for ri in range(2):
                    P = pZ[n % 4]; bP = b_pZ[n % 4]
                    k.op('pe', lambda P=P, l2=l2, ri=ri: pe.matmul(P[:], lhsT=T1[:, l2, ri, :], rhs=Ul[:, l2, :], start=True, stop=True),
                         [b_T1, b_Ul[l2 // 8]], [bP])
                    bz = b_zs[grp % 2][i][ri]
                    if n % 2 == 0:
                        k.op('act', lambda P=P, Z=Z, i=i, ri=ri: act.copy(out=Z[:, i, ri, :], in_=P[:]), [bP], [bz])
                    else:
                        k.op('dve', lambda P=P, Z=Z, i=i, ri=ri: dve.tensor_copy(out=Z[:, i, ri, :], in_=P[:]), [bP], [bz])
                    n += 1
                if i == 3:
                    for ri in range(2):
                        rb = [b_zs[grp % 2][ii][ri] for ii in range(4)]
                        k.dma('pool', ZD_d[ri, l2 - 3:l2 + 1].rearrange("l k c -> k l c"), Z[:, :, ri, :], rb, [bZD], rb[0])
            k.barrier()
        if dbg == 'p3a':
            k.barrier()
            return
        with contextlib.ExitStack() as f2:
            W2 = sb(f2, "W2", [128, 64], BF16); b_W2 = k.buf('W2')
            cd = sb(f2, "cd", [128, 256], BF16); b_cd = k.buf('cd')
            Yt = sb(f2, "Yt", [128, 4, 2, T], BF16)
            b_Yt = [[k.buf('Yt') for _ in range(32)] for _ in range(4)]
            Zt = [sb(f2, "Zt%d" % i, [128, 16, 512], BF16) for i in range(2)]; b_Zt = k.bufs('Zt', 2)
            for i in range(2):
                k.op('dve', lambda i=i: dve.memset(Zt[i][:], 0.0), [], [b_Zt[i]])
            pY = [ps(f2, "pY%d" % i, [128, 8, 64]) for i in range(4)]; b_pY = k.bufs('pY', 4)
            pF = [ps(f2, "pF%d" % i, [128, 512]) for i in range(4)]; b_pF = k.bufs('pF', 4)
            yst = [sb(f2, "yst%d" % i, [128, 512], BF16) for i in range(4)]; b_yst = k.bufs('yst', 4)
            k.dma('sp', W2[:], w2_d, [bIN], [b_W2], b_W2)
            k.dma('sp', cd[:], cd_d, [bIN], [b_cd], b_cd)
            ZDv = ZD_d.rearrange("r l k c -> (r l) k c")
            n = 0
            for slab in range(8):
                Zs = Zt[slab % 2]; bZs = b_Zt[slab % 2]
                k.dma('sp', Zs[0:64], ZDv[:, slab * 16:(slab + 1) * 16, :], [bZD], [bZs], bZs)
                if dbg == 'p3b0':
                    k.barrier()
                    return
                for g in range(4):
                    for half in range(2):
                        P = pY[n % 4]; bP = b_pY[n % 4]

                        def mmy(P=P, Zs=Zs, g=g, half=half):
                            last = None
                            for i in range(8):
                                last = pe.matmul(P[:, i, :], lhsT=Zs[:, half * 8 + i, g * 128:(g + 1) * 128], rhs=W2[:], start=True, stop=True)
                            return last
                        k.op('pe', mmy, [bZs, b_W2], [bP])
                        k1_0 = slab * 16 + half * 8
                        for ri in range(2):
                            bo = b_Yt[g][(slab * 2 + half) * 2 + ri]
                            ov = Yt[:, g, ri, :].rearrange("p (k2 k1) -> p k2 k1", k1=128)[:, :, k1_0:k1_0 + 8]
                            iv = P[:, :, ri * 32:(ri + 1) * 32].rearrange("p i k2 -> p k2 i")
                            if n % 2 == 0:
                                k.op('act', lambda ov=ov, iv=iv: act.copy(out=ov, in_=iv), [bP], [bo])
                            else:
                                k.op('dve', lambda ov=ov, iv=iv: dve.tensor_copy(out=ov, in_=iv), [bP], [bo])
                        n += 1
                if dbg in ('p3b1', 'p3b2'):
                    k.barrier()
                    return
            if dbg == 'p3b':
                k.barrier()
                return
            n = 0
            for g in range(4):
                for tt in range(8):
                    P = pF[n % 4]; bP = b_pF[n % 4]

                    def mmc2(P=P, g=g, tt=tt):
                        pe.matmul(P[:], lhsT=cd[:, 0:128], rhs=Yt[:, g, 0, tt * 512:(tt + 1) * 512], start=True, stop=False)
                        return pe.matmul(P[:], lhsT=cd[:, 128:256], rhs=Yt[:, g, 1, tt * 512:(tt + 1) * 512], start=False, stop=True)
                    k.op('pe', mmc2, [b_cd] + b_Yt[g], [bP])
                    ys = yst[n % 4]; bys = b_yst[n % 4]
                    if n % 2 == 0:
                        k.op('act', lambda P=P, ys=ys: act.copy(out=ys[:], in_=P[:]), [bP], [bys])
                    else:
                        k.op('dve', lambda P=P, ys=ys: dve.tensor_copy(out=ys[:], in_=P[:]), [bP], [bys])
                    k.dma('pool', YT_d[g, :, tt * 512:(tt + 1) * 512], ys[:], [bys], [bYT], bys)
                    n += 1
            k.barrier()
        if dbg == 'p3':
            with contextlib.ExitStack() as pd:
                tmpd = sb(pd, "tmpd", [128, 8, T], BF16); b_tmpd = k.buf('tmpd')
                k.dma('sp', tmpd[:], YT_d.rearrange("a p t -> p a t"), [bYT], [b_tmpd], b_tmpd)
                t = dbg_tensor('yt', [128, 8 * T], BF16)
                k.dma('sp', t, tmpd[:].rearrange("p a t -> p (a t)"), [b_tmpd], [], b_tmpd)
                k.barrier()
            return
        gw7 = contextlib.ExitStack()
        wd = sb(gw7, "wd", [128, 20, D], BF16); b_wd = k.bufs('wd', 20)
        wdf = [sb(gw7, "wdf%d" % i, [128, D]) for i in range(1)] * 2; b_wdf = k.bufs('wdf', 1) * 2
        bdg = sb(gw7, "bdg", [128, D]); b_bdg = k.buf('bdg')
        fnwb = sb(gw7, "fnwb", [128, D]); b_fnwb = k.buf('fnwb')

        def prep_wd(j):
            if j == 0:
                k.dma('sp', bdg[:], bdn_d.partition_broadcast(128), [bIN], [b_bdg], b_bdg)
                k.dma('sp', fnwb[:], fnw_d.partition_broadcast(128), [bIN], [b_fnwb], b_fnwb)
                k.op('dve', lambda: dve.tensor_tensor(out=bdg[:], in0=bdg[:], in1=g2b[:], op=ALU.mult), [b_bdg, b_g2b], [b_bdg])
            k.dma('sp', wdf[j % 2][:], wdn_d[j * 128:(j + 1) * 128, :], [bIN], [b_wdf[j % 2]], b_wdf[j % 2])
            k.op('dve', lambda j=j: dve.tensor_tensor(out=wd[:, j, :], in0=wdf[j % 2][:], in1=g2b[:], op=ALU.mult),
                 [b_wdf[j % 2], b_g2b], [b_wd[j]])
        with contextlib.ExitStack() as gf:
            hx2T = sb(gf, "hx2T", [128, 8, T], BF16); b_hx2T = k.bufs('hx2T', NT)
            with contextlib.ExitStack() as p5:
                wo = sb(p5, "wo", [128, 8, D], BF16); b_wo = k.bufs('wo', 8)
                wof = [sb(p5, "wof%d" % i, [128, D]) for i in range(2)]; b_wof = k.bufs('wof', 2)
                yl = [sb(p5, "yl%d" % i, [128, 8, 512], BF16) for i in range(2)]; b_yl = k.bufs('yl', 2)
                xt = [sb(p5, "x5t%d" % i, [128, D]) for i in range(2)]; b_xt = k.bufs('x5t', 2)
                x1t = [sb(p5, "x1t%d" % i, [128, D]) for i in range(2)]; b_x1t = k.bufs('x1t', 2)
                xn = [sb(p5, "x5n%d" % i, [128, D], BF16) for i in range(2)]; b_xn = k.bufs('x5n', 2)
                sq = sb(p5, "sq5", [128, D], BF16); b_sq = k.buf('sq5')
                st = [sb(p5, "st5_%d" % i, [128, 2]) for i in range(2)]; b_st = k.bufs('st5', 2)
                pO = [ps(p5, "pO%d" % i, [128, 512]) for i in range(4)]; b_pO = k.bufs('pO', 4)
                ptrA = [ps(p5, "ptr5A%d" % i, [128, 4, 128], BF16) for i in range(2)]; b_ptrA = k.bufs('ptr5A', 2)
                ptrB = [ps(p5, "ptr5B%d" % i, [128, 4, 128], BF16) for i in range(2)]; b_ptrB = k.bufs('ptr5B', 2)
                for j in range(8):
                    k.dma('sp', wof[j % 2][:], wout_d[j * 128:(j + 1) * 128, :], [bIN], [b_wof[j % 2]], b_wof[j % 2])
                    k.op('dve', lambda j=j: dve.tensor_tensor(out=wo[:, j, :], in0=wof[j % 2][:], in1=g1b[:], op=ALU.mult),
                         [b_wof[j % 2], b_g1b], [b_wo[j]])
                YTv = YT_d.rearrange("a p t -> p a t")

                def ld_yl(tb):
                    k.dma('sp', yl[tb % 2][:], YTv[:, :, tb * 512:(tb + 1) * 512], [bYT], [b_yl[tb % 2]], b_yl[tb % 2])

                def ld_x(i):
                    k.dma('sp', xt[i % 2][:], x_d[i * 128:(i + 1) * 128, :], [bIN], [b_xt[i % 2]], b_xt[i % 2])

                def stageA(i):
                    if i % 4 == 0 and i // 4 + 1 < 8:
                        ld_yl(i // 4 + 1)
                    Y = yl[(i // 4) % 2]; bY = b_yl[(i // 4) % 2]
                    X = xt[i % 2]; bX = b_xt[i % 2]
                    X1 = x1t[i % 2]; bX1t = b_x1t[i % 2]
                    for hf in range(2):
                        P = pO[(i * 2 + hf) % 4]; bP = b_pO[(i * 2 + hf) % 4]

                        def mmo(P=P, Y=Y, i=i, hf=hf):
                            last = None
                            for j in range(8):
                                last = pe.matmul(P[:], lhsT=Y[:, j, (i % 4) * 128:(i % 4 + 1) * 128], rhs=wo[:, j, hf * 512:(hf + 1) * 512],
                                                 start=(j == 0), stop=(j == 7))
                            return last
                        k.op('pe', mmo, [bY] + b_wo, [bP])
                        k.op('dve', lambda P=P, X=X, X1=X1, hf=hf: dve.tensor_tensor(
                            out=X1[:, hf * 512:(hf + 1) * 512], in0=P[:], in1=X[:, hf * 512:(hf + 1) * 512], op=ALU.add), [bP, bX], [bX1t])
                    if i + 2 < NT:
                        ld_x(i + 2)
                    k.dma('pool', X1_d[i * 128:(i + 1) * 128, :], X1[:], [bX1t], [bX1], bX1t)

                def stageB(i):
                    X1 = x1t[i % 2]; bX1t = b_x1t[i % 2]
                    S = st[i % 2]; bS = b_st[i % 2]
                    XN = xn[i % 2]; bXN = b_xn[i % 2]
                    PA = ptrA[i % 2]; bPA = b_ptrA[i % 2]; PB = ptrB[i % 2]; bPB = b_ptrB[i % 2]
                    k.op('act', lambda X1=X1, S=S: act.activation(out=sq[:], in_=X1[:], func=AF.Square, accum_out=S[:, 0:1]),
                         [bX1t], [b_sq, bS])
                    k.op('dve', lambda S=S: dve.tensor_scalar(out=S[:, 1:2], in0=S[:, 0:1], scalar1=1.0 / D, scalar2=EPS,
                                                              op0=ALU.mult, op1=ALU.add), [bS], [bS])
                    k.op('act', lambda S=S: act.activation(out=S[:, 1:2], in_=S[:, 1:2], func=AF.Sqrt), [bS], [bS])
                    k.op('dve', lambda S=S: dve.reciprocal(out=S[:, 1:2], in_=S[:, 1:2]), [bS], [bS])
                    k.op('dve', lambda X1=X1, S=S, XN=XN: dve.tensor_scalar(out=XN[:], in0=X1[:], scalar1=S[:, 1:2], scalar2=None, op0=ALU.mult),
                         [bX1t, bS], [bXN])

                    def tr5(XN=XN, PA=PA, PB=PB):
                        last = None
                        for j in range(8):
                            last = pe.transpose((PA if j % 2 == 0 else PB)[:, j // 2, :], XN[:, j * 128:(j + 1) * 128], idb[:])
                        return last
                    k.op('pe', tr5, [bXN, b_idb], [bPA, bPB])

                def stageB2(i):
                    PA = ptrA[i % 2]; bPA = b_ptrA[i % 2]; PB = ptrB[i % 2]; bPB = b_ptrB[i % 2]
                    for j in range(8):
                        if j % 2 == 1:
                            k.op('act', lambda j=j, PB=PB, i=i: act.activation(
                                out=hx2T[:, j, i * 128:(i + 1) * 128], in_=PB[:, j // 2, :], func=AF.Identity,
                                scale=s2[:, j:j + 1], bias=modT[:, 24 + j, 0:1]), [bPB, b_s2, b_modT], [], aw=[b_hx2T[i]])
                        else:
                            k.op('dve', lambda j=j, PA=PA, i=i: dve.tensor_scalar(
                                out=hx2T[:, j, i * 128:(i + 1) * 128], in0=PA[:, j // 2, :], scalar1=s2[:, j:j + 1],
                                scalar2=modT[:, 24 + j, 0:1], op0=ALU.mult, op1=ALU.add), [bPA, b_s2, b_modT], [], aw=[b_hx2T[i]])
                ld_yl(0); ld_x(0); ld_x(1)
                stageA(0)
                for i in range(NT + 1):
                    if i + 1 < NT:
                        stageA(i + 1)
                    if i < NT:
                        stageB(i)
                    if i >= 1:
                        stageB2(i - 1)
                k.barrier()
            with contextlib.ExitStack() as p6:
                wu = [sb(p6, "wu%d" % i, [128, 8, 2, 128], BF16) for i in range(2)]; b_wu = [k.bufs('wu', 2) for i in range(2)]
                wuf = [sb(p6, "wuf%d" % i, [128, 8, 2, 128]) for i in range(2)]; b_wuf = [k.bufs('wuf', 2) for i in range(2)]
                dg = [sb(p6, "dg%d" % i, [128, 2, 9, 128], BF16) for i in range(2)]; b_dg = k.bufs('dg', 2)
                upad = [sb(p6, "upad%d" % i, [128, 2, 66 * 66], BF16) for i in range(2)]
                b_up = [k.bufs('upad', 2) for i in range(2)]
                vs = [sb(p6, "vs%d" % i, [128, 512]) for i in range(2)]; b_vs = k.bufs('vs', 2)
                gs = [sb(p6, "gs%d" % i, [128, 512]) for i in range(2)]; b_gs = k.bufs('gs', 2)
                hst = [sb(p6, "hst%d" % i, [128, 512], BF16) for i in range(2)]; b_hst = k.bufs('hst', 2)
                pU = [ps(p6, "pU%d" % i, [128, 512]) for i in range(3)]; b_pU = k.bufs('pU', 3)
                pV = [ps(p6, "pV%d" % i, [128, 512]) for i in range(4)]; b_pV = k.bufs('pV', 4)
                for i in range(2):
                    k.op('dve', lambda i=i: dve.memset(upad[i][:], 0.0), [], b_up[i])
                wupv = wup_d.rearrange("(kk p) n -> p kk n", p=128)
                nu = 0
                nv = 0
                def ld_wu(jj):
                    for v in range(2):
                        c0 = v * 2560 + jj * 128
                        k.dma('sp', wuf[jj % 2][:, :, v, :], wupv[:, :, c0:c0 + 128], [bIN], [b_wuf[jj % 2][v]], b_wuf[jj % 2][v])
                ld_wu(0)
                for jj in range(20):
                    bi = jj % 2
                    if jj + 1 < 20:
                        ld_wu(jj + 1)
                    for v in range(2):
                        k.op('dve', lambda bi=bi, v=v: dve.tensor_copy(out=wu[bi][:, :, v, :], in_=wuf[bi][:, :, v, :]),
                             [b_wuf[bi][v]], [b_wu[bi][v]])
                    prep_wd(jj)
                    for v in range(2):
                        ch = v * 20 + jj
                        for t in range(9):
                            k.op('dve', lambda bi=bi, v=v, t=t, ch=ch: dve.tensor_scalar(
                                out=dg[bi][:, v, t, :], in0=idb[:], scalar1=vec[:, V_FCW + ch * 9 + t:V_FCW + ch * 9 + t + 1], scalar2=None,
                                op0=ALU.mult), [b_idb, b_vec], [b_dg[bi]])
                    for v in range(2):
                        ch = v * 20 + jj
                        g3 = upad[bi][:, v, :].rearrange("p (r c) -> p r c", c=66)
                        for tt in range(8):
                            P = pU[nu % 3]; bP = b_pU[nu % 3]; nu += 1

                            def mmu(P=P, bi=bi, v=v, tt=tt):
                                last = None
                                for kk in range(8):
                                    last = pe.matmul(P[:], lhsT=wu[bi][:, kk, v, :], rhs=hx2T[:, kk, tt * 512:(tt + 1) * 512],
                                                     start=(kk == 0), stop=(kk == 7))
                                return last
                            k.op('pe', mmu, [b_wu[bi][v]] + b_hx2T[tt * 4:(tt + 1) * 4], [bP])
                            k.op('act', lambda P=P, g3=g3, tt=tt, ch=ch: act.activation(
                                out=g3[:, 1 + 8 * tt:9 + 8 * tt, 1:65], in_=P[:].rearrange("p (r c) -> p r c", c=64), func=AF.Identity,
                                bias=vec[:, V_BUP + ch:V_BUP + ch + 1]), [bP, b_vec], [b_up[bi][v]])
                    for tt in range(8):
                        outs = []
                        for v in range(2):
                            ch = v * 20 + jj
                            g3 = upad[bi][:, v, :].rearrange("p (r c) -> p r c", c=66)
                            P = pV[nv % 4]; bP = b_pV[nv % 4]; nv += 1

                            def mmv(P=P, bi=bi, v=v, tt=tt, g3=g3):
                                last = None
                                for t in range(9):
                                    dr, dc_ = t // 3 - 1, t % 3 - 1
                                    last = pe.matmul(P[:].rearrange("p (r c) -> p r c", c=64), lhsT=dg[bi][:, v, t, :],
                                                     rhs=g3[:, 8 * tt + dr + 1:8 * tt + dr + 9, dc_ + 1:dc_ + 65],
                                                     start=(t == 0), stop=(t == 8))
                                return last
                            k.op('pe', mmv, [b_dg[bi], b_up[bi][v]], [bP])
                            outs.append((P, bP, ch))
                        n2 = (jj * 8 + tt) % 2
                        (Pa, bPa, cha), (Pb, bPb, chb) = outs
                        k.op('act', lambda Pa=Pa, n2=n2, cha=cha: act.activation(out=vs[n2][:], in_=Pa[:], func=AF.Identity,
                                                                              bias=vec[:, V_FCB + cha:V_FCB + cha + 1]),
                             [bPa, b_vec], [b_vs[n2]])
                        k.op('act', lambda Pb=Pb, n2=n2, chb=chb: act.activation(out=gs[n2][:], in_=Pb[:], func=AF.Silu,
                                                                              bias=vec[:, V_FCB + chb:V_FCB + chb + 1]),
                             [bPb, b_vec], [b_gs[n2]])
                        k.op('dve', lambda n2=n2: dve.tensor_tensor(out=hst[n2][:], in0=vs[n2][:], in1=gs[n2][:], op=ALU.mult),
                             [b_vs[n2], b_gs[n2]], [b_hst[n2]])
                        k.dma('pool', HT_d[jj, :, tt * 512:(tt + 1) * 512], hst[n2][:], [b_hst[n2]], [bHT], b_hst[n2])
                k.barrier()
        with contextlib.ExitStack() as p7:
            hl = [sb(p7, "hl%d" % i, [128, 20, 512], BF16) for i in range(2)]; b_hl = k.bufs('hl', 2)
            x1l = [sb(p7, "x1l%d" % i, [128, D]) for i in range(2)]; b_x1l = k.bufs('x1l', 2)
            x2 = [sb(p7, "x2_%d" % i, [128, D]) for i in range(2)]; b_x2 = k.bufs('x2', 2)
            ot = [sb(p7, "ot%d" % i, [128, D]) for i in range(2)]; b_ot = k.bufs('ot', 2)
            sq = sb(p7, "sq7", [128, D], BF16); b_sq = k.buf('sq7')
            st = [sb(p7, "st7_%d" % i, [128, 2]) for i in range(2)]; b_st = k.bufs('st7', 2)
            pD = [ps(p7, "pD%d" % i, [128, 512]) for i in range(4)]; b_pD = k.bufs('pD', 4)
            HTv = HT_d.rearrange("a p t -> p a t")
            def ld_hl(tb):
                k.dma('sp', hl[tb % 2][:], HTv[:, :, tb * 512:(tb + 1) * 512], [bHT], [b_hl[tb % 2]], b_hl[tb % 2])

            def ld_x1(i):
                k.dma('sp', x1l[i % 2][:], X1_d[i * 128:(i + 1) * 128, :], [bX1], [b_x1l[i % 2]], b_x1l[i % 2])
            ld_hl(0); ld_x1(0); ld_x1(1)
            for i in range(NT):
                if i % 4 == 0 and i // 4 + 1 < 8:
                    ld_hl(i // 4 + 1)
                Hh = hl[(i // 4) % 2]; bHh = b_hl[(i // 4) % 2]
                XL = x1l[i % 2]; bXL = b_x1l[i % 2]
                X2 = x2[i % 2]; bX2 = b_x2[i % 2]
                O = ot[i % 2]; bO = b_ot[i % 2]
                S = st[i % 2]; bS = b_st[i % 2]
                k.op('dve', lambda XL=XL: dve.tensor_tensor(out=XL[:], in0=XL[:], in1=bdg[:], op=ALU.add), [bXL, b_bdg], [bXL])
                for hf in range(2):
                    P = pD[(i * 2 + hf) % 4]; bP = b_pD[(i * 2 + hf) % 4]

                    def mmd(P=P, Hh=Hh, i=i, hf=hf):
                        last = None
                        for j in range(20):
                            last = pe.matmul(P[:], lhsT=Hh[:, j, (i % 4) * 128:(i % 4 + 1) * 128], rhs=wd[:, j, hf * 512:(hf + 1) * 512],
                                             start=(j == 0), stop=(j == 19))
                        return last
                    k.op('pe', mmd, [bHh] + b_wd, [bP])
                    k.op('dve', lambda P=P, XL=XL, X2=X2, hf=hf: dve.tensor_tensor(
                        out=X2[:, hf * 512:(hf + 1) * 512], in0=P[:], in1=XL[:, hf * 512:(hf + 1) * 512], op=ALU.add), [bP, bXL], [bX2])
                k.op('act', lambda X2=X2, S=S: act.activation(out=sq[:], in_=X2[:], func=AF.Square, accum_out=S[:, 0:1]),
                     [bX2], [b_sq, bS])
                k.op('dve', lambda S=S: dve.tensor_scalar(out=S[:, 1:2], in0=S[:, 0:1], scalar1=1.0 / D, scalar2=EPS,
                                                          op0=ALU.mult, op1=ALU.add), [bS], [bS])
                k.op('act', lambda S=S: act.activation(out=S[:, 1:2], in_=S[:, 1:2], func=AF.Sqrt), [bS], [bS])
                k.op('dve', lambda S=S: dve.reciprocal(out=S[:, 1:2], in_=S[:, 1:2]), [bS], [bS])
                k.op('dve', lambda X2=X2, S=S, O=O: dve.scalar_tensor_tensor(out=O[:], in0=X2[:], scalar=S[:, 1:2], in1=fnwb[:],
                                                                           op0=ALU.mult, op1=ALU.mult), [bX2, bS, b_fnwb], [bO])
                if i + 2 < NT:
                    ld_x1(i + 2)
                k.dma('pool', out_d[i * 128:(i + 1) * 128, :], O[:], [bO], [], bO)
            k.barrier()
        gw7.close()


def _consts():
    f32 = np.float32
    bf = ml_dtypes.bfloat16
    c = {}
    c['ident_bf'] = np.eye(128, dtype=f32).astype(bf)
    c['ident_f'] = np.eye(128, dtype=f32)
    j = np.arange(128)[:, None]
    l = np.arange(128)[None, :]
    c['masks'] = np.concatenate([(j <= l), (j >= l)], axis=1).astype(f32)
    sel = np.zeros((4, 4, 128), f32)
    for h in range(4):
        sel[h, h, :] = 1.0
    c['sel'] = sel.reshape(4, 512)
    l1 = np.arange(128, dtype=np.float64)[:, None, None]
    l2 = np.arange(32, dtype=np.float64)[None, :, None]
    k1 = np.arange(128, dtype=np.float64)[None, None, :]
    th = 2 * np.pi * (l1 * k1 / 128.0 + l2 * k1 / 4096.0)
    t1 = np.stack([np.cos(th), np.sin(th)], axis=2) / np.sqrt(128.0)
    c['dft1'] = t1.reshape(128, 32 * 256).astype(f32).astype(bf)
    a = np.arange(32, dtype=np.float64)
    ph = 2 * np.pi * np.outer(a, a) / 32.0
    cph, sph = np.cos(ph), np.sin(ph)
    w2 = np.block([[cph, sph], [-sph, cph]]) / np.sqrt(32.0)
    c['dft2'] = np.concatenate([w2, np.zeros_like(w2)], axis=0).astype(f32).astype(bf)
    b = np.arange(128, dtype=np.float64)
    ps_ = 2 * np.pi * np.outer(b, b) / 128.0
    c['dftc'] = (np.concatenate([np.cos(ps_), -np.sin(ps_)], axis=1) / np.sqrt(128.0)).astype(f32).astype(bf)
    return c


def prep_inputs(inp):
    f32 = np.float32
    g = lambda n: np.asarray(inp[n], dtype=f32)
    consts = _consts()
    pm = lambda v: np.ascontiguousarray(v.reshape(-1, 128).T)
    vec = np.zeros((128, V_END), f32)
    vec[:, V_N1W:V_N1W + 8] = pm(g('norm1_w')[0])
    vec[:, V_N2W:V_N2W + 8] = pm(g('norm2_w')[0])
    vec[:, V_BADA:V_BADA + 48] = pm(g('b_ada')[0])
    mcw = g('mconv_w')[0]
    for t in range(3):
        vec[:, V_MCW + np.arange(4) * 3 + t] = pm(mcw[t])
    vec[:, V_MCB:V_MCB + 4] = pm(g('mconv_b')[0])
    vec[:, V_MNW:V_MNW + 4] = pm(g('mnorm_w')[0])
    vec[:, V_MSK:V_MSK + 4] = pm(g('m_skip')[0])
    vec[:, V_BUP:V_BUP + 40] = pm(g('b_up')[0])
    fcw = g('fconv_w')[0].reshape(9, 5120)
    for t in range(9):
        vec[:, V_FCW + np.arange(40) * 9 + t] = pm(fcw[t])
    vec[:, V_FCB:V_FCB + 40] = pm(g('fconv_b')[0])
    vec[0:16, V_BG] = g('b_gate')[0]
    shared = {
        'vecs': vec,
        'b_ada': g('b_ada'), 'b_down': g('b_down'), 'final_norm_w': g('final_norm_w').reshape(1, D),
        'w_ada': g('w_ada')[0], 'w_in': g('w_in')[0], 'w_q': g('w_q')[0], 'w_k': g('w_k')[0],
        'w_out': g('w_out')[0], 'w_up': g('w_up')[0], 'w_down': g('w_down')[0],
    }
    shared.update(consts)
    x = g('x'); ctx = g('ctx'); c = g('c'); cctx = g('c_ctx')
    maps = []
    for b in range(8):
        cc = np.stack([pm(c[b]), pm(cctx)], axis=2).reshape(128, 16)
        m = dict(shared)
        m['x'] = x[b]
        m['ctx'] = ctx[b]
        m['cc'] = np.ascontiguousarray(cc)
        maps.append(m)
    return maps


def kernel(**inputs):
    nc = build()
    maps = prep_inputs(inputs)
    res = run_bass_kernel_spmd(nc, maps, core_ids=list(range(8)))
    return np.stack([np.asarray(r['out'], dtype=np.float32) for r in res.results], axis=0)
```

```python
import contextlib
import numpy as np
import ml_dtypes
import concourse.bass as bass
import concourse.mybir as mybir
from concourse.bass_utils import run_bass_kernel_spmd

F32 = mybir.dt.float32
BF16 = mybir.dt.bfloat16
ALU = mybir.AluOpType
AF = mybir.ActivationFunctionType
AX = mybir.AxisListType

T = 4096
D = 1024
CT = 256
NT = T // 128
EPS = 1e-6
import os
EVY_ACT = os.environ.get('EVY_ACT', '0') == '1'
SAME_ENG_SYNC = {'act': True, 'dve': True, 'pool': True, 'pe': False, 'sp': False}

V_N1W, V_N2W, V_BADA, V_MCW, V_MCB, V_MNW, V_MSK, V_BUP, V_FCW, V_FCB, V_BG, V_END = (
    0, 8, 16, 64, 76, 80, 84, 88, 128, 488, 528, 529)


def _merge(d, s):
    for k, v in s.items():
        if d.get(k, 0) < v:
            d[k] = v


class Buf:
    __slots__ = ('name', 'w', 'r', 'sem', 'semval', 'dram', 'key', 'excl')

    def __init__(self, name, dram=False):
        self.name = name
        self.w = {}
        self.r = {}
        self.sem = None
        self.semval = 0
        self.dram = dram
        self.key = None
        self.excl = name.startswith('p')


class K:
    def __init__(self, nc, es):
        self.nc = nc
        self.es = es
        self.eng = {'pe': nc.tensor, 'act': nc.scalar, 'dve': nc.vector, 'pool': nc.gpsimd, 'sp': nc.sync}
        self.sem = {}
        self.cur = {}
        self.waited = {n: {} for n in self.eng}
        for n in self.eng:
            self.sem[n] = es.enter_context(nc.semaphore('s_' + n))
            self.cur[n] = 0
        self.nbuf = 0
        self.ninst = 0

    def buf(self, name, dram=False):
        self.nbuf += 1
        return Buf('%s_%d' % (name, self.nbuf), dram)

    def bufs(self, name, n):
        return [self.buf(name) for _ in range(n)]

    def _wait(self, e, deps):
        for key, val in deps.items():
            if key == e and not SAME_ENG_SYNC[e]:
                continue
            if self.waited[e].get(key, 0) >= val:
                continue
            self.eng[e].wait_ge(self.sem[key], val)
            self.waited[e][key] = val
            self.ninst += 1

    def _deps(self, reads, writes):
        deps = {}
        for b in reads:
            _merge(deps, b.w)
            if b.excl:
                _merge(deps, b.r)
        for b in writes:
            if not b.dram:
                _merge(deps, b.w)
            _merge(deps, b.r)
        return deps

    def _book(self, key, val, reads, writes):
        for b in writes:
            if b.dram:
                if b.w.get(key, 0) < val:
                    b.w[key] = val
            else:
                b.w = {key: val}
                b.r = {}
        for b in reads:
            if b.r.get(key, 0) < val:
                b.r[key] = val

    def op(self, e, fn, reads=(), writes=(), aw=()):
        deps = self._deps(reads, writes)
        for b in aw:
            _merge(deps, b.r)
        self._wait(e, deps)
        ins = fn()
        self.cur[e] += 1
        ins.then_inc(self.sem[e], 1)
        self.ninst += 1
        self._book(e, self.cur[e], reads, writes)
        for b in aw:
            if b.w.get(e, 0) < self.cur[e]:
                b.w[e] = self.cur[e]

    def dma(self, q, out, in_, reads, writes, sb):
        self._wait(q, self._deps(reads, writes))
        if sb.sem is None:
            sb.key = 'd_' + sb.name
            sb.sem = self.es.enter_context(self.nc.semaphore(sb.key))
            self.sem[sb.key] = sb.sem
            self.cur[sb.key] = 0
        self.cur[sb.key] += 16
        self.eng[q].dma_start(out=out, in_=in_).then_inc(sb.sem, 16)
        self.ninst += 1
        self._book(sb.key, self.cur[sb.key], reads, writes)

    def barrier(self, engines=None):
        allv = dict(self.cur)
        for e in (engines or self.eng):
            self._wait(e, {k: v for k, v in allv.items() if v > 0 and k != e})


def build(dbg=None):
    nc = bass.Bass("TRN2", target_bir_lowering=False)
    es = contextlib.ExitStack()
    with es:
        _build(nc, es, dbg)
    return nc


def _build(nc, es, dbg):
    k = K(nc, es)

    def din(name, shape, dt=F32):
        return nc.dram_tensor(name, list(shape), dt, kind="ExternalInput").ap()

    x_d = din("x", [T, D])
    ctx_d = din("ctx", [CT, D])
    cc_d = din("cc", [128, 16])
    vec_d = din("vecs", [128, V_END])
    bada_d = din("b_ada", [1, 6 * D])
    bdn_d = din("b_down", [1, D])
    fnw_d = din("final_norm_w", [1, D])
    wada_d = din("w_ada", [D, 6 * D])
    win_d = din("w_in", [D, 2064])
    wq_d = din("w_q", [4, 128, 128])
    wk_d = din("w_k", [4, 128, 128])
    wout_d = din("w_out", [D, D])
    wup_d = din("w_up", [D, 5120])
    wdn_d = din("w_down", [2560, D])
    idb_d = din("ident_bf", [128, 128], BF16)
    idf_d = din("ident_f", [128, 128])
    msk_d = din("masks", [128, 256])
    sel_d = din("sel", [4, 512])
    t1_d = din("dft1", [128, 32 * 256], BF16)
    w2_d = din("dft2", [128, 64], BF16)
    cd_d = din("dftc", [128, 256], BF16)
    out_d = nc.dram_tensor("out", [T, D], F32, kind="ExternalOutput").ap()

    U_d = nc.dram_tensor("scr_u", [T, 512], BF16).ap()
    ZD_d = nc.dram_tensor("scr_z", [2, 32, 128, 512], BF16).ap()
    SZ_d = nc.dram_tensor("scr_sz", [4, 128, T], BF16).ap()
    YT_d = nc.dram_tensor("scr_yt", [8, 128, T], BF16).ap()
    X1_d = nc.dram_tensor("scr_x1", [T, D], F32).ap()
    HT_d = nc.dram_tensor("scr_ht", [20, 128, T], BF16).ap()
    bU, bZD, bSZ, bYT, bX1, bHT = [k.buf(n, True) for n in ('U', 'ZD', 'SZ', 'YT', 'X1', 'HT')]
    bIN = k.buf('inputs', True)
    GD_d = nc.dram_tensor("scr_g", [16, T + CT], F32).ap()
    bGD = k.buf('GD', True)

    dbg_out = {}

    def dbg_tensor(name, shape, dt=F32):
        t = nc.dram_tensor("dbg_" + name, list(shape), dt, kind="ExternalOutput").ap()
        dbg_out[name] = t
        return t

    def sb(ph, name, shape, dt=F32):
        return ph.enter_context(nc.sbuf_tensor("sb_" + name, list(shape), dt))

    def ps(ph, name, shape, dt=F32):
        return ph.enter_context(nc.psum_tensor("ps_" + name, list(shape), dt))

    act, dve, pool, pe = nc.scalar, nc.vector, nc.gpsimd, nc.tensor

    with contextlib.ExitStack() as g0:
        vec = sb(g0, "vec", [128, V_END]); b_vec = k.buf('vec')
        idb = sb(g0, "idb", [128, 128], BF16); b_idb = k.buf('idb')
        idf = sb(g0, "idf", [128, 128]); b_idf = k.buf('idf')
        modT = sb(g0, "modT", [128, 48, 2]); b_modT = k.buf('modT')
        s1 = sb(g0, "s1", [128, 8, 2]); b_s1 = k.buf('s1')
        s2 = sb(g0, "s2", [128, 8]); b_s2 = k.buf('s2')
        g1b = sb(g0, "g1b", [128, D]); b_g1b = k.buf('g1b')
        g2b = sb(g0, "g2b", [128, D]); b_g2b = k.buf('g2b')
        k.dma('sp', vec[:], vec_d, [bIN], [b_vec], b_vec)
        k.dma('sp', idb[:], idb_d, [bIN], [b_idb], b_idb)
        k.dma('sp', idf[:], idf_d, [bIN], [b_idf], b_idf)

        with contextlib.ExitStack() as ph:
            wada = sb(ph, "wada", [128, 8, 6 * D], BF16); b_wada = k.bufs('wada', 8)
            cc = sb(ph, "cc", [128, 16]); b_cc = k.buf('cc')
            scc = sb(ph, "scc", [128, 8, 2], BF16); b_scc = k.buf('scc')
            rep = sb(ph, "rep", [128, 8, 128], BF16); b_rep = k.buf('rep')
            badab = sb(ph, "badab", [128, 2, D]); b_badab = k.buf('badab')
            psm = ps(ph, "psm", [128, 48, 2]); b_psm = k.buf('psm')
            psg = [ps(ph, "psg%d" % i, [128, 512]) for i in range(4)]; b_psg = k.bufs('psg', 4)
            k.dma('sp', cc[:], cc_d, [bIN], [b_cc], b_cc)
            wv = wada_d.rearrange("(j p) n -> p j n", p=128)
            for j in range(8):
                k.dma('pool', wada[:, j, :], wv[:, j, :], [bIN], [b_wada[j]], b_wada[j])
            k.dma('sp', badab[:, 0, :], bada_d[:, 2 * D:3 * D].partition_broadcast(128), [bIN], [b_badab], b_badab)
            k.dma('sp', badab[:, 1, :], bada_d[:, 5 * D:6 * D].partition_broadcast(128), [bIN], [b_badab], b_badab)
            k.op('act', lambda: act.activation(out=scc[:].rearrange("p j r -> p (j r)"), in_=cc[:], func=AF.Silu),
                 [b_cc], [b_scc])
            for j in range(8):
                k.op('dve', lambda j=j: dve.tensor_copy(out=rep[:, j, :], in_=scc[:, j, 0:1].to_broadcast([128, 128])),
                     [b_scc], [b_rep])
            secs = [0, 1, 3, 4]

            def mm_mod():
                last = None
                for s in secs:
                    for jj in range(8):
                        col = s * 8 + jj
                        for kk in range(8):
                            last = pe.matmul(psm[:, col, :], lhsT=wada[:, kk, col * 128:(col + 1) * 128],
                                             rhs=scc[:, kk, :], start=(kk == 0), stop=(kk == 7))
                return last
            k.op('pe', mm_mod, b_wada + [b_scc], [b_psm])
            k.op('dve', lambda: dve.tensor_tensor(
                out=modT[:], in0=psm[:], in1=vec[:, V_BADA:V_BADA + 48].unsqueeze(2).to_broadcast([128, 48, 2]),
                op=ALU.add), [b_psm, b_vec], [b_modT])
            k.op('dve', lambda: dve.scalar_tensor_tensor(
                out=s1[:], in0=modT[:, 8:16, :], scalar=1.0,
                in1=vec[:, V_N1W:V_N1W + 8].unsqueeze(2).to_broadcast([128, 8, 2]),
                op0=ALU.add, op1=ALU.mult), [b_modT, b_vec], [b_s1])
            k.op('dve', lambda: dve.scalar_tensor_tensor(
                out=s2[:], in0=modT[:, 32:40, 0], scalar=1.0, in1=vec[:, V_N2W:V_N2W + 8],
                op0=ALU.add, op1=ALU.mult), [b_modT, b_vec], [b_s2])
            for gi, sec in enumerate((2, 5)):
                for hf in range(2):
                    pt = psg[gi * 2 + hf]
                    c0 = sec * D + hf * 512

                    def mm_g(pt=pt, c0=c0):
                        last = None
                        for kk in range(8):
                            last = pe.matmul(pt[:], lhsT=rep[:, kk, :], rhs=wada[:, kk, c0:c0 + 512],
                                             start=(kk == 0), stop=(kk == 7))
                        return last
                    k.op('pe', mm_g, b_wada + [b_rep], [b_psg[gi * 2 + hf]])
                    dst = (g1b, g2b)[gi]
                    bd = (b_g1b, b_g2b)[gi]
                    k.op('dve', lambda pt=pt, dst=dst, gi=gi, hf=hf: dve.tensor_tensor(
                        out=dst[:, hf * 512:(hf + 1) * 512], in0=pt[:], in1=badab[:, gi, hf * 512:(hf + 1) * 512],
                        op=ALU.add), [b_psg[gi * 2 + hf], b_badab], [bd])
            if dbg == 'p0':
                t = dbg_tensor('modT', [128, 96])
                k.dma('sp', t, modT[:].rearrange("p a b -> p (a b)"), [b_modT], [], b_modT)
                t = dbg_tensor('g1b', [128, D])
                k.dma('sp', t, g1b[:], [b_g1b], [], b_g1b)
                t = dbg_tensor('s1', [128, 16])
                k.dma('sp', t, s1[:].rearrange("p a b -> p (a b)"), [b_s1], [], b_s1)
            k.barrier()
        if dbg == 'p0':
            k.barrier()
            return

        with contextlib.ExitStack() as gm:
            xmT = sb(gm, "xmT", [128, 4, T + 2], BF16); b_xmT = k.bufs('xmT', 4)
            xcT = sb(gm, "xcT", [128, 4, CT + 2], BF16); b_xcT = k.bufs('xcT', 4)
            Vaug = sb(gm, "Vaug", [128, NT, 4, 130], BF16); b_V = k.bufs('V', NT)
            Vc = sb(gm, "Vc", [128, 2, 4, 130], BF16); b_Vc = k.bufs('Vc', 2)
            with contextlib.ExitStack() as ph:
                hxT = sb(ph, "hxT", [128, 8, T], BF16); b_hxT = k.bufs('hxT', NT)
                hcT = sb(ph, "hcT", [128, 8, CT], BF16); b_hcT = k.bufs('hcT', 2)
                win = sb(ph, "win", [128, 8, 2064], BF16); b_win = k.bufs('win', 8)
                wvw = win_d.rearrange("(j p) n -> p j n", p=128)
                for j in range(8):
                    k.dma('pool', win[:, j, :], wvw[:, j, :], [bIN], [b_win[j]], b_win[j])
                with contextlib.ExitStack() as p1:
                    NB = 3
                    xt = [sb(p1, "xt%d" % i, [128, D]) for i in range(NB)]; b_xt = k.bufs('xt', NB)
                    xn = [sb(p1, "xn%d" % i, [128, D], BF16) for i in range(2)]; b_xn = k.bufs('xn', 2)
                    sq = sb(p1, "sq", [128, D], BF16); b_sq = k.buf('sq')
                    st = [sb(p1, "st%d" % i, [128, 2]) for i in range(2)]; b_st = k.bufs('st', 2)
                    ptrA = [ps(p1, "ptrA%d" % i, [128, 4, 128], BF16) for i in range(2)]; b_ptrA = k.bufs('ptrA', 2)
                    ptrB = [ps(p1, "ptrB%d" % i, [128, 4, 128], BF16) for i in range(2)]; b_ptrB = k.bufs('ptrB', 2)
                    tiles = [('c', i) for i in range(2)] + [('x', i) for i in range(NT)]

                    def load(n):
                        kind, i = tiles[n]
                        src = (ctx_d if kind == 'c' else x_d)[i * 128:(i + 1) * 128, :]
                        k.dma('sp', xt[n % NB][:], src, [bIN], [b_xt[n % NB]], b_xt[n % NB])
                    load(0); load(1)

                    def p1A(n):
                        kind, i = tiles[n]
                        if n + 2 < len(tiles):
                            load(n + 2)
                        X = xt[n % NB]; bX = b_xt[n % NB]
                        S = st[n % 2]; bS = b_st[n % 2]
                        XN = xn[n % 2]; bXN = b_xn[n % 2]
                        PA = ptrA[n % 2]; bPA = b_ptrA[n % 2]; PB = ptrB[n % 2]; bPB = b_ptrB[n % 2]
                        k.op('act', lambda X=X, S=S: act.activation(out=sq[:], in_=X[:], func=AF.Square, accum_out=S[:, 0:1]),
                             [bX], [b_sq, bS])
                        k.op('dve', lambda S=S: dve.tensor_scalar(out=S[:, 1:2], in0=S[:, 0:1], scalar1=1.0 / D, scalar2=EPS,
                                                                  op0=ALU.mult, op1=ALU.add), [bS], [bS])
                        k.op('act', lambda S=S: act.activation(out=S[:, 1:2], in_=S[:, 1:2], func=AF.Sqrt), [bS], [bS])
                        k.op('dve', lambda S=S: dve.reciprocal(out=S[:, 1:2], in_=S[:, 1:2]), [bS], [bS])
                        k.op('dve', lambda X=X, S=S, XN=XN: dve.tensor_scalar(out=XN[:], in0=X[:], scalar1=S[:, 1:2], scalar2=None, op0=ALU.mult),
                             [bX, bS], [bXN])

                        def tr(XN=XN, PA=PA, PB=PB):
                            last = None
                            for j in range(8):
                                last = pe.transpose((PA if j % 2 == 0 else PB)[:, j // 2, :], XN[:, j * 128:(j + 1) * 128], idb[:])
                            return last
                        k.op('pe', tr, [bXN, b_idb], [bPA, bPB])

                    def p1B(n):
                        kind, i = tiles[n]
                        PA = ptrA[n % 2]; bPA = b_ptrA[n % 2]; PB = ptrB[n % 2]; bPB = b_ptrB[n % 2]
                        col = 1 if kind == 'c' else 0
                        dstT = hcT if kind == 'c' else hxT
                        bD = (b_hcT if kind == 'c' else b_hxT)[i]
                        for j in range(8):
                            if j % 2 == 1:
                                k.op('act', lambda j=j, PB=PB, dstT=dstT, i=i, col=col: act.activation(
                                    out=dstT[:, j, i * 128:(i + 1) * 128], in_=PB[:, j // 2, :], func=AF.Identity,
                                    scale=s1[:, j, col:col + 1], bias=modT[:, j, col:col + 1]),
                                    [bPB, b_s1, b_modT], [], aw=[bD])
                            else:
                                k.op('dve', lambda j=j, PA=PA, dstT=dstT, i=i, col=col: dve.tensor_scalar(
                                    out=dstT[:, j, i * 128:(i + 1) * 128], in0=PA[:, j // 2, :],
                                    scalar1=s1[:, j, col:col + 1], scalar2=modT[:, j, col:col + 1],
                                    op0=ALU.mult, op1=ALU.add), [bPA, b_s1, b_modT], [], aw=[bD])
                    p1A(0)
                    for n in range(len(tiles)):
                        if n + 1 < len(tiles):
                            p1A(n + 1)
                        p1B(n)
                    if dbg == 'p1':
                        t = dbg_tensor('hxT', [128, 8 * T], BF16)
                        k.dma('sp', t, hxT[:].rearrange("p a b -> p (a b)"), b_hxT, [], b_hxT[0])
                        t = dbg_tensor('hcT', [128, 8 * CT], BF16)
                        k.dma('sp', t, hcT[:].rearrange("p a b -> p (a b)"), b_hcT, [], b_hcT[0])
                    k.barrier()
                if dbg == 'p1':
                    k.barrier()
                    return
                with contextlib.ExitStack() as p2:
                    pA = [ps(p2, "pA%d" % i, [128, 512]) for i in range(4)]; b_pA = k.bufs('pA', 4)
                    ust = [sb(p2, "ust%d" % i, [128, 512], BF16) for i in range(2)]; b_ust = k.bufs('ust', 2)
                    szs = [sb(p2, "szs%d" % i, [128, 512], BF16) for i in range(2)]; b_szs = k.bufs('szs', 2)
                    gst = [sb(p2, "gst%d" % i, [16, 512]) for i in range(2)]; b_gst = k.bufs('gst', 2)
                    pi = [0]

                    def nextp():
                        pi[0] += 1
                        return pA[pi[0] % 4], b_pA[pi[0] % 4]
                    k.op('dve', lambda: dve.memset(Vaug[:, :, :, 128:129], 1.0), [], b_V)
                    k.op('dve', lambda: dve.memset(Vc[:, :, :, 128:129], 1.0), [], b_Vc)
                    k.op('dve', lambda: dve.memset(xmT[:, :, 0:1], 0.0), [], b_xmT)
                    k.op('dve', lambda: dve.memset(xmT[:, :, T + 1:T + 2], 0.0), [], b_xmT)
                    k.op('dve', lambda: dve.memset(xcT[:, :, 0:1], 0.0), [], b_xcT)
                    k.op('dve', lambda: dve.memset(xcT[:, :, CT + 1:CT + 2], 0.0), [], b_xcT)
                    for i in range(NT):
                        P, bP = nextp()

                        def mm(P=P, i=i, c0=0):
                            last = None
                            for kk in range(8):
                                last = pe.matmul(P[:], lhsT=hxT[:, kk, i * 128:(i + 1) * 128], rhs=win[:, kk, c0:c0 + 512],
                                                 start=(kk == 0), stop=(kk == 7))
                            return last
                        k.op('pe', mm, [b_hxT[i]] + b_win, [bP])
                        us = ust[i % 2]; bus = b_ust[i % 2]
                        k.op('act', lambda P=P, us=us: act.copy(out=us[:], in_=P[:]), [bP], [bus])
                        k.dma('pool', U_d[i * 128:(i + 1) * 128, :], us[:], [bus], [bU], bus)
                        P, bP = nextp()
                        k.op('pe', lambda P=P, i=i: mm(P, i, 1024), [b_hxT[i]] + b_win, [bP])
                        k.op('dve', lambda P=P, i=i: dve.tensor_copy(out=Vaug[:, i, :, 0:128],
                                                                   in_=P[:].rearrange("p (h e) -> p h e", h=4)),
                             [bP], [b_V[i]])
                    for i in range(2):
                        P, bP = nextp()

                        def mmc(P=P, i=i):
                            last = None
                            for kk in range(8):
                                last = pe.matmul(P[:], lhsT=hcT[:, kk, i * 128:(i + 1) * 128], rhs=win[:, kk, 1024:1536],
                                                 start=(kk == 0), stop=(kk == 7))
                            return last
                        k.op('pe', mmc, [b_hcT[i]] + b_win, [bP])
                        k.op('dve', lambda P=P, i=i: dve.tensor_copy(out=Vc[:, i, :, 0:128],
                                                                   in_=P[:].rearrange("p (h e) -> p h e", h=4)),
                             [bP], [b_Vc[i]])
                    for tt in range(8):
                        hsl = b_hxT[tt * 4:(tt + 1) * 4]
                        for ch in range(4):
                            for which in range(2):
                                c0 = (512 if which == 0 else 1552) + ch * 128
                                P, bP = nextp()

                                def mmf(P=P, c0=c0, tt=tt, M=128):
                                    last = None
                                    for kk in range(8):
                                        last = pe.matmul(P[0:M, :], lhsT=win[:, kk, c0:c0 + M], rhs=hxT[:, kk, tt * 512:(tt + 1) * 512],
                                                         start=(kk == 0), stop=(kk == 7))
                                    return last
                                k.op('pe', mmf, hsl + b_win, [bP])
                                if which == 0:
                                    k.op('dve', lambda P=P, ch=ch, tt=tt: dve.tensor_copy(
                                        out=xmT[:, ch, 1 + tt * 512:1 + (tt + 1) * 512], in_=P[:]), [bP], [b_xmT[ch]])
                                else:
                                    zs = szs[(tt * 4 + ch) % 2]; bzs = b_szs[(tt * 4 + ch) % 2]
                                    k.op('act', lambda P=P, zs=zs: act.activation(out=zs[:], in_=P[:], func=AF.Silu), [bP], [bzs])
                                    k.dma('pool', SZ_d[ch, :, tt * 512:(tt + 1) * 512], zs[:], [bzs], [bSZ], bzs)
                        P, bP = nextp()
                        k.op('pe', lambda P=P, tt=tt: mmf(P, 1536, tt, 16), hsl + b_win, [bP])
                        gs = gst[tt % 2]; bgs = b_gst[tt % 2]
                        k.op('act', lambda P=P, gs=gs: act.activation(out=gs[:], in_=P[0:16, :],
                                                                     func=AF.Identity, bias=vec[0:16, V_BG:V_BG + 1]),
                             [bP, b_vec], [bgs])
                        k.dma('pool', GD_d[:, tt * 512:(tt + 1) * 512], gs[:], [bgs], [bGD], bgs)
                    for ch in range(4):
                        P, bP = nextp()

                        def mmx(P=P, c0=512 + ch * 128, M=128):
                            last = None
                            for kk in range(8):
                                last = pe.matmul(P[0:M, 0:CT], lhsT=win[:, kk, c0:c0 + M], rhs=hcT[:, kk, :],
                                                 start=(kk == 0), stop=(kk == 7))
                            return last
                        k.op('pe', mmx, b_hcT + b_win, [bP])
                        k.op('dve', lambda P=P, ch=ch: dve.tensor_copy(out=xcT[:, ch, 1:1 + CT], in_=P[:, 0:CT]), [bP], [b_xcT[ch]])
                    P, bP = nextp()
                    k.op('pe', lambda P=P: mmx(P, 1536, 16), b_hcT + b_win, [bP])
                    k.op('act', lambda P=P: act.activation(out=gst[0][:, 0:CT], in_=P[0:16, 0:CT], func=AF.Identity,
                                                           bias=vec[0:16, V_BG:V_BG + 1]), [bP, b_vec], [b_gst[0]])
                    k.dma('pool', GD_d[:, T:T + CT], gst[0][:, 0:CT], [b_gst[0]], [bGD], b_gst[0])
                    if dbg == 'p2':
                        t = dbg_tensor('xmT', [128, 4 * (T + 2)], BF16)
                        k.dma('sp', t, xmT[:].rearrange("p a b -> p (a b)"), b_xmT, [], b_xmT[0])
                        t = dbg_tensor('V', [128, NT * 4 * 130], BF16)
                        k.dma('sp', t, Vaug[:].rearrange("p a b c -> p (a b c)"), b_V, [], b_V[0])
                        k.barrier()
                        t = dbg_tensor('GD', [16, T + CT])
                        k.dma('sp', t, GD_d, [bGD], [], b_gst[0])
                        t = dbg_tensor('U', [T, 512], BF16)
                        k.dma('sp', t, U_d, [bU], [], b_gst[0])
                        t = dbg_tensor('SZ', [4 * 128, T], BF16)
                        k.dma('sp', t, SZ_d.rearrange("a p t -> (a p) t"), [bSZ], [], b_gst[0])
                    k.barrier()
            if dbg == 'p2':
                k.barrier()
                return
            NCH = NT + 2
            with contextlib.ExitStack() as p4:
                def aTv(h, a, b):
                    if b <= T:
                        return xmT[:, h, 1 + a:1 + b]
                    return xcT[:, h, 1 + a - T:1 + b - T]
                b_aT = [[b_xmT[i], b_xcT[i]] for i in range(4)]
                EC = sb(p4, "EC", [128, 2, NCH, 3, 4]); b_EC = k.bufs('EC', 2)
                WO = sb(p4, "WO", [128, 2, 4, NCH]); b_WO = k.buf('WO')
                mask = sb(p4, "mask", [128, 256]); b_mask = k.buf('mask')
                wqb = sb(p4, "wqb", [128, 4, 128], BF16); b_wqb = k.buf('wqb')
                wkb = sb(p4, "wkb", [128, 4, 128], BF16); b_wkb = k.buf('wkb')
                k.dma('sp', mask[:], msk_d, [bIN], [b_mask], b_mask)
                k.dma('pool', wqb[:], wq_d.rearrange("h d e -> d h e"), [bIN], [b_wqb], b_wqb)
                k.dma('pool', wkb[:], wk_d.rearrange("h d e -> d h e"), [bIN], [b_wkb], b_wkb)
                with contextlib.ExitStack() as pa:
                    ctmp = [sb(pa, "ctmp%d" % i, [128, 1024]) for i in range(2)]; b_ctmp = k.bufs('ctmp', 2)
                    nonlocal_n = [0]
                    n = 0
                    for ch in range(4):
                        w = lambda t, ch=ch: vec[:, V_MCW + ch * 3 + t:V_MCW + ch * 3 + t + 1]

                        def ctmp_of(src, bsrc, t0, ln, ch=ch, w=w):
                            nonlocal_n[0] += 1
                            tmp = ctmp[nonlocal_n[0] % 2]; btmp = b_ctmp[nonlocal_n[0] % 2]
                            k.op('dve', lambda: dve.tensor_scalar(
                                out=tmp[:, 0:ln], in0=src[:, ch, t0:t0 + ln], scalar1=w(0), scalar2=vec[:, V_MCB + ch:V_MCB + ch + 1],
                                op0=ALU.mult, op1=ALU.add), [bsrc[ch], b_vec], [btmp])
                            for t in (1, 2):
                                k.op('dve', lambda t=t: dve.scalar_tensor_tensor(
                                    out=tmp[:, 0:ln], in0=src[:, ch, t0 + t:t0 + t + ln], scalar=w(t), in1=tmp[:, 0:ln],
                                    op0=ALU.mult, op1=ALU.add), [bsrc[ch], b_vec, btmp], [btmp])
                            return tmp, btmp

                        def cwrite(src, bsrc, t0, ln, tmp, btmp, ch=ch):
                            k.op('act', lambda: act.activation(out=src[:, ch, 1 + t0:1 + t0 + ln], in_=tmp[:, 0:ln], func=AF.Silu),
                                 [btmp], [bsrc[ch]])
                        cur = ctmp_of(xmT, b_xmT, 0, 1024)
                        for sidx in range(4):
                            nxt = ctmp_of(xmT, b_xmT, (sidx + 1) * 1024, 1024) if sidx < 3 else None
                            cwrite(xmT, b_xmT, sidx * 1024, 1024, *cur)
                            cur = nxt
                        cc_ = ctmp_of(xcT, b_xcT, 0, CT)
                        cwrite(xcT, b_xcT, 0, CT, *cc_)
                    k.barrier()
                TT = T + CT
                orders = [[32, 33] + list(range(32)), [33, 32] + list(range(31, -1, -1))]
                with contextlib.ExitStack() as pb:
                    sel = sb(pb, "sel", [4, 512]); b_sel = k.buf('sel')
                    k.dma('sp', sel[:], sel_d, [bIN], [b_sel], b_sel)
                    t_li_ = [sb(pb, "t_li%d" % i, [4, TT]) for i in range(2)]; b_li_ = k.bufs('t_li', 2)
                    t_g_ = [sb(pb, "t_g%d" % i, [4, TT]) for i in range(2)]; b_g_ = k.bufs('t_g', 2)
                    t_p_ = [sb(pb, "t_p%d" % i, [4, TT]) for i in range(2)]; b_p_ = k.bufs('t_p', 2)
                    ones = sb(pb, "ones", [4, T], BF16); b_ones = k.buf('ones')
                    sm_ = [sb(pb, "sm%d" % i, [4, 8 + 4 * NCH]) for i in range(2)]; b_sm_ = k.bufs('sm', 2)
                    pEC = [ps(pb, "pEC%d" % i, [128, NCH, 3, 4]) for i in range(2)]; b_pEC = k.bufs('pEC', 2)
                    pWO = ps(pb, "pWO", [128, 2, 4, NCH]); b_pWO = k.buf('pWO')
                    k.op('dve', lambda: dve.memset(ones[:], 1.0), [], [b_ones])
                    segs = [(0, T), (T, TT)]
                    v3 = lambda tl: tl[:].rearrange("p (c j) -> p c j", j=128)
                    bc3 = lambda ap: ap.unsqueeze(2).to_broadcast([4, NCH, 128])
                    D2 = range(2)
                    for d in D2:
                        k.dma('sp', t_li_[d][:], GD_d[d * 8:d * 8 + 4, :], [bGD], [b_li_[d]], b_li_[d])
                        k.dma('sp', t_g_[d][:], GD_d[d * 8 + 4:d * 8 + 8, :], [bGD], [b_g_[d]], b_g_[d])
                    for d in D2:
                        t_g = t_g_[d]; b_g = b_g_[d]
                        k.op('act', lambda t_g=t_g: act.activation(out=t_g[:], in_=t_g[:], func=AF.Exp, scale=-1.0), [b_g], [b_g])
                        k.op('act', lambda t_g=t_g: act.activation(out=t_g[:], in_=t_g[:], func=AF.Ln, bias=1.0), [b_g], [b_g])
                    for si, (a0, a1) in enumerate(segs):
                        for d in D2:
                            t_g = t_g_[d]; b_g = b_g_[d]; t_p = t_p_[d]; b_p = b_p_[d]; sm = sm_[d]; b_sm = b_sm_[d]
                            k.op('dve', lambda a0=a0, a1=a1, t_p=t_p, t_g=t_g: dve.tensor_tensor_scan(
                                out=t_p[:, a0:a1], data0=ones[:, 0:a1 - a0], data1=t_g[:, a0:a1], initial=0.0,
                                op0=ALU.mult, op1=ALU.add), [b_ones, b_g], [b_p])
                            k.op('dve', lambda a1=a1, si=si, sm=sm, t_p=t_p: dve.tensor_copy(out=sm[:, si:si + 1], in_=t_p[:, a1 - 1:a1]),
                                 [b_p], [b_sm])
                            if d == 1:
                                k.op('dve', lambda a0=a0, a1=a1, si=si, sm=sm, t_p=t_p, t_g=t_g: dve.scalar_tensor_tensor(
                                    out=t_p[:, a0:a1], in0=t_g[:, a0:a1], scalar=sm[:, si:si + 1], in1=t_p[:, a0:a1],
                                    op0=ALU.add, op1=ALU.subtract), [b_g, b_sm, b_p], [b_p])
                    for d in D2:
                        t_li = t_li_[d]; b_li = b_li_[d]; t_p = t_p_[d]; b_p = b_p_[d]; sm = sm_[d]; b_sm = b_sm_[d]
                        cm = sm[:, 8:8 + NCH]
                        k.op('dve', lambda t_li=t_li, t_p=t_p: dve.tensor_tensor(out=t_li[:], in0=t_li[:], in1=t_p[:], op=ALU.add),
                             [b_li, b_p], [b_li])
                        k.op('dve', lambda cm=cm, t_li=t_li: dve.tensor_reduce(out=cm, in_=v3(t_li), axis=AX.X, op=ALU.max), [b_li], [b_sm])
                    prevs = [None, None]
                    for s_ in range(NCH):
                        for d in D2:
                            sm = sm_[d]; b_sm = b_sm_[d]
                            cm = sm[:, 8:8 + NCH]; MS = sm[:, 8 + NCH:8 + 2 * NCH]; ME = sm[:, 8 + 2 * NCH:8 + 3 * NCH]
                            c = orders[d][s_]; prev = prevs[d]
                            if s_ == 0:
                                k.op('dve', lambda c=c, MS=MS: dve.memset(MS[:, c:c + 1], 0.0), [], [b_sm])
                            elif s_ == 2:
                                k.op('dve', lambda c=c, prev=prev, MS=MS, ME=ME, sm=sm: dve.tensor_tensor(
                                    out=MS[:, c:c + 1], in0=ME[:, prev:prev + 1], in1=sm[:, 1:2], op=ALU.subtract), [b_sm], [b_sm])
                            else:
                                k.op('dve', lambda c=c, prev=prev, MS=MS, ME=ME: dve.tensor_copy(out=MS[:, c:c + 1], in_=ME[:, prev:prev + 1]),
                                     [b_sm], [b_sm])
                            k.op('dve', lambda c=c, MS=MS, ME=ME, cm=cm: dve.tensor_tensor(
                                out=ME[:, c:c + 1], in0=MS[:, c:c + 1], in1=cm[:, c:c + 1], op=ALU.max), [b_sm], [b_sm])
                            prevs[d] = c
                    for d in D2:
                        t_li = t_li_[d]; b_li = b_li_[d]; t_g = t_g_[d]; b_g = b_g_[d]; t_p = t_p_[d]; b_p = b_p_[d]
                        sm = sm_[d]; b_sm = b_sm_[d]
                        MS = sm[:, 8 + NCH:8 + 2 * NCH]; ME = sm[:, 8 + 2 * NCH:8 + 3 * NCH]
                        k.op('dve', lambda t_p=t_p, MS=MS: dve.tensor_tensor(out=v3(t_p), in0=v3(t_p), in1=bc3(MS), op=ALU.subtract),
                             [b_p, b_sm], [b_p])
                        k.op('act', lambda t_p=t_p: act.activation(out=t_p[:], in_=t_p[:], func=AF.Exp), [b_p], [b_p])
                        k.op('dve', lambda t_g=t_g, t_li=t_li, MS=MS: dve.tensor_tensor(out=v3(t_g), in0=v3(t_li), in1=bc3(MS), op=ALU.subtract),
                             [b_li, b_sm], [b_g])
                        k.op('act', lambda t_g=t_g: act.activation(out=t_g[:], in_=t_g[:], func=AF.Exp), [b_g], [b_g])
                    for d in D2:
                        t_li = t_li_[d]; b_li = b_li_[d]; sm = sm_[d]; b_sm = b_sm_[d]
                        MS = sm[:, 8 + NCH:8 + 2 * NCH]; ME = sm[:, 8 + 2 * NCH:8 + 3 * NCH]; WOL = sm[:, 8 + 3 * NCH:8 + 4 * NCH]
                        k.op('dve', lambda t_li=t_li, ME=ME: dve.tensor_tensor(out=v3(t_li), in0=v3(t_li), in1=bc3(ME), op=ALU.subtract),
                             [b_li, b_sm], [b_li])
                        k.op('act', lambda t_li=t_li: act.activation(out=t_li[:], in_=t_li[:], func=AF.Exp), [b_li], [b_li])
                        k.op('dve', lambda WOL=WOL, MS=MS, ME=ME: dve.tensor_tensor(out=WOL, in0=MS, in1=ME, op=ALU.subtract), [b_sm], [b_sm])
                        k.op('act', lambda WOL=WOL: act.activation(out=WOL, in_=WOL, func=AF.Exp), [b_sm], [b_sm])
                    for d in D2:
                        t_li = t_li_[d]; b_li = b_li_[d]; t_g = t_g_[d]; b_g = b_g_[d]; t_p = t_p_[d]; b_p = b_p_[d]
                        sm = sm_[d]; b_sm = b_sm_[d]; WOL = sm[:, 8 + 3 * NCH:8 + 4 * NCH]

                        def trE(d=d, t_g=t_g, t_li=t_li, t_p=t_p):
                            last = None
                            for c in range(NCH):
                                for q_, tl in enumerate((t_g, t_li, t_p)):
                                    last = pe.transpose(pEC[d][:, c, q_, :], tl[:, c * 128:(c + 1) * 128], idf[0:4, 0:4])
                            return last
                        k.op('pe', trE, [b_g, b_li, b_p, b_idf], [b_pEC[d]])
                        k.op('dve', lambda d=d: dve.tensor_copy(out=EC[:, d], in_=pEC[d][:]), [b_pEC[d]], [b_EC[d]])

                        def mmW(d=d, WOL=WOL):
                            last = None
                            for h in range(4):
                                last = pe.matmul(pWO[:, d, h, :], lhsT=sel[:, h * 128:(h + 1) * 128], rhs=WOL, start=True, stop=True)
                            return last
                        k.op('pe', mmW, [b_sel, b_sm], [b_pWO])
                        k.op('dve', lambda d=d: dve.tensor_copy(out=WO[:, d], in_=pWO[:, d]), [b_pWO], [b_WO])
                    k.barrier()
                if dbg == 'p4b':
                    t = dbg_tensor('EC', [128, 2 * NCH * 12])
                    k.dma('sp', t, EC[:].rearrange("p a b c d -> p (a b c d)"), b_EC, [], b_EC[0])
                    t = dbg_tensor('WO', [128, 8 * NCH])
                    k.dma('sp', t, WO[:].rearrange("p a b c -> p (a b c)"), [b_WO], [], b_WO)
                    k.barrier()
                    return
                with contextlib.ExitStack() as pc:
                    qT = sb(pc, "qT", [128, T], BF16); b_qT = k.bufs('qT', 8)
                    kT = sb(pc, "kT", [128, T], BF16); b_kT = k.bufs('kT', 8)
                    ktm = sb(pc, "ktm", [128, NCH, 128], BF16); b_ktm = k.bufs('ktm', NCH)
                    Hacc = sb(pc, "Hacc", [128, NT, 128]); b_H = k.bufs('Hacc', NT)
                    Hraw = sb(pc, "Hraw", [128, 2, NT, 130]); b_Hraw = [k.bufs('Hraw', NT) for i in range(2)]
                    rcb = sb(pc, "rcb", [128, 2, 3, NT]); b_rcb = k.bufs('rcb', 2)
                    Cf = [sb(pc, "Cf%d" % i, [128, 129]) for i in range(2)]; b_Cf = k.bufs('Cf', 2)
                    Cball = sb(pc, "Cball", [128, 2, NCH, 130], BF16); b_Cball = [k.bufs('Cball', NCH) for i in range(2)]
                    sTall = sb(pc, "sTall", [128, 2, NT, 128], BF16); b_sTall = [k.bufs('sTall', NT) for i in range(2)]
                    kw = [[sb(pc, "kw%d_%d" % (i, j), [128, 128], BF16) for j in range(3)] for i in range(2)]
                    b_kw = [k.bufs('kw', 3) for i in range(2)]
                    pP = [ps(pc, "pP%d" % i, [128, 512]) for i in range(2)]; b_pP = k.bufs('pP', 2)
                    pS = [ps(pc, "pS%d" % i, [128, 128]) for i in range(2)]; b_pS = k.bufs('pS', 2)
                    pH = [ps(pc, "pH%d" % i, [128, 129]) for i in range(2)]; b_pH = k.bufs('pH', 2)
                    pC = [ps(pc, "pC%d" % i, [128, 129]) for i in range(2)]; b_pC = k.bufs('pC', 2)
                    pT = [pP[i][:].bitcast(BF16)[:, 0:512].rearrange("p (a b) -> p a b", b=128) for i in range(2)]; b_pT = b_pP
                    t1 = [sb(pc, "t1_%d" % i, [128, 512]) for i in range(2)]; b_t1 = k.bufs('t1', 2)
                    szl = [sb(pc, "szl%d" % i, [128, 512], BF16) for i in range(2)]; b_szl = k.bufs('szl', 2)
                    ysb = [sb(pc, "ysb%d" % i, [128, 512], BF16) for i in range(2)]; b_ysb = k.bufs('ysb', 2)
                    ppi = [0]
                    KS = 128 ** -0.5
                    for h in ([1] if dbg == 'p4only1' else range(4)):
                        for tt in range(8):
                            for which in range(2):
                                ppi[0] += 1
                                P = pP[ppi[0] % 2]; bP = b_pP[ppi[0] % 2]
                                wb = wqb if which == 0 else wkb
                                bwb = b_wqb if which == 0 else b_wkb
                                k.op('pe', lambda P=P, wb=wb, tt=tt, h=h: pe.matmul(
                                    P[:], lhsT=wb[:, h, :], rhs=aTv(h, tt * 512, (tt + 1) * 512), start=True, stop=True),
                                    [bwb] + b_aT[h], [bP])
                                if which == 0:
                                    k.op('act', lambda P=P, tt=tt: act.copy(out=qT[:, tt * 512:(tt + 1) * 512], in_=P[:]), [bP], [b_qT[tt]])
                                else:
                                    k.op('act', lambda P=P, tt=tt: act.mul(out=kT[:, tt * 512:(tt + 1) * 512], in_=P[:], mul=KS), [bP], [b_kT[tt]])
                        for c0 in range(0, NCH, 4):
                            ppi[0] += 1
                            P = pP[ppi[0] % 2]; bP = b_pP[ppi[0] % 2]
                            nn = min(4, NCH - c0)

                            def mmk(P=P, c0=c0, nn=nn, h=h):
                                last = None
                                for i in range(nn):
                                    c = c0 + i
                                    last = pe.matmul(P[:, i * 128:(i + 1) * 128], lhsT=aTv(h, c * 128, (c + 1) * 128), rhs=wkb[:, h, :],
                                                     start=True, stop=True)
                                return last
                            k.op('pe', mmk, [b_wkb] + b_aT[h], [bP])
                            k.op('dve', lambda P=P, c0=c0, nn=nn: dve.tensor_scalar(
                                out=ktm[:, c0:c0 + nn, :], in0=P[:, 0:nn * 128].rearrange("p (a b) -> p a b", b=128),
                                scalar1=KS, scalar2=None, op0=ALU.mult), [bP], b_ktm[c0:c0 + nn])
                        nS = 0
                        nC = 0
                        for s_ in range(NCH):
                            for d in range(2):
                                c = orders[d][s_]
                                isctx = c >= NT
                                Vt = Vc[:, c - NT, h, 0:129] if isctx else Vaug[:, c, h, 0:129]
                                bV = b_Vc[c - NT] if isctx else b_V[c]
                                if not isctx:
                                    tq = c // 4
                                    S_ = pS[nS % 2]; bS_ = b_pS[nS % 2]; nS += 1
                                    k.op('pe', lambda S_=S_, c=c: pe.matmul(
                                        S_[:], lhsT=kT[:, c * 128:(c + 1) * 128], rhs=qT[:, c * 128:(c + 1) * 128], start=True, stop=True),
                                        [b_kT[tq], b_qT[tq]], [bS_])
                                    k.op('dve', lambda S_=S_, d=d, c=c, h=h: dve.scalar_tensor_tensor(
                                        out=sTall[:, d, c, :], in0=S_[:], scalar=EC[:, d, c, 0, h:h + 1], in1=mask[:, d * 128:(d + 1) * 128],
                                        op0=ALU.mult, op1=ALU.mult), [bS_, b_EC[d], b_mask], [b_sTall[d][c]])
                                kwt = kw[d][s_ % 3]; bkw = b_kw[d][s_ % 3]
                                k.op('act', lambda kwt=kwt, c=c, d=d, h=h: act.activation(
                                    out=kwt[:], in_=ktm[:, c, :], func=AF.Copy, scale=EC[:, d, c, 1, h:h + 1]),
                                    [b_ktm[c], b_EC[d]], [bkw])
                                C_ = pC[nC % 2]; bC_ = b_pC[nC % 2]; nC += 1
                                k.op('pe', lambda C_=C_, kwt=kwt, Vt=Vt: pe.matmul(C_[:], lhsT=kwt[:], rhs=Vt, start=True, stop=True),
                                     [bkw, bV], [bC_])
                                if s_ == 0:
                                    k.op('dve', lambda C_=C_, d=d: dve.tensor_copy(out=Cf[d][:], in_=C_[:]), [bC_], [b_Cf[d]])
                                else:
                                    k.op('dve', lambda C_=C_, d=d, h=h, c=c: dve.scalar_tensor_tensor(
                                        out=Cf[d][:], in0=Cf[d][:], scalar=WO[:, d, h, c:c + 1], in1=C_[:],
                                        op0=ALU.mult, op1=ALU.add), [bC_, b_Cf[d], b_WO], [b_Cf[d]])
                                if s_ < NCH - 1:
                                    k.op('pool', lambda d=d, s_=s_: pool.tensor_copy(out=Cball[:, d, s_, 0:129], in_=Cf[d][:]),
                                         [b_Cf[d]], [b_Cball[d][s_]])
                        for s_ in range(2, NCH):
                            for d in range(2):
                                c = orders[d][s_]
                                tq = c // 4
                                H_ = pH[d]; bH_ = b_pH[d]

                                def mmh(H_=H_, c=c, d=d, s_=s_, h=h):
                                    pe.matmul(H_[:], lhsT=qT[:, c * 128:(c + 1) * 128], rhs=Cball[:, d, s_ - 1, 0:129], start=True, stop=False)
                                    return pe.matmul(H_[:], lhsT=sTall[:, d, c, :], rhs=Vaug[:, c, h, 0:129], start=False, stop=True)
                                k.op('pe', mmh, [b_qT[tq], b_Cball[d][s_ - 1], b_sTall[d][c], b_V[c]], [bH_])
                                if d == 0:
                                    k.op('act', lambda H_=H_, c=c: act.copy(out=Hraw[:, 0, c, 0:129], in_=H_[:]), [bH_], [b_Hraw[0][c]])
                                else:
                                    k.op('dve', lambda H_=H_, c=c: dve.tensor_copy(out=Hraw[:, 1, c, 0:129], in_=H_[:]), [bH_], [b_Hraw[1][c]])
                        for d in range(2):
                            den = Hraw[:, d, :, 128]
                            e3 = EC[:, d, 0:NT, 2, h]
                            neg = rcb[:, d, 0, :]; cl = rcb[:, d, 1, :]; rc = rcb[:, d, 2, :]
                            k.op('dve', lambda den=den, neg=neg: dve.tensor_scalar(out=neg, in0=den, scalar1=-1.0, scalar2=None, op0=ALU.mult),
                                 b_Hraw[d], [b_rcb[d]])
                            k.op('dve', lambda den=den, neg=neg, cl=cl: dve.tensor_tensor(out=cl, in0=den, in1=neg, op=ALU.max),
                                 b_Hraw[d] + [b_rcb[d]], [b_rcb[d]])
                            k.op('dve', lambda e3=e3, cl=cl: dve.tensor_tensor(out=cl, in0=cl, in1=e3, op=ALU.max), [b_EC[d], b_rcb[d]], [b_rcb[d]])
                            k.op('dve', lambda cl=cl, rc=rc: dve.reciprocal(out=rc, in_=cl), [b_rcb[d]], [b_rcb[d]])
                        bcr = lambda ap: ap.unsqueeze(2).to_broadcast([128, NT, 128])
                        k.op('dve', lambda: dve.tensor_tensor(out=Hacc[:], in0=Hraw[:, 0, :, 0:128], in1=bcr(rcb[:, 0, 2, :]), op=ALU.mult),
                             b_Hraw[0] + [b_rcb[0]], b_H)
                        k.op('dve', lambda: dve.tensor_tensor(out=Hraw[:, 1, :, 0:128], in0=Hraw[:, 1, :, 0:128], in1=bcr(rcb[:, 1, 2, :]),
                                                              op=ALU.mult), b_Hraw[1] + [b_rcb[1]], b_Hraw[1])
                        k.op('dve', lambda: dve.tensor_tensor(out=Hacc[:], in0=Hacc[:], in1=Hraw[:, 1, :, 0:128], op=ALU.add),
                             b_H + b_Hraw[1], b_H)
                        if dbg == 'p4s1' and h == 1:
                            k.barrier()
                            return
                        if dbg == 'p4c' and h == 0:
                            t = dbg_tensor('Hacc', [128, NT * 128])
                            k.dma('sp', t, Hacc[:].rearrange("p a b -> p (a b)"), b_H, [], b_H[0])
                            t = dbg_tensor('Cf', [128, 129])
                            k.dma('sp', t, Cf[0][:], [b_Cf[0]], [], b_Cf[0])
                            k.barrier()
                            return
                        HC = Hraw[:, 0, :, 0:128]
                        SQ = Hraw[:, 1, :, 0:128]
                        HB = sTall[:, 0]
                        mean = rcb[:, 0, 0, :]; rstd = rcb[:, 0, 1, :]
                        k.op('dve', lambda: dve.tensor_reduce(out=mean, in_=Hacc[:], axis=AX.X, op=ALU.add), b_H, [b_rcb[0]])
                        k.op('dve', lambda: dve.tensor_scalar(out=mean, in0=mean, scalar1=1.0 / 128, scalar2=None, op0=ALU.mult),
                             [b_rcb[0]], [b_rcb[0]])
                        k.op('dve', lambda: dve.tensor_tensor(out=HC, in0=Hacc[:], in1=bcr(mean), op=ALU.subtract),
                             b_H + [b_rcb[0]], b_Hraw[0])
                        k.op('act', lambda: act.activation(out=SQ, in_=HC, func=AF.Square), b_Hraw[0], b_Hraw[1])
                        k.op('dve', lambda: dve.tensor_reduce(out=rstd, in_=SQ, axis=AX.X, op=ALU.add), b_Hraw[1], [b_rcb[0]])
                        k.op('dve', lambda: dve.tensor_scalar(out=rstd, in0=rstd, scalar1=1.0 / 128, scalar2=EPS, op0=ALU.mult, op1=ALU.add),
                             [b_rcb[0]], [b_rcb[0]])
                        k.op('act', lambda: act.activation(out=rstd, in_=rstd, func=AF.Sqrt), [b_rcb[0]], [b_rcb[0]])
                        k.op('dve', lambda: dve.reciprocal(out=rstd, in_=rstd), [b_rcb[0]], [b_rcb[0]])
                        k.op('dve', lambda: dve.tensor_tensor(out=HB, in0=HC, in1=bcr(rstd), op=ALU.mult),
                             b_Hraw[0] + [b_rcb[0]], b_sTall[0])

                        def ho_tr(tb):
                            pt = pT[tb % 2]; bpt = b_pT[tb % 2]

                            def trh(pt=pt, tb=tb):
                                last = None
                                for i in range(4):
                                    last = pe.transpose(pt[:, i, :], HB[:, tb * 4 + i, :], idb[:])
                                return last
                            k.op('pe', trh, b_sTall[0][tb * 4:(tb + 1) * 4] + [b_idb], [bpt])
                            zl = szl[tb % 2]; bzl = b_szl[tb % 2]
                            k.dma('sp', zl[:], SZ_d[h, :, tb * 512:(tb + 1) * 512], [bSZ], [bzl], bzl)

                        def ho_out(tb):
                            pt = pT[tb % 2]; bpt = b_pT[tb % 2]
                            tl = t1[tb % 2]; btl = b_t1[tb % 2]
                            zl = szl[tb % 2]; bzl = b_szl[tb % 2]
                            yb = ysb[tb % 2]; byb = b_ysb[tb % 2]
                            k.op('act', lambda tl=tl, tb=tb, h=h: act.activation(
                                out=tl[:], in_=aTv(h, tb * 512, (tb + 1) * 512), func=AF.Copy, scale=vec[:, V_MSK + h:V_MSK + h + 1]),
                                b_aT[h] + [b_vec], [btl])
                            k.op('dve', lambda tl=tl, pt=pt, h=h: dve.scalar_tensor_tensor(
                                out=tl[:], in0=pt.rearrange("p a b -> p (a b)"), scalar=vec[:, V_MNW + h:V_MNW + h + 1], in1=tl[:],
                                op0=ALU.mult, op1=ALU.add), [bpt, btl, b_vec], [btl])
                            k.op('pool', lambda tl=tl, zl=zl, yb=yb: pool.tensor_tensor(out=yb[:], in0=tl[:], in1=zl[:], op=ALU.mult),
                                 [btl, bzl], [byb])
                            k.dma('pool', YT_d[4 + h, :, tb * 512:(tb + 1) * 512], yb[:], [byb], [bYT], byb)
                        ho_tr(0)
                        for tb in range(8):
                            if tb + 1 < 8:
                                ho_tr(tb + 1)
                            ho_out(tb)
                        if dbg in ('p4h0', 'p4only1') or (dbg == 'p4h1' and h == 1):
                            k.barrier()
                            return
                    k.barrier()
            k.barrier()
        if dbg == 'p4':
            with contextlib.ExitStack() as pd:
                tmpd = sb(pd, "tmpd", [128, 4, T], BF16); b_tmpd = k.buf('tmpd')
                k.dma('sp', tmpd[:], YT_d[4:8].rearrange("a p t -> p a t"), [bYT], [b_tmpd], b_tmpd)
                t = dbg_tensor('ym', [128, 4 * T], BF16)
                k.dma('sp', t, tmpd[:].rearrange("p a t -> p (a t)"), [b_tmpd], [], b_tmpd)
                k.barrier()
            return
        with contextlib.ExitStack() as f1:
            T1 = sb(f1, "T1", [128, 32, 2, 128], BF16); b_T1 = k.buf('T1')
            Ul = sb(f1, "Ul", [128, 32, 512], BF16); b_Ul = k.bufs('Ul', 4)
            zs = [sb(f1, "zs%d" % i, [128, 4, 2, 512], BF16) for i in range(2)]
            b_zs = [[[k.buf('zs') for _ in range(2)] for _ in range(4)] for _ in range(2)]
            pZ = [ps(f1, "pZ%d" % i, [128, 512]) for i in range(4)]; b_pZ = k.bufs('pZ', 4)
            k.dma('sp', T1[:].rearrange("p a b c -> p (a b c)"), t1_d, [bIN], [b_T1], b_T1)
            Uv = U_d.rearrange("(a b) c -> a b c", b=32)
            for q in range(4):
                k.dma('sp', Ul[:, q * 8:(q + 1) * 8, :], Uv[:, q * 8:(q + 1) * 8, :], [bU], [b_Ul[q]], b_Ul[q])
            n = 0
            for l2 in range(32):
                grp, i = l2 // 4, l2 % 4
                Z = zs[grp % 2]
                for ri in range(2):
                    P = pZ[n % 4]; bP = b_pZ[n % 4]
                    k.op('pe', lambda P=P, l2=l2, ri=ri: pe.matmul(P[:], lhsT=T1[:, l2, ri, :], rhs=Ul[:, l2, :], start=True, stop=True),
                         [b_T1, b_Ul[l2 // 8]], [bP])
                    bz = b_zs[grp % 2][i][ri]
                    if n % 2 == 0:
                        k.op('act', lambda P=P, Z=Z, i=i, ri=ri: act.copy(out=Z[:, i, ri, :], in_=P[:]), [bP], [bz])
                    else:
                        k.op('dve', lambda P=P, Z=Z, i=i, ri=ri: dve.tensor_copy(out=Z[:, i, ri, :], in_=P[:]), [bP], [bz])
                    n += 1
                if i == 3:
                    for ri in range(2):
                        rb = [b_zs[grp % 2][ii][ri] for ii in range(4)]
                        k.dma('pool', ZD_d[ri, l2 - 3:l2 + 1].rearrange("l k c -> k l c"), Z[:, :, ri, :], rb, [bZD], rb[0])
            k.barrier()
        if dbg == 'p3a':
            k.barrier()
            return
        with contextlib.ExitStack() as f2:
            W2 = sb(f2, "W2", [128, 64], BF16); b_W2 = k.buf('W2')
            cd = sb(f2, "cd", [128, 256], BF16); b_cd = k.buf('cd')
            Yt = sb(f2, "Yt", [128, 4, 2, T], BF16)
            b_Yt = [[k.buf('Yt') for _ in range(32)] for _ in range(4)]
            Zt = [sb(f2, "Zt%d" % i, [128, 16, 512], BF16) for i in range(2)]; b_Zt = k.bufs('Zt', 2)
            for i in range(2):
                k.op('dve', lambda i=i: dve.memset(Zt[i][:], 0.0), [], [b_Zt[i]])
            pY = [ps(f2, "pY%d" % i, [128, 8, 64]) for i in range(4)]; b_pY = k.bufs('pY', 4)
            pF = [ps(f2, "pF%d" % i, [128, 512]) for i in range(4)]; b_pF = k.bufs('pF', 4)
            yst = [sb(f2, "yst%d" % i, [128, 512], BF16) for i in range(4)]; b_yst = k.bufs('yst', 4)
            k.dma('sp', W2[:], w2_d, [bIN], [b_W2], b_W2)
            k.dma('sp', cd[:], cd_d, [bIN], [b_cd], b_cd)
            ZDv = ZD_d.rearrange("r l k c -> (r l) k c")
            n = 0
            for slab in range(8):
                Zs = Zt[slab % 2]; bZs = b_Zt[slab % 2]
                k.dma('sp', Zs[0:64], ZDv[:, slab * 16:(slab + 1) * 16, :], [bZD], [bZs], bZs)
                if dbg == 'p3b0':
                    k.barrier()
                    return
                for g in range(4):
                    for half in range(2):
                        P = pY[n % 4]; bP = b_pY[n % 4]

                        def mmy(P=P, Zs=Zs, g=g, half=half):
                            last = None
                            for i in range(8):
                                last = pe.matmul(P[:, i, :], lhsT=Zs[:, half * 8 + i, g * 128:(g + 1) * 128], rhs=W2[:], start=True, stop=True)
                            return last
                        k.op('pe', mmy, [bZs, b_W2], [bP])
                        k1_0 = slab * 16 + half * 8
                        for ri in range(2):
                            bo = b_Yt[g][(slab * 2 + half) * 2 + ri]
                            ov = Yt[:, g, ri, :].rearrange("p (k2 k1) -> p k2 k1", k1=128)[:, :, k1_0:k1_0 + 8]
                            iv = P[:, :, ri * 32:(ri + 1) * 32].rearrange("p i k2 -> p k2 i")
                            if n % 2 == 0:
                                k.op('act', lambda ov=ov, iv=iv: act.copy(out=ov, in_=iv), [bP], [bo])
                            else:
                                k.op('dve', lambda ov=ov, iv=iv: dve.tensor_copy(out=ov, in_=iv), [bP], [bo])
                        n += 1
                if dbg in ('p3b1', 'p3b2'):
                    k.barrier()
                    return
            if dbg == 'p3b':
                k.barrier()
                return
            n = 0
            for g in range(4):
                for tt in range(8):
                    P = pF[n % 4]; bP = b_pF[n % 4]

                    def mmc2(P=P, g=g, tt=tt):
                        pe.matmul(P[:], lhsT=cd[:, 0:128], rhs=Yt[:, g, 0, tt * 512:(tt + 1) * 512], start=True, stop=False)
                        return pe.matmul(P[:], lhsT=cd[:, 128:256], rhs=Yt[:, g, 1, tt * 512:(tt + 1) * 512], start=False, stop=True)
                    k.op('pe', mmc2, [b_cd] + b_Yt[g], [bP])
                    ys = yst[n % 4]; bys = b_yst[n % 4]
                    if n % 2 == 0:
                        k.op('act', lambda P=P, ys=ys: act.copy(out=ys[:], in_=P[:]), [bP], [bys])
                    else:
                        k.op('dve', lambda P=P, ys=ys: dve.tensor_copy(out=ys[:], in_=P[:]), [bP], [bys])
                    k.dma('pool', YT_d[g, :, tt * 512:(tt + 1) * 512], ys[:], [bys], [bYT], bys)
                    n += 1
            k.barrier()
        if dbg == 'p3':
            with contextlib.ExitStack() as pd:
                tmpd = sb(pd, "tmpd", [128, 8, T], BF16); b_tmpd = k.buf('tmpd')
                k.dma('sp', tmpd[:], YT_d.rearrange("a p t -> p a t"), [bYT], [b_tmpd], b_tmpd)
                t = dbg_tensor('yt', [128, 8 * T], BF16)
                k.dma('sp', t, tmpd[:].rearrange("p a t -> p (a t)"), [b_tmpd], [], b_tmpd)
                k.barrier()
            return
        gw7 = contextlib.ExitStack()
        wd = sb(gw7, "wd", [128, 20, D], BF16); b_wd = k.bufs('wd', 20)
        wdf = [sb(gw7, "wdf%d" % i, [128, D]) for i in range(1)] * 2; b_wdf = k.bufs('wdf', 1) * 2
        bdg = sb(gw7, "bdg", [128, D]); b_bdg = k.buf('bdg')
        fnwb = sb(gw7, "fnwb", [128, D]); b_fnwb = k.buf('fnwb')

        def prep_wd(j):
            if j == 0:
                k.dma('sp', bdg[:], bdn_d.partition_broadcast(128), [bIN], [b_bdg], b_bdg)
                k.dma('sp', fnwb[:], fnw_d.partition_broadcast(128), [bIN], [b_fnwb], b_fnwb)
                k.op('dve', lambda: dve.tensor_tensor(out=bdg[:], in0=bdg[:], in1=g2b[:], op=ALU.mult), [b_bdg, b_g2b], [b_bdg])
            k.dma('sp', wdf[j % 2][:], wdn_d[j * 128:(j + 1) * 128, :], [bIN], [b_wdf[j % 2]], b_wdf[j % 2])
            k.op('dve', lambda j=j: dve.tensor_tensor(out=wd[:, j, :], in0=wdf[j % 2][:], in1=g2b[:], op=ALU.mult),
                 [b_wdf[j % 2], b_g2b], [b_wd[j]])
        with contextlib.ExitStack() as gf:
            hx2T = sb(gf, "hx2T", [128, 8, T], BF16); b_hx2T = k.bufs('hx2T', NT)
            with contextlib.ExitStack() as p5:
                wo = sb(p5, "wo", [128, 8, D], BF16); b_wo = k.bufs('wo', 8)
                wof = [sb(p5, "wof%d" % i, [128, D]) for i in range(2)]; b_wof = k.bufs('wof', 2)
                yl = [sb(p5, "yl%d" % i, [128, 8, 512], BF16) for i in range(2)]; b_yl = k.bufs('yl', 2)
                xt = [sb(p5, "x5t%d" % i, [128, D]) for i in range(2)]; b_xt = k.bufs('x5t', 2)
                x1t = [sb(p5, "x1t%d" % i, [128, D]) for i in range(2)]; b_x1t = k.bufs('x1t', 2)
                xn = [sb(p5, "x5n%d" % i, [128, D], BF16) for i in range(2)]; b_xn = k.bufs('x5n', 2)
                sq = sb(p5, "sq5", [128, D], BF16); b_sq = k.buf('sq5')
                st = [sb(p5, "st5_%d" % i, [128, 2]) for i in range(2)]; b_st = k.bufs('st5', 2)
                pO = [ps(p5, "pO%d" % i, [128, 512]) for i in range(4)]; b_pO = k.bufs('pO', 4)
                ptrA = [ps(p5, "ptr5A%d" % i, [128, 4, 128], BF16) for i in range(2)]; b_ptrA = k.bufs('ptr5A', 2)
                ptrB = [ps(p5, "ptr5B%d" % i, [128, 4, 128], BF16) for i in range(2)]; b_ptrB = k.bufs('ptr5B', 2)
                for j in range(8):
                    k.dma('sp', wof[j % 2][:], wout_d[j * 128:(j + 1) * 128, :], [bIN], [b_wof[j % 2]], b_wof[j % 2])
                    k.op('dve', lambda j=j: dve.tensor_tensor(out=wo[:, j, :], in0=wof[j % 2][:], in1=g1b[:], op=ALU.mult),
                         [b_wof[j % 2], b_g1b], [b_wo[j]])
                YTv = YT_d.rearrange("a p t -> p a t")

                def ld_yl(tb):
                    k.dma('sp', yl[tb % 2][:], YTv[:, :, tb * 512:(tb + 1) * 512], [bYT], [b_yl[tb % 2]], b_yl[tb % 2])

                def ld_x(i):
                    k.dma('sp', xt[i % 2][:], x_d[i * 128:(i + 1) * 128, :], [bIN], [b_xt[i % 2]], b_xt[i % 2])

                def stageA(i):
                    if i % 4 == 0 and i // 4 + 1 < 8:
                        ld_yl(i // 4 + 1)
                    Y = yl[(i // 4) % 2]; bY = b_yl[(i // 4) % 2]
                    X = xt[i % 2]; bX = b_xt[i % 2]
                    X1 = x1t[i % 2]; bX1t = b_x1t[i % 2]
                    for hf in range(2):
                        P = pO[(i * 2 + hf) % 4]; bP = b_pO[(i * 2 + hf) % 4]

                        def mmo(P=P, Y=Y, i=i, hf=hf):
                            last = None
                            for j in range(8):
                                last = pe.matmul(P[:], lhsT=Y[:, j, (i % 4) * 128:(i % 4 + 1) * 128], rhs=wo[:, j, hf * 512:(hf + 1) * 512],
                                                 start=(j == 0), stop=(j == 7))
                            return last
                        k.op('pe', mmo, [bY] + b_wo, [bP])
                        k.op('dve', lambda P=P, X=X, X1=X1, hf=hf: dve.tensor_tensor(
                            out=X1[:, hf * 512:(hf + 1) * 512], in0=P[:], in1=X[:, hf * 512:(hf + 1) * 512], op=ALU.add), [bP, bX], [bX1t])
                    if i + 2 < NT:
                        ld_x(i + 2)
                    k.dma('pool', X1_d[i * 128:(i + 1) * 128, :], X1[:], [bX1t], [bX1], bX1t)

                def stageB(i):
                    X1 = x1t[i % 2]; bX1t = b_x1t[i % 2]
                    S = st[i % 2]; bS = b_st[i % 2]
                    XN = xn[i % 2]; bXN = b_xn[i % 2]
                    PA = ptrA[i % 2]; bPA = b_ptrA[i % 2]; PB = ptrB[i % 2]; bPB = b_ptrB[i % 2]
                    k.op('act', lambda X1=X1, S=S: act.activation(out=sq[:], in_=X1[:], func=AF.Square, accum_out=S[:, 0:1]),
                         [bX1t], [b_sq, bS])
                    k.op('dve', lambda S=S: dve.tensor_scalar(out=S[:, 1:2], in0=S[:, 0:1], scalar1=1.0 / D, scalar2=EPS,
                                                              op0=ALU.mult, op1=ALU.add), [bS], [bS])
                    k.op('act', lambda S=S: act.activation(out=S[:, 1:2], in_=S[:, 1:2], func=AF.Sqrt), [bS], [bS])
                    k.op('dve', lambda S=S: dve.reciprocal(out=S[:, 1:2], in_=S[:, 1:2]), [bS], [bS])
                    k.op('dve', lambda X1=X1, S=S, XN=XN: dve.tensor_scalar(out=XN[:], in0=X1[:], scalar1=S[:, 1:2], scalar2=None, op0=ALU.mult),
                         [bX1t, bS], [bXN])

                    def tr5(XN=XN, PA=PA, PB=PB):
                        last = None
                        for j in range(8):
                            last = pe.transpose((PA if j % 2 == 0 else PB)[:, j // 2, :], XN[:, j * 128:(j + 1) * 128], idb[:])
                        return last
                    k.op('pe', tr5, [bXN, b_idb], [bPA, bPB])

                def stageB2(i):
                    PA = ptrA[i % 2]; bPA = b_ptrA[i % 2]; PB = ptrB[i % 2]; bPB = b_ptrB[i % 2]
                    for j in range(8):
                        if j % 2 == 1:
                            k.op('act', lambda j=j, PB=PB, i=i: act.activation(
                                out=hx2T[:, j, i * 128:(i + 1) * 128], in_=PB[:, j // 2, :], func=AF.Identity,
                                scale=s2[:, j:j + 1], bias=modT[:, 24 + j, 0:1]), [bPB, b_s2, b_modT], [], aw=[b_hx2T[i]])
                        else:
                            k.op('dve', lambda j=j, PA=PA, i=i: dve.tensor_scalar(
                                out=hx2T[:, j, i * 128:(i + 1) * 128], in0=PA[:, j // 2, :], scalar1=s2[:, j:j + 1],
                                scalar2=modT[:, 24 + j, 0:1], op0=ALU.mult, op1=ALU.add), [bPA, b_s2, b_modT], [], aw=[b_hx2T[i]])
                ld_yl(0); ld_x(0); ld_x(1)
                stageA(0)
                for i in range(NT + 1):
                    if i + 1 < NT:
                        stageA(i + 1)
                    if i < NT:
                        stageB(i)
                    if i >= 1:
                        stageB2(i - 1)
                k.barrier()
            with contextlib.ExitStack() as p6:
                wu = [sb(p6, "wu%d" % i, [128, 8, 2, 128], BF16) for i in range(2)]; b_wu = [k.bufs('wu', 2) for i in range(2)]
                wuf = [sb(p6, "wuf%d" % i, [128, 8, 2, 128]) for i in range(2)]; b_wuf = [k.bufs('wuf', 2) for i in range(2)]
                dg = [sb(p6, "dg%d" % i, [128, 2, 9, 128], BF16) for i in range(2)]; b_dg = k.bufs('dg', 2)
                upad = [sb(p6, "upad%d" % i, [128, 2, 66 * 66], BF16) for i in range(2)]
                b_up = [k.bufs('upad', 2) for i in range(2)]
                vs = [sb(p6, "vs%d" % i, [128, 512]) for i in range(2)]; b_vs = k.bufs('vs', 2)
                gs = [sb(p6, "gs%d" % i, [128, 512]) for i in range(2)]; b_gs = k.bufs('gs', 2)
                hst = [sb(p6, "hst%d" % i, [128, 512], BF16) for i in range(2)]; b_hst = k.bufs('hst', 2)
                pU = [ps(p6, "pU%d" % i, [128, 512]) for i in range(3)]; b_pU = k.bufs('pU', 3)
                pV = [ps(p6, "pV%d" % i, [128, 512]) for i in range(4)]; b_pV = k.bufs('pV', 4)
                for i in range(2):
                    k.op('dve', lambda i=i: dve.memset(upad[i][:], 0.0), [], b_up[i])
                wupv = wup_d.rearrange("(kk p) n -> p kk n", p=128)
                nu = 0
                nv = 0
                def ld_wu(jj):
                    for v in range(2):
                        c0 = v * 2560 + jj * 128
                        k.dma('sp', wuf[jj % 2][:, :, v, :], wupv[:, :, c0:c0 + 128], [bIN], [b_wuf[jj % 2][v]], b_wuf[jj % 2][v])
                ld_wu(0)
                for jj in range(20):
                    bi = jj % 2
                    if jj + 1 < 20:
                        ld_wu(jj + 1)
                    for v in range(2):
                        k.op('dve', lambda bi=bi, v=v: dve.tensor_copy(out=wu[bi][:, :, v, :], in_=wuf[bi][:, :, v, :]),
                             [b_wuf[bi][v]], [b_wu[bi][v]])
                    prep_wd(jj)
                    for v in range(2):
                        ch = v * 20 + jj
                        for t in range(9):
                            k.op('dve', lambda bi=bi, v=v, t=t, ch=ch: dve.tensor_scalar(
                                out=dg[bi][:, v, t, :], in0=idb[:], scalar1=vec[:, V_FCW + ch * 9 + t:V_FCW + ch * 9 + t + 1], scalar2=None,
                                op0=ALU.mult), [b_idb, b_vec], [b_dg[bi]])
                    for v in range(2):
                        ch = v * 20 + jj
                        g3 = upad[bi][:, v, :].rearrange("p (r c) -> p r c", c=66)
                        for tt in range(8):
                            P = pU[nu % 3]; bP = b_pU[nu % 3]; nu += 1

                            def mmu(P=P, bi=bi, v=v, tt=tt):
                                last = None
                                for kk in range(8):
                                    last = pe.matmul(P[:], lhsT=wu[bi][:, kk, v, :], rhs=hx2T[:, kk, tt * 512:(tt + 1) * 512],
                                                     start=(kk == 0), stop=(kk == 7))
                                return last
                            k.op('pe', mmu, [b_wu[bi][v]] + b_hx2T[tt * 4:(tt + 1) * 4], [bP])
                            k.op('act', lambda P=P, g3=g3, tt=tt, ch=ch: act.activation(
                                out=g3[:, 1 + 8 * tt:9 + 8 * tt, 1:65], in_=P[:].rearrange("p (r c) -> p r c", c=64), func=AF.Identity,
                                bias=vec[:, V_BUP + ch:V_BUP + ch + 1]), [bP, b_vec], [b_up[bi][v]])
                    for tt in range(8):
                        outs = []
                        for v in range(2):
                            ch = v * 20 + jj
                            g3 = upad[bi][:, v, :].rearrange("p (r c) -> p r c", c=66)
                            P = pV[nv % 4]; bP = b_pV[nv % 4]; nv += 1

                            def mmv(P=P, bi=bi, v=v, tt=tt, g3=g3):
                                last = None
                                for t in range(9):
                                    dr, dc_ = t // 3 - 1, t % 3 - 1
                                    last = pe.matmul(P[:].rearrange("p (r c) -> p r c", c=64), lhsT=dg[bi][:, v, t, :],
                                                     rhs=g3[:, 8 * tt + dr + 1:8 * tt + dr + 9, dc_ + 1:dc_ + 65],
                                                     start=(t == 0), stop=(t == 8))
                                return last
                            k.op('pe', mmv, [b_dg[bi], b_up[bi][v]], [bP])
                            outs.append((P, bP, ch))
                        n2 = (jj * 8 + tt) % 2
                        (Pa, bPa, cha), (Pb, bPb, chb) = outs
                        k.op('act', lambda Pa=Pa, n2=n2, cha=cha: act.activation(out=vs[n2][:], in_=Pa[:], func=AF.Identity,
                                                                              bias=vec[:, V_FCB + cha:V_FCB + cha + 1]),
                             [bPa, b_vec], [b_vs[n2]])
                        k.op('act', lambda Pb=Pb, n2=n2, chb=chb: act.activation(out=gs[n2][:], in_=Pb[:], func=AF.Silu,
                                                                              bias=vec[:, V_FCB + chb:V_FCB + chb + 1]),
                             [bPb, b_vec], [b_gs[n2]])
                        k.op('dve', lambda n2=n2: dve.tensor_tensor(out=hst[n2][:], in0=vs[n2][:], in1=gs[n2][:], op=ALU.mult),
                             [b_vs[n2], b_gs[n2]], [b_hst[n2]])
                        k.dma('pool', HT_d[jj, :, tt * 512:(tt + 1) * 512], hst[n2][:], [b_hst[n2]], [bHT], b_hst[n2])
                k.barrier()
        with contextlib.ExitStack() as p7:
            hl = [sb(p7, "hl%d" % i, [128, 20, 512], BF16) for i in range(2)]; b_hl = k.bufs('hl', 2)
            x1l = [sb(p7, "x1l%d" % i, [128, D]) for i in range(2)]; b_x1l = k.bufs('x1l', 2)
            x2 = [sb(p7, "x2_%d" % i, [128, D]) for i in range(2)]; b_x2 = k.bufs('x2', 2)
            ot = [sb(p7, "ot%d" % i, [128, D]) for i in range(2)]; b_ot = k.bufs('ot', 2)
            sq = sb(p7, "sq7", [128, D], BF16); b_sq = k.buf('sq7')
            st = [sb(p7, "st7_%d" % i, [128, 2]) for i in range(2)]; b_st = k.bufs('st7', 2)
            pD = [ps(p7, "pD%d" % i, [128, 512]) for i in range(4)]; b_pD = k.bufs('pD', 4)
            HTv = HT_d.rearrange("a p t -> p a t")
            def ld_hl(tb):
                k.dma('sp', hl[tb % 2][:], HTv[:, :, tb * 512:(tb + 1) * 512], [bHT], [b_hl[tb % 2]], b_hl[tb % 2])

            def ld_x1(i):
                k.dma('sp', x1l[i % 2][:], X1_d[i * 128:(i + 1) * 128, :], [bX1], [b_x1l[i % 2]], b_x1l[i % 2])
            ld_hl(0); ld_x1(0); ld_x1(1)
            for i in range(NT):
                if i % 4 == 0 and i // 4 + 1 < 8:
                    ld_hl(i // 4 + 1)
                Hh = hl[(i // 4) % 2]; bHh = b_hl[(i // 4) % 2]
                XL = x1l[i % 2]; bXL = b_x1l[i % 2]
                X2 = x2[i % 2]; bX2 = b_x2[i % 2]
                O = ot[i % 2]; bO = b_ot[i % 2]
                S = st[i % 2]; bS = b_st[i % 2]
                k.op('dve', lambda XL=XL: dve.tensor_tensor(out=XL[:], in0=XL[:], in1=bdg[:], op=ALU.add), [bXL, b_bdg], [bXL])
                for hf in range(2):
                    P = pD[(i * 2 + hf) % 4]; bP = b_pD[(i * 2 + hf) % 4]

                    def mmd(P=P, Hh=Hh, i=i, hf=hf):
                        last = None
                        for j in range(20):
                            last = pe.matmul(P[:], lhsT=Hh[:, j, (i % 4) * 128:(i % 4 + 1) * 128], rhs=wd[:, j, hf * 512:(hf + 1) * 512],
                                             start=(j == 0), stop=(j == 19))
                        return last
                    k.op('pe', mmd, [bHh] + b_wd, [bP])
                    k.op('dve', lambda P=P, XL=XL, X2=X2, hf=hf: dve.tensor_tensor(
                        out=X2[:, hf * 512:(hf + 1) * 512], in0=P[:], in1=XL[:, hf * 512:(hf + 1) * 512], op=ALU.add), [bP, bXL], [bX2])
                k.op('act', lambda X2=X2, S=S: act.activation(out=sq[:], in_=X2[:], func=AF.Square, accum_out=S[:, 0:1]),
                     [bX2], [b_sq, bS])
                k.op('dve', lambda S=S: dve.tensor_scalar(out=S[:, 1:2], in0=S[:, 0:1], scalar1=1.0 / D, scalar2=EPS,
                                                          op0=ALU.mult, op1=ALU.add), [bS], [bS])
                k.op('act', lambda S=S: act.activation(out=S[:, 1:2], in_=S[:, 1:2], func=AF.Sqrt), [bS], [bS])
                k.op('dve', lambda S=S: dve.reciprocal(out=S[:, 1:2], in_=S[:, 1:2]), [bS], [bS])
                k.op('dve', lambda X2=X2, S=S, O=O: dve.scalar_tensor_tensor(out=O[:], in0=X2[:], scalar=S[:, 1:2], in1=fnwb[:],
                                                                           op0=ALU.mult, op1=ALU.mult), [bX2, bS, b_fnwb], [bO])
                if i + 2 < NT:
                    ld_x1(i + 2)
                k.dma('pool', out_d[i * 128:(i + 1) * 128, :], O[:], [bO], [], bO)
            k.barrier()
        gw7.close()


def _consts():
    f32 = np.float32
    bf = ml_dtypes.bfloat16
    c = {}
    c['ident_bf'] = np.eye(128, dtype=f32).astype(bf)
    c['ident_f'] = np.eye(128, dtype=f32)
    j = np.arange(128)[:, None]
    l = np.arange(128)[None, :]
    c['masks'] = np.concatenate([(j <= l), (j >= l)], axis=1).astype(f32)
    sel = np.zeros((4, 4, 128), f32)
    for h in range(4):
        sel[h, h, :] = 1.0
    c['sel'] = sel.reshape(4, 512)
    l1 = np.arange(128, dtype=np.float64)[:, None, None]
    l2 = np.arange(32, dtype=np.float64)[None, :, None]
    k1 = np.arange(128, dtype=np.float64)[None, None, :]
    th = 2 * np.pi * (l1 * k1 / 128.0 + l2 * k1 / 4096.0)
    t1 = np.stack([np.cos(th), np.sin(th)], axis=2) / np.sqrt(128.0)
    c['dft1'] = t1.reshape(128, 32 * 256).astype(f32).astype(bf)
    a = np.arange(32, dtype=np.float64)
    ph = 2 * np.pi * np.outer(a, a) / 32.0
    cph, sph = np.cos(ph), np.sin(ph)
    w2 = np.block([[cph, sph], [-sph, cph]]) / np.sqrt(32.0)
    c['dft2'] = np.concatenate([w2, np.zeros_like(w2)], axis=0).astype(f32).astype(bf)
    b = np.arange(128, dtype=np.float64)
    ps_ = 2 * np.pi * np.outer(b, b) / 128.0
    c['dftc'] = (np.concatenate([np.cos(ps_), -np.sin(ps_)], axis=1) / np.sqrt(128.0)).astype(f32).astype(bf)
    return c


def prep_inputs(inp):
    f32 = np.float32
    g = lambda n: np.asarray(inp[n], dtype=f32)
    consts = _consts()
    pm = lambda v: np.ascontiguousarray(v.reshape(-1, 128).T)
    vec = np.zeros((128, V_END), f32)
    vec[:, V_N1W:V_N1W + 8] = pm(g('norm1_w')[0])
    vec[:, V_N2W:V_N2W + 8] = pm(g('norm2_w')[0])
    vec[:, V_BADA:V_BADA + 48] = pm(g('b_ada')[0])
    mcw = g('mconv_w')[0]
    for t in range(3):
        vec[:, V_MCW + np.arange(4) * 3 + t] = pm(mcw[t])
    vec[:, V_MCB:V_MCB + 4] = pm(g('mconv_b')[0])
    vec[:, V_MNW:V_MNW + 4] = pm(g('mnorm_w')[0])
    vec[:, V_MSK:V_MSK + 4] = pm(g('m_skip')[0])
    vec[:, V_BUP:V_BUP + 40] = pm(g('b_up')[0])
    fcw = g('fconv_w')[0].reshape(9, 5120)
    for t in range(9):
        vec[:, V_FCW + np.arange(40) * 9 + t] = pm(fcw[t])
    vec[:, V_FCB:V_FCB + 40] = pm(g('fconv_b')[0])
    vec[0:16, V_BG] = g('b_gate')[0]
    shared = {
        'vecs': vec,
        'b_ada': g('b_ada'), 'b_down': g('b_down'), 'final_norm_w': g('final_norm_w').reshape(1, D),
        'w_ada': g('w_ada')[0], 'w_in': g('w_in')[0], 'w_q': g('w_q')[0], 'w_k': g('w_k')[0],
        'w_out': g('w_out')[0], 'w_up': g('w_up')[0], 'w_down': g('w_down')[0],
    }
    shared.update(consts)
    x = g('x'); ctx = g('ctx'); c = g('c'); cctx = g('c_ctx')
    maps = []
    for b in range(8):
        cc = np.stack([pm(c[b]), pm(cctx)], axis=2).reshape(128, 16)
        m = dict(shared)
        m['x'] = x[b]
        m['ctx'] = ctx[b]
        m['cc'] = np.ascontiguousarray(cc)
        maps.append(m)
    return maps


def kernel(**inputs):
    nc = build()
    maps = prep_inputs(inputs)
    res = run_bass_kernel_spmd(nc, maps, core_ids=list(range(8)))
    return np.stack([np.asarray(r['out'], dtype=np.float32) for r in res.results], axis=0)
```
